# Optimizing a Trainium2 kernel written in Bass

```python
import jax, jax.numpy as jnp
from jax import lax
import numpy as np

D_MODEL = 4096
BATCH = 4
SEQ = 4096
DEPTH = 4

N_MIXERS = 2
N_CONV_LAYERS = (DEPTH + 1) // 2
N_MLSTM_LAYERS = DEPTH // 2
COND_RANK = 512
CONV_WIDTH = 31
MLSTM_HEADS = 8
MLSTM_DQK = D_MODEL // (2 * MLSTM_HEADS)
MLSTM_DV = D_MODEL // MLSTM_HEADS
MLSTM_CHUNK = 64
MLSTM_PROJ = MLSTM_HEADS * (2 * MLSTM_DQK + 2 * MLSTM_DV + 2)
D_FF = 2 * D_MODEL
N_EXPERTS = 8
TOP_K = 2
D_FF_EXPERT = D_MODEL // 2
EPS = 1e-6

kernel_name = "hybrid_conv_mlstm_moe_adaln_block"


def rms_norm(x, g):
    xf = x.astype(jnp.float32)
    y = xf * lax.rsqrt(jnp.mean(xf * xf, axis=-1, keepdims=True) + EPS)
    return (y * g.astype(jnp.float32)).astype(x.dtype)


def layer_norm(x, g, b):
    xf = x.astype(jnp.float32)
    mu = jnp.mean(xf, axis=-1, keepdims=True)
    var = jnp.mean(jnp.square(xf - mu), axis=-1, keepdims=True)
    return ((xf - mu) * lax.rsqrt(var + EPS) * g.astype(jnp.float32) + b.astype(jnp.float32)).astype(x.dtype)


def swiglu(t, w_gate, w_up, w_down):
    return (jax.nn.silu(t @ w_gate) * (t @ w_up)) @ w_down


def conformer_conv(h, w_in, b_in, w_dw, b_dw, ln_g, ln_b, w_out, b_out):
    u = h @ w_in + b_in
    val, gate = jnp.split(u, 2, axis=-1)
    u = val * jax.nn.sigmoid(gate)
    u = lax.conv_general_dilated(
        u, w_dw[:, None, :], window_strides=(1,), padding=[(CONV_WIDTH - 1, 0)],
        dimension_numbers=("NWC", "WIO", "NWC"), feature_group_count=D_MODEL) + b_dw
    u = jax.nn.silu(layer_norm(u, ln_g, ln_b))
    return u @ w_out + b_out


def mlstm_chunk_step(carry, xs):
    C, n, m = carry
    q, k, v, ig, lf = xs
    L = q.shape[2]
    causal = jnp.tril(jnp.ones((L, L), dtype=bool))
    b = jnp.cumsum(lf, axis=-1)
    a = b + m[..., None]
    dmat = jnp.where(causal, b[..., :, None] - b[..., None, :] + ig[..., None, :], -jnp.inf)
    m_s = jnp.maximum(a, jnp.max(dmat, axis=-1))
    w_inter = jnp.exp(a - m_s)
    s = jnp.einsum("bhsd,bhjd->bhsj", q, k) * jnp.exp(dmat - m_s[..., None])
    num = jnp.einsum("bhsj,bhjv->bhsv", s, v) + w_inter[..., None] * jnp.einsum("bhsd,bhdv->bhsv", q, C)
    den = jnp.sum(s, axis=-1) + w_inter * jnp.einsum("bhsd,bhd->bhs", q, n)
    h = num / jnp.maximum(jnp.abs(den), jnp.exp(-m_s))[..., None]
    b_end = b[..., -1]
    g = b_end[..., None] - b + ig
    m_new = jnp.maximum(b_end + m, jnp.max(g, axis=-1))
    decay = jnp.exp(b_end + m - m_new)
    wk = k * jnp.exp(g - m_new[..., None])[..., None]
    C = decay[..., None, None] * C + jnp.einsum("bhjd,bhjv->bhdv", wk, v)
    n = decay[..., None] * n + jnp.sum(wk, axis=-2)
    return (C, n, m_new), h


def mlstm_mixer(h, w_in, b_gates, norm_g, w_out):
    B, S, _ = h.shape
    H, DK, DV, L = MLSTM_HEADS, MLSTM_DQK, MLSTM_DV, MLSTM_CHUNK
    NC = S // L
    f32 = jnp.float32
    proj = h @ w_in
    q, k, v, o, gates = jnp.split(proj, [H * DK, 2 * H * DK, 2 * H * DK + H * DV, 2 * H * DK + 2 * H * DV], axis=-1)
    gates = gates.astype(f32) + b_gates.astype(f32)
    ig = gates[..., :H]
    lf = jax.nn.log_sigmoid(gates[..., H:])

    def to_chunks(t, d):
        return t.astype(f32).reshape(B, NC, L, H, d).transpose(1, 0, 3, 2, 4)

    def gate_chunks(t):
        return t.reshape(B, NC, L, H).transpose(1, 0, 3, 2)

    xs = (to_chunks(q, DK), to_chunks(k, DK) * (DK ** -0.5), to_chunks(v, DV), gate_chunks(ig), gate_chunks(lf))
    init = (jnp.zeros((B, H, DK, DV), f32), jnp.zeros((B, H, DK), f32), jnp.zeros((B, H), f32))
    _, hc = lax.scan(mlstm_chunk_step, init, xs)
    hs = hc.transpose(1, 0, 3, 2, 4).reshape(B, S, H, DV)
    hs = hs * lax.rsqrt(jnp.mean(hs * hs, axis=-1, keepdims=True) + EPS)
    hs = hs.reshape(B, S, H * DV) * norm_g.astype(f32)
    y = (hs * jax.nn.sigmoid(o.astype(f32))).astype(h.dtype)
    return y @ w_out


def moe_swiglu(h, w_router, b_router, w_gate, w_up, w_down):
    B, S, D = h.shape
    t = h.reshape(B * S, D)
    logits = (t @ w_router).astype(jnp.float32) + b_router.astype(jnp.float32)
    top_vals, top_idx = lax.top_k(logits, TOP_K)
    top_w = jax.nn.softmax(top_vals, axis=-1)
    combine = jnp.sum(jax.nn.one_hot(top_idx, N_EXPERTS, dtype=jnp.float32) * top_w[..., None], axis=1)
    combine = combine.astype(t.dtype)
    out = jnp.zeros_like(t)
    for e in range(N_EXPERTS):
        out = out + combine[:, e:e + 1] * swiglu(t, w_gate[e], w_up[e], w_down[e])
    return out.reshape(B, S, D)


def setup_inputs(seed: int = 0) -> dict:
    key = jax.random.key(seed)
    ks = list(jax.random.split(key, 40))
    D, R, E = D_MODEL, COND_RANK, N_EXPERTS
    NA, NB = N_CONV_LAYERS, N_MLSTM_LAYERS
    H = MLSTM_HEADS

    def nrm(shape, scale):
        return jax.random.normal(ks.pop(), shape, jnp.float32) * scale

    def gain(shape):
        return 1.0 + nrm(shape, 0.05)

    b_i = nrm((NB, H), 0.1)
    b_f = jnp.linspace(3.0, 6.0, H, dtype=jnp.float32)[None, :] + nrm((NB, H), 0.1)
    return {
        "x": nrm((BATCH, SEQ, D), 1.0),
        "c": nrm((BATCH, D), 1.0),
        "cond_w": nrm((D, R), D ** -0.5),
        "cond_b": nrm((R,), 0.02),
        "ada_w": nrm((DEPTH, R, 6 * D), 0.5 * R ** -0.5),
        "ada_b": nrm((DEPTH, 6 * D), 0.02),
        "mix_norm_g": gain((DEPTH, D)),
        "ffn_norm_g": gain((DEPTH, D)),
        "final_norm_g": gain((D,)),
        "conv_w_in": nrm((NA, D, 2 * D), D ** -0.5),
        "conv_b_in": nrm((NA, 2 * D), 0.02),
        "conv_w_dw": nrm((NA, CONV_WIDTH, D), CONV_WIDTH ** -0.5),
        "conv_b_dw": nrm((NA, D), 0.02),
        "conv_ln_g": gain((NA, D)),
        "conv_ln_b": nrm((NA, D), 0.02),
        "conv_w_out": nrm((NA, D, D), D ** -0.5),
        "conv_b_out": nrm((NA, D), 0.02),
        "mlstm_w_in": nrm((NB, D, MLSTM_PROJ), D ** -0.5),
        "mlstm_b_gates": jnp.concatenate([b_i, b_f], axis=-1),
        "mlstm_norm_g": gain((NB, H * MLSTM_DV)),
        "mlstm_w_out": nrm((NB, H * MLSTM_DV, D), (H * MLSTM_DV) ** -0.5),
        "ffn_w_gate": nrm((NA, D, D_FF), D ** -0.5),
        "ffn_w_up": nrm((NA, D, D_FF), D ** -0.5),
        "ffn_w_down": nrm((NA, D_FF, D), D_FF ** -0.5),
        "moe_w_router": nrm((NB, D, E), D ** -0.5),
        "moe_b_router": nrm((NB, E), 0.01),
        "moe_w_gate": nrm((NB, E, D, D_FF_EXPERT), D ** -0.5),
        "moe_w_up": nrm((NB, E, D, D_FF_EXPERT), D ** -0.5),
        "moe_w_down": nrm((NB, E, D_FF_EXPERT, D), D_FF_EXPERT ** -0.5),
    }


def reference(x, c, cond_w, cond_b, ada_w, ada_b, mix_norm_g, ffn_norm_g, final_norm_g,
              conv_w_in, conv_b_in, conv_w_dw, conv_b_dw, conv_ln_g, conv_ln_b, conv_w_out, conv_b_out,
              mlstm_w_in, mlstm_b_gates, mlstm_norm_g, mlstm_w_out,
              ffn_w_gate, ffn_w_up, ffn_w_down,
              moe_w_router, moe_b_router, moe_w_gate, moe_w_up, moe_w_down):
    e = jax.nn.silu(c @ cond_w + cond_b)
    for i in range(DEPTH):
        j = i // N_MIXERS
        mod = (e @ ada_w[i] + ada_b[i])[:, None, :]
        shift1, scale1, gate1, shift2, scale2, gate2 = jnp.split(mod, 6, axis=-1)
        h = rms_norm(x, mix_norm_g[i]) * (1.0 + scale1) + shift1
        if i % N_MIXERS == 0:
            y = conformer_conv(h, conv_w_in[j], conv_b_in[j], conv_w_dw[j], conv_b_dw[j],
                               conv_ln_g[j], conv_ln_b[j], conv_w_out[j], conv_b_out[j])
        else:
            y = mlstm_mixer(h, mlstm_w_in[j], mlstm_b_gates[j], mlstm_norm_g[j], mlstm_w_out[j])
        x = x + gate1 * y
        h = rms_norm(x, ffn_norm_g[i]) * (1.0 + scale2) + shift2
        if i % 2 == 0:
            y = swiglu(h, ffn_w_gate[j], ffn_w_up[j], ffn_w_down[j])
        else:
            y = moe_swiglu(h, moe_w_router[j], moe_b_router[j], moe_w_gate[j], moe_w_up[j], moe_w_down[j])
        x = x + gate2 * y
    return rms_norm(x, final_norm_g)
```

```python
import numpy as np
import concourse.bass as bass
import concourse.mybir as mybir
from concourse.bass_utils import run_bass_kernel_spmd

F32 = mybir.dt.float32
BF16 = mybir.dt.bfloat16
AF = mybir.ActivationFunctionType
ALU = mybir.AluOpType

D = 4096
KC = 32
TP = 1024
NPASS = 2
TCORE = TP * NPASS
NT = TP // 128
R = 512
CW = 31
HALO = 32
H = 8
DK = 256
DV = 512
LCH = 64
NPROJ = 12304
EPS = 1e-6
NCORES = 8

ENGS = ("tensor", "vector", "scalar", "gpsimd", "sync")
SEM_LIMIT = 1 << 40


class Prog:
    def __init__(self, nc):
        self.nc = nc
        self.q = {e: [] for e in ENGS}
        self.esem = {e: nc.alloc_semaphore(name=f"es_{e}") for e in ENGS if e != "sync"}
        self.ecnt = {e: 0 for e in ENGS}
        self.waited = {e: {} for e in ENGS}
        self.rings = {}
        self.n_inst = 0

    def _wait(self, eng, deps):
        for d in deps:
            if d is None:
                continue
            sem, val = d
            key = id(sem)
            if self.waited[eng].get(key, 0) >= val:
                continue
            self.waited[eng][key] = val
            self.q[eng].append(lambda e, sem=sem, val=val: e.wait_ge(sem, val))
            self.n_inst += 1

    def op(self, eng, fn, deps=(), sig=True):
        self._wait(eng, deps)
        self.n_inst += 1
        if sig:
            if self.ecnt[eng] >= SEM_LIMIT:
                self.esem[eng] = self.nc.alloc_semaphore(name=f"es_{eng}_{self.n_inst}")
                self.ecnt[eng] = 0
            self.ecnt[eng] += 1
            n = self.ecnt[eng]
            sem = self.esem[eng]
            self.q[eng].append(lambda e, fn=fn, sem=sem: fn(e).then_inc(sem, 1))
            return (sem, n)
        self.q[eng].append(lambda e, fn=fn: fn(e))
        return None

    def dma(self, queue, out, in_, ring, nring=1, deps=()):
        r = self.rings.setdefault(ring, {"i": 0, "sems": [], "cnt": []})
        j = r["i"] % nring
        r["i"] += 1
        if len(r["sems"]) <= j:
            r["sems"].append(self.nc.alloc_semaphore(name=f"ds_{ring}_{j}"))
            r["cnt"].append(0)
        sem = r["sems"][j]
        deps = list(deps)
        if r["cnt"][j] > 0:
            deps.append((sem, r["cnt"][j]))
        self._wait(queue, deps)
        if r["cnt"][j] >= SEM_LIMIT:
            sem = self.nc.alloc_semaphore(name=f"ds_{ring}_{j}_{self.n_inst}")
            r["sems"][j] = sem
            r["cnt"][j] = 0
        r["cnt"][j] += 16
        self.n_inst += 1
        self.q[queue].append(lambda e, out=out, in_=in_, sem=sem: e.dma_start(out=out, in_=in_).then_inc(sem, 16))
        return (sem, r["cnt"][j])

    def emit(self, final_waits):
        nc = self.nc
        self._wait("sync", final_waits)
        with nc.Block() as block:
            @block.tensor
            def _(e):
                for f in self.q["tensor"]:
                    f(e)

            @block.vector
            def _(e):
                for f in self.q["vector"]:
                    f(e)

            @block.scalar
            def _(e):
                for f in self.q["scalar"]:
                    f(e)

            @block.gpsimd
            def _(e):
                for f in self.q["gpsimd"]:
                    f(e)

            @block.sync
            def _(e):
                for f in self.q["sync"]:
                    f(e)


class Ctx:
    def __init__(self, nc, P):
        self.nc = nc
        self.P = P
        a = nc.alloc_sbuf_tensor
        self.RA = a("RA", [128, 16384], F32)
        self.RB = a("RB", [128, 8192], F32)
        self.RC = a("RC", [128, 8192], F32)
        self.RD = a("RD", [128, 8192], F32)
        self.RE = a("RE", [128, 4096], F32)
        self.RF = a("RF", [128, 4096], F32)
        self.XS = a("XS", [128, 2, 512], F32)
        self.TMP = a("TMP", [128, 2, 512], F32)
        self.ident = a("ident_sb", [128, 128], BF16)
        self.identf = None
        self.small = a("small", [128, 256], F32)
        self.HHt = a("HH", [128, KC * 32], BF16)
        self.HH = self.HHt[:, :].rearrange("p (k t) -> p k t", k=KC)
        self.ones_bf = a("ones_bf", [128, 128], BF16)
        self.epsb = self.small[:, 60:61]
        self.oneb = self.small[:, 62:63]
        self.WRt = a("WRt", [128, KC * 16], BF16)
        self.comb = a("comb", [128, NT, 8], F32)
        self.comb_free = []
        self.nbt = a("nbt", [128, 16], BF16)
        self.trif = a("trif", [64, 64], F32)
        self.onesf = a("onesf", [64, 128], F32)
        self.gwork = a("gwork", [64, 128], F32)
        self.decb = a("decb", [128, 8], F32)
        self.smt = a("smt", [64, 128], BF16)
        self.hwork = a("hwork", [64, 16], F32)
        self.junk = self.HHt[:, 0:512]
        self.wkt = self.HHt[:, 512:1024]
        self.ybt = self.RD[:, 6144:8192].bitcast(BF16)
        self.sm_free = [None, None]
        self.hw_free = [None, None]
        self.wk_free = [None, None]
        self.pn_free = [None, None]
        self.ps = [nc.alloc_psum_tensor(f"ps{i}", [128, 512], F32) for i in range(8)]
        self.hT = self.RA[:, :].bitcast(BF16).rearrange("p (k t) -> p k t", k=KC)
        self.midT = self.RB[:, :].bitcast(BF16).rearrange("p (k t) -> p k t", k=16)
        self.xt = [self.RB[:, 0:4096], self.RB[:, 4096:8192]]
        self.wA = [self.RC[:, i * 4096:(i + 1) * 4096].bitcast(BF16).rearrange("p (k c) -> p k c", k=KC) for i in range(2)]
        self.wB16 = [self.RD[:, i * 4096:(i + 1) * 4096].bitcast(BF16).rearrange("p (k c) -> p k c", k=16) for i in range(2)]
        self.wB32 = [self.RD[:, i * 4096:(i + 1) * 4096].bitcast(BF16).rearrange("p (k c) -> p k c", k=KC) for i in range(2)]
        self.geff = self.RD[:, 0:4096]
        self.shift = self.RD[:, 4096:8192]
        self.gate = self.RE[:, :]
        self.hb = [self.RF[:, i * 2048:(i + 1) * 2048].bitcast(BF16) for i in range(2)]
        self.free = {k: [] for k in ("RA", "RB", "RC", "RD", "RE", "RF")}
        self.psfree = [None] * 8
        self.xs_free = [None, None]
        self.tmp_free = [None, None]
        self.xs_i = 0
        self.tmp_i = 0


def load_consts(cx, ident_d, identf_d):
    P = cx.P
    t1 = P.dma("sync", cx.ident[:, :], ident_d, "const")
    t2 = P.dma("sync", cx.identf[:, :], identf_d, "const2")
    return [t1, t2]


def phase_mods(cx, cT_d, condw_d, condbT_d, adaw_d, adab_d, mod_d, start_deps, nblk=None):
    P, nc = cx.P, cx.nc
    sm = cx.small
    cT = sm[:, 0:32]
    cb = sm[:, 32:36]
    eT = sm[:, 36:40]
    deps0 = list(start_deps) + cx.free["RB"] + cx.free["RF"]
    t_c = P.dma("sync", cT, cT_d, "mods_in", 2, deps=deps0)
    t_cb = P.dma("sync", cb, condbT_d, "mods_in", 2)
    cw_v = condw_d.rearrange("(kc p) r -> p kc r", p=128)
    psb = cx.ps[6]
    last = None
    act_tok = None
    for j in range(4):
        buf = cx.xt[j % 2].rearrange("p (k c) -> p k c", k=KC)
        tw = P.dma("sync", buf, cw_v[:, :, j * 128:(j + 1) * 128], "mods_w", 2, deps=deps0 + [act_tok])
        for k in range(KC):
            mm = P.op("tensor", lambda e, buf=buf, k=k: e.matmul(psb[:, 0:1], buf[:, k, :], cT[:, k:k + 1],
                                                               start=(k == 0), stop=(k == KC - 1)),
                      deps=[tw, t_c, cx.psfree[6]] if k == 0 else [], sig=(k == KC - 1))
        act_tok = P.op("scalar", lambda e, j=j: e.activation(out=eT[:, j:j + 1], in_=psb[:, 0:1], func=AF.Silu,
                                                             bias=cb[:, j:j + 1], scale=1.0), deps=[mm, t_cb])
        cx.psfree[6] = act_tok
    aw_v = adaw_d.rearrange("(j p) c -> p j c", p=128)
    nblk = (6 * D) // 1024 if nblk is None else nblk
    row = cx.hb[0].bitcast(F32)
    ev_tok = [None, None]
    st_tok = [None, None]
    stores = []
    for b in range(nblk):
        buf = cx.xt[b % 2].rearrange("p (j c) -> p j c", j=4)
        tw = P.dma("sync", buf, aw_v[:, :, b * 1024:(b + 1) * 1024], "mods_w", 2, deps=[act_tok, ev_tok[b % 2]])
        slot = b % 2
        brow = row[0:1, slot * 1024:(slot + 1) * 1024]
        tb = P.dma("sync", brow, adab_d[0:1, b * 1024:(b + 1) * 1024], "mods_b", 2, deps=[st_tok[slot]] + deps0)
        evs = []
        for hf in range(2):
            pst = cx.ps[6 + hf]
            for j in range(4):
                mm = P.op("tensor", lambda e, buf=buf, j=j, hf=hf, pst=pst: e.matmul(
                    pst[0:1, :], eT[:, j:j + 1], buf[:, j, hf * 512:(hf + 1) * 512], start=(j == 0), stop=(j == 3)),
                    deps=[tw, act_tok, cx.psfree[6 + hf]] if j == 0 else [], sig=(j == 3))
            ev = P.op("vector", lambda e, pst=pst, brow=brow, hf=hf: e.tensor_tensor(
                out=brow[:, hf * 512:(hf + 1) * 512], in0=pst[0:1, :], in1=brow[:, hf * 512:(hf + 1) * 512], op=ALU.add),
                deps=[mm, tb])
            cx.psfree[6 + hf] = ev
            evs.append(ev)
        ev_tok[b % 2] = evs[-1]
        st = P.dma("sync", mod_d[0:1, b * 1024:(b + 1) * 1024], brow, "mods_st", 2, deps=evs)
        st_tok[slot] = st
        stores.append(st)
    cx.free["RB"] = [ev_tok[0], ev_tok[1]]
    cx.free["RF"] = stores[-2:]
    return stores[-2:]


def load_bc(cx, dst, row_ap, ring, deps):
    return cx.P.dma("sync", dst, row_ap.to_broadcast([128, row_ap.shape[-1]]), ring, 1, deps=deps)


def phase_prep(cx, x_rows, g_row, mod_d, i_scale, i_shift, i_gate, x_ready, mod_ready, n_tiles=NT, tok_off=0, halo=False):
    P = cx.P
    pre = list(x_ready) + list(mod_ready)
    d_rd = pre + cx.free["RD"]
    d_rb = pre + cx.free["RB"]
    t_sc = load_bc(cx, cx.geff, mod_d[0:1, i_scale * D:(i_scale + 1) * D], "bc0", d_rd)
    t_g = load_bc(cx, cx.xt[0], g_row, "bc1", d_rb)
    t_sh = load_bc(cx, cx.shift, mod_d[0:1, i_shift * D:(i_shift + 1) * D], "bc2", d_rd)
    t_gt = load_bc(cx, cx.gate, mod_d[0:1, i_gate * D:(i_gate + 1) * D], "bc3", pre + cx.free["RE"])
    tg = P.op("vector", lambda e: e.scalar_tensor_tensor(out=cx.geff, in0=cx.geff, scalar=1.0, in1=cx.xt[0],
                                                         op0=ALU.add, op1=ALU.mult), deps=[t_sc, t_g])
    sm = cx.small
    xt_free = [tg, None]
    xt_free[1] = None
    hb_free = [None, None]
    rf_dep = cx.free["RF"]
    ra_dep = cx.free["RA"]
    done = []
    xt_last = []
    for tt in range(n_tiles):
        b = tt % 2
        xt = cx.xt[b]
        hb = cx.hb[b]
        ss = sm[:, 40 + b:41 + b]
        rs = sm[:, 42 + b:43 + b]
        tl = P.dma("sync", xt, x_rows[tt * 128:(tt + 1) * 128, :], "xt", 2, deps=d_rb + [xt_free[b]])
        t1 = P.op("scalar", lambda e, xt=xt, hb=hb, ss=ss: e.activation(out=hb, in_=xt, func=AF.Square, accum_out=ss),
                  deps=[tl, hb_free[b]] + rf_dep)
        t2 = P.op("scalar", lambda e, ss=ss, rs=rs: e.activation(out=rs, in_=ss, func=AF.Sqrt, bias=cx.epsb, scale=1.0 / D),
                  deps=[t1])
        t3 = P.op("vector", lambda e, rs=rs: e.reciprocal(out=rs, in_=rs), deps=[t2])
        t4 = P.op("vector", lambda e, xt=xt, rs=rs: e.scalar_tensor_tensor(out=xt, in0=xt, scalar=rs, in1=cx.geff,
                                                                           op0=ALU.mult, op1=ALU.mult), deps=[t3, tg, tl])
        t5 = P.op("vector", lambda e, xt=xt, hb=hb: e.tensor_tensor(out=hb, in0=xt, in1=cx.shift, op=ALU.add),
                  deps=[t4, t_sh, t1])
        xt_free[b] = t5
        xt_last.append(t5)
        cps = []
        for gq in range(4):
            pb = 6 + (gq % 2)
            pst = cx.ps[pb][:, :].bitcast(BF16).rearrange("p (k t) -> p k t", k=8)
            for kk in range(8):
                k = gq * 8 + kk
                mm = P.op("tensor", lambda e, pst=pst, kk=kk, k=k, hb=hb: e.transpose(pst[:, kk, :], hb[:, k * 128:(k + 1) * 128], cx.ident[:, :]),
                          deps=[t5, cx.psfree[pb]] if kk == 0 else [], sig=(kk == 7))
            dst = cx.hT[:, gq * 8:(gq + 1) * 8, tok_off + tt * 128: tok_off + (tt + 1) * 128]
            if halo:
                dst = cx.HH[:, gq * 8:(gq + 1) * 8, :]
                pst = pst[:, :, 96:128]
            if gq % 2 == 0:
                cp = P.op("scalar", lambda e, dst=dst, pst=pst: e.activation(out=dst, in_=pst, func=AF.Copy), deps=[mm] + ra_dep)
            else:
                cp = P.op("vector", lambda e, dst=dst, pst=pst: e.tensor_copy(out=dst, in_=pst), deps=[mm] + ra_dep)
            cx.psfree[pb] = cp
            cps.append(cp)
        hb_free[b] = mm
        done += cps[-2:]
    cx.free["RB"] = xt_last[-2:]
    cx.free["RF"] = [hb_free[0], hb_free[1]]
    cx.free["RD"] = xt_last[-2:]
    if not halo:
        cx.free["RA"] = []
    return done, t_gt


def defer_store(cx, dst, xs, dep, xi, blk_store, key):
    cx.pending = (dst, xs, dep, xi, blk_store, key)


def flush_store(cx):
    if getattr(cx, "pending", None) is None:
        return
    dst, xs, dep, xi, blk_store, key = cx.pending
    cx.pending = None
    st = cx.P.dma("sync", dst, xs, "xs_st", 4, deps=[dep])
    cx.xs_free[xi] = st
    blk_store[key] = st


def phase_ffn(cx, x_rows_in, x_rows_out, blocks, hT_ready, gate_ready, comb=None, comb_ready=()):
    P = cx.P
    FB = 2048
    MB = FB // 128
    wa_free = [None, None]
    wb_free = [None, None]
    blk_store = {}
    wa_it = 0
    wb_it = 0
    g1_ps = 0
    g2_ps = 0
    last_stores = []
    mid_ready_prev = cx.free["RB"]
    for bi, (wg, wu, wd, ei) in enumerate(blocks):
        wg_v = wg.rearrange("(kc p) c -> p kc c", p=128)
        wu_v = wu.rearrange("(kc p) c -> p kc c", p=128)
        wd_v = wd.rearrange("(kc p) c -> p kc c", p=128)
        mid_toks = []
        for m in range(MB):
            slot = wa_it % 2
            wa_it += 1
            wsl = cx.wA[slot]
            dd = [wa_free[slot]] + (cx.free["RC"] if wa_it <= 2 else [])
            t_wg = P.dma("gpsimd", wsl[:, :, 0:128], wg_v[:, :, m * 128:(m + 1) * 128], "wA", 2, deps=dd)
            t_wu = P.dma("gpsimd", wsl[:, :, 128:256], wu_v[:, :, m * 128:(m + 1) * 128], "wAu", 2, deps=dd)
            for n in range(TP // 512):
                pg = cx.ps[(g1_ps % 2) * 2]
                pu = cx.ps[(g1_ps % 2) * 2 + 1]
                pgi = (g1_ps % 2) * 2
                g1_ps += 1
                for k in range(KC):
                    P.op("tensor", lambda e, pg=pg, wsl=wsl, k=k, n=n: e.matmul(pg[:, :], wsl[:, k, 0:128], cx.hT[:, k, n * 512:(n + 1) * 512],
                                                                               start=(k == 0), stop=(k == KC - 1)),
                         deps=([t_wg, cx.psfree[pgi]] + list(hT_ready)) if k == 0 else [], sig=False)
                for k in range(KC):
                    mm = P.op("tensor", lambda e, pu=pu, wsl=wsl, k=k, n=n: e.matmul(pu[:, :], wsl[:, k, 128:256], cx.hT[:, k, n * 512:(n + 1) * 512],
                                                                                    start=(k == 0), stop=(k == KC - 1)),
                              deps=[t_wu, cx.psfree[pgi + 1]] if k == 0 else [], sig=(k == KC - 1))
                ti = cx.tmp_i % 2
                cx.tmp_i += 1
                tmp = cx.TMP[:, ti, :]
                a1 = P.op("scalar", lambda e, tmp=tmp, pg=pg: e.activation(out=tmp, in_=pg[:, :], func=AF.Silu),
                          deps=[mm, cx.tmp_free[ti]])
                dst = cx.midT[:, m, n * 512:(n + 1) * 512]
                v1 = P.op("vector", lambda e, dst=dst, tmp=tmp, pu=pu: e.tensor_tensor(out=dst, in0=tmp, in1=pu[:, :], op=ALU.mult),
                          deps=[a1, mm] + list(mid_ready_prev))
                cx.tmp_free[ti] = v1
                cx.psfree[pgi] = a1
                cx.psfree[pgi + 1] = v1
                mid_toks.append(v1)
            wa_free[slot] = mm
        mid_done = mid_toks[-1:]
        gemm2_last = None
        for cb in range(D // 512):
            slot = wb_it % 2
            wb_it += 1
            wsl = cx.wB16[slot]
            dd = [wb_free[slot]] + (cx.free["RD"] if wb_it <= 2 else [])
            t_wd = P.dma("gpsimd", wsl, wd_v[:, :, cb * 512:(cb + 1) * 512], "wB", 2, deps=dd)
            for tt in range(NT):
                pbi = 4 + (g2_ps % 2)
                g2_ps += 1
                pb = cx.ps[pbi]
                for k in range(MB):
                    mm = P.op("tensor", lambda e, pb=pb, wsl=wsl, k=k, tt=tt: e.matmul(pb[:, :], cx.midT[:, k, tt * 128:(tt + 1) * 128], wsl[:, k, :],
                                                                                      start=(k == 0), stop=(k == MB - 1)),
                              deps=([t_wd, cx.psfree[pbi]] + mid_done) if k == 0 else [], sig=(k == MB - 1))
                xi = cx.xs_i % 2
                cx.xs_i += 1
                xs = cx.XS[:, xi, :]
                key = (tt, cb)
                src = x_rows_in if bi == 0 else x_rows_out
                tl = P.dma("sync", xs, src[tt * 128:(tt + 1) * 128, cb * 512:(cb + 1) * 512], "xs_ld", 2,
                           deps=[cx.xs_free[xi], blk_store.get(key)])
                flush_store(cx)
                ti = cx.tmp_i % 2
                cx.tmp_i += 1
                tmp = cx.TMP[:, ti, :]
                gsl = cx.gate[:, cb * 512:(cb + 1) * 512]
                if ei is None:
                    v1 = P.op("vector", lambda e, tmp=tmp, pb=pb, gsl=gsl: e.tensor_tensor(out=tmp, in0=pb[:, :], in1=gsl, op=ALU.mult),
                              deps=[mm, cx.tmp_free[ti], gate_ready])
                else:
                    csc = comb[:, tt, ei:ei + 1]
                    v1 = P.op("vector", lambda e, tmp=tmp, pb=pb, gsl=gsl, csc=csc: e.scalar_tensor_tensor(
                        out=tmp, in0=pb[:, :], scalar=csc, in1=gsl, op0=ALU.mult, op1=ALU.mult),
                        deps=[mm, cx.tmp_free[ti], gate_ready] + list(comb_ready))
                cx.psfree[pbi] = v1
                v2 = P.op("vector", lambda e, xs=xs, tmp=tmp: e.tensor_tensor(out=xs, in0=xs, in1=tmp, op=ALU.add), deps=[v1, tl])
                cx.tmp_free[ti] = v2
                defer_store(cx, x_rows_out[tt * 128:(tt + 1) * 128, cb * 512:(cb + 1) * 512], xs, v2, xi, blk_store, key)
                gemm2_last = mm
            wb_free[slot] = gemm2_last
        mid_ready_prev = [gemm2_last]
    flush_store(cx)
    cx.free["RB"] = [gemm2_last]
    cx.free["RC"] = [wa_free[0], wa_free[1]]
    cx.free["RD"] = [wb_free[0], wb_free[1]]
    cx.free["RA"] = [wa_free[0], wa_free[1]]
    cx.free["RE"] = [v2]
    return list(blk_store.values())


def phase_conv(cx, ps_idx, x_rows, x_rows_out, w_in, w_out, bo_d, cT_d, hs_d, hT_ready, gate_ready, cpar_ready, hh_ready):
    P = cx.P
    cp = cx.RF
    bval, bgate, bdw, lng, lnb = (cp[:, i * 32:(i + 1) * 32] for i in range(5))
    wdw = cp[:, 160:160 + 32 * CW].rearrange("p (m k) -> p m k", k=CW)
    flag = cx.small[:, 61:62]
    win_v = w_in.rearrange("(kc p) c -> p kc c", p=128)
    wout_v = w_out.rearrange("(kc p) c -> p kc c", p=128)
    GLW = 32 + TP
    GL = [cx.RB[:, i * GLW:(i + 1) * GLW] for i in range(2)]
    o = 2 * GLW
    ACC = [cx.RB[:, o + i * TP:o + (i + 1) * TP] for i in range(2)]
    o += 2 * TP
    CBF = [cx.RB[:, o + i * 512:o + (i + 1) * 512].bitcast(BF16) for i in range(2)]
    o += 1024
    CSQ = [cx.RB[:, o + i * 512:o + (i + 1) * 512].bitcast(BF16) for i in range(2)]
    o += 1024
    assert o <= 8192
    ones = cx.ones_bf
    rb_dep = cx.free["RB"]
    rc_dep = cx.free["RC"]
    wa_free = [None, None]
    gl_free = [None, None]
    acc_free = [None, None]
    cb_free = [None, None]
    cst_tok = []
    pe_last = None
    for m in range(32):
        slot = m % 2
        wsl = cx.wA[slot]
        dd = [wa_free[slot]] + (rc_dep if m < 2 else [])
        t_wv = P.dma("gpsimd", wsl[:, :, 0:128], win_v[:, :, m * 128:(m + 1) * 128], "wA", 2, deps=dd)
        t_wg = P.dma("gpsimd", wsl[:, :, 128:256], win_v[:, :, D + m * 128:D + (m + 1) * 128], "wAu", 2, deps=dd)
        gl = GL[slot]
        acc = ACC[slot]
        glu_toks = []
        segs = [(n * 512, 512, 32 + n * 512, cx.hT[:, :, n * 512:(n + 1) * 512], 0, 1) for n in range(2)]
        halo_mode = getattr(cx, "halo_mode", None) or ("hh" if ps_idx == 0 else "load")
        save_halo = getattr(cx, "save_halo", None)
        if save_halo is None:
            save_halo = (ps_idx == 0)
        if halo_mode == "hh":
            segs.append((0, 32, 0, cx.HH, 6, 7))
        for (t0, tn, g0, src, bv, bg) in segs:
            pv = cx.ps[bv]
            pg = cx.ps[bg]
            for k in range(KC):
                P.op("tensor", lambda e, pv=pv, wsl=wsl, k=k, src=src, tn=tn: e.matmul(pv[:, 0:tn], wsl[:, k, 0:128], src[:, k, :],
                                                                                      start=(k == 0), stop=(k == KC - 1)),
                     deps=([t_wv, cx.psfree[bv]] + list(hT_ready) + list(hh_ready)) if k == 0 else [], sig=False)
            for k in range(KC):
                mm = P.op("tensor", lambda e, pg=pg, wsl=wsl, k=k, src=src, tn=tn: e.matmul(pg[:, 0:tn], wsl[:, k, 128:256], src[:, k, :],
                                                                                           start=(k == 0), stop=(k == KC - 1)),
                          deps=[t_wg, cx.psfree[bg]] if k == 0 else [], sig=(k == KC - 1))
            ti = cx.tmp_i % 2
            cx.tmp_i += 1
            tmp = cx.TMP[:, ti, 0:tn]
            a1 = P.op("scalar", lambda e, tmp=tmp, pg=pg, tn=tn, m=m: e.activation(out=tmp, in_=pg[:, 0:tn], func=AF.Sigmoid,
                                                                                 bias=bgate[:, m:m + 1], scale=1.0),
                      deps=[mm, cx.tmp_free[ti], cpar_ready])
            v1 = P.op("vector", lambda e, gl=gl, g0=g0, tn=tn, pv=pv, tmp=tmp, m=m: e.scalar_tensor_tensor(
                out=gl[:, g0:g0 + tn], in0=pv[:, 0:tn], scalar=bval[:, m:m + 1], in1=tmp, op0=ALU.add, op1=ALU.mult),
                deps=[a1, gl_free[slot], cpar_ready] + rb_dep)
            cx.tmp_free[ti] = v1
            cx.psfree[bv] = v1
            cx.psfree[bg] = a1
            glu_toks.append(v1)
            pe_last = mm
        wa_free[slot] = pe_last
        hl = None
        if halo_mode == "hh":
            hv = P.op("vector", lambda e, gl=gl: e.tensor_scalar(out=gl[:, 0:32], in0=gl[:, 0:32], scalar1=flag, scalar2=None, op0=ALU.mult),
                      deps=[glu_toks[-1]])
            glu_toks.append(hv)
        elif halo_mode == "zero":
            hv = P.op("vector", lambda e, gl=gl: e.memset(gl[:, 0:32], 0.0), deps=[gl_free[slot]] + rb_dep)
            glu_toks.append(hv)
        else:
            hl = P.dma("sync", gl[:, 0:32], hs_d[m], "hs_ld", 2, deps=[gl_free[slot]] + rb_dep)
            glu_toks.append(hl)
        if save_halo:
            P.dma("sync", hs_d[m], gl[:, TP:TP + 32], "hs_st", 2, deps=[glu_toks[1], hl])
        c0 = P.op("vector", lambda e, acc=acc, gl=gl, m=m: e.tensor_scalar(out=acc, in0=gl[:, 2:2 + TP], scalar1=wdw[:, m, 0:1], scalar2=bdw[:, m:m + 1],
                                                                          op0=ALU.mult, op1=ALU.add),
                  deps=glu_toks + [acc_free[slot]])
        for k in range(1, CW):
            ck = P.op("vector", lambda e, acc=acc, gl=gl, m=m, k=k: e.scalar_tensor_tensor(
                out=acc, in0=gl[:, 2 + k:2 + k + TP], scalar=wdw[:, m, k:k + 1], in1=acc, op0=ALU.mult, op1=ALU.add),
                deps=[], sig=(k == CW - 1))
        gl_free[slot] = ck
        cbf = CBF[slot]
        csq = CSQ[slot]
        s1 = P.op("scalar", lambda e, cbf=cbf, acc=acc: e.activation(out=cbf, in_=acc, func=AF.Copy), deps=[ck, cb_free[slot]])
        s2 = P.op("scalar", lambda e, csq=csq, acc=acc: e.activation(out=csq, in_=acc, func=AF.Square), deps=[])
        st = P.dma("sync", cT_d[m], acc, "ct_st", 2, deps=[ck])
        cst_tok.append(st)
        for n in range(2):
            P.op("tensor", lambda e, n=n, cbf=cbf, m=m: e.matmul(cx.ps[2 + n][:, :], ones[:, :], cbf[:, n * 512:(n + 1) * 512],
                                                                start=(m == 0), stop=(m == 31)),
                 deps=[s2] + ([cx.psfree[2 + n], cx.psfree[4 + n]] if m == 0 else []), sig=False)
            mm = P.op("tensor", lambda e, n=n, csq=csq, m=m: e.matmul(cx.ps[4 + n][:, :], ones[:, :], csq[:, n * 512:(n + 1) * 512],
                                                                     start=(m == 0), stop=(m == 31)),
                      deps=[], sig=(n == 1))
        cb_free[slot] = mm
        acc_free[slot] = s2
        acc_free[slot] = st
        pe_last = mm
    o = 0
    MU = cx.RB[:, 0:TP]
    RS = cx.RB[:, TP:2 * TP]
    CT = [cx.RB[:, (2 + i) * TP:(3 + i) * TP] for i in range(3)]
    stat_deps = [pe_last, gl_free[0], gl_free[1], acc_free[0], acc_free[1]]
    for n in range(2):
        sl = slice(n * 512, (n + 1) * 512)
        a = P.op("vector", lambda e, n=n, sl=sl: e.tensor_scalar(out=MU[:, sl], in0=cx.ps[2 + n][:, :], scalar1=1.0 / D, scalar2=None, op0=ALU.mult),
                 deps=stat_deps)
        b = P.op("vector", lambda e, n=n, sl=sl: e.tensor_tensor(out=RS[:, sl], in0=MU[:, sl], in1=MU[:, sl], op=ALU.mult), deps=[a])
        c = P.op("vector", lambda e, n=n, sl=sl: e.scalar_tensor_tensor(out=RS[:, sl], in0=cx.ps[4 + n][:, :], scalar=1.0 / D, in1=RS[:, sl],
                                                                       op0=ALU.mult, op1=ALU.subtract), deps=[b])
        d = P.op("scalar", lambda e, sl=sl: e.activation(out=RS[:, sl], in_=RS[:, sl], func=AF.Sqrt, bias=cx.epsb, scale=1.0), deps=[c])
        f = P.op("vector", lambda e, sl=sl: e.reciprocal(out=RS[:, sl], in_=RS[:, sl]), deps=[d])
        cx.psfree[2 + n] = a
        cx.psfree[4 + n] = c
    stats_done = f
    ra_dep = [wa_free[0], wa_free[1]]
    ct_free = [None] * 3
    zt_toks = []
    for m in range(32):
        bi = m % 3
        ct = CT[bi]
        tl = P.dma("sync", ct, cT_d[m], "ct_ld", 3, deps=[cst_tok[m], ct_free[bi], stats_done])
        v1 = P.op("vector", lambda e, ct=ct: e.tensor_tensor(out=ct, in0=ct, in1=MU, op=ALU.subtract), deps=[tl, stats_done])
        v2 = P.op("vector", lambda e, ct=ct: e.tensor_tensor(out=ct, in0=ct, in1=RS, op=ALU.mult), deps=[v1])
        a1 = P.op("scalar", lambda e, ct=ct, m=m: e.activation(out=cx.hT[:, m, :], in_=ct, func=AF.Silu, bias=lnb[:, m:m + 1], scale=lng[:, m:m + 1]),
                  deps=[v2] + ra_dep)
        ct_free[bi] = a1
        zt_toks.append(a1)
    z_done = zt_toks[-1:]
    wb_free = [None, None]
    rd_dep = cx.free["RD"]
    bo_free = [None, None]
    stores = {}
    g2 = 0
    BO = [cx.small[:, 128:192], cx.small[:, 192:256]]
    BOB = [cx.RB[:, (5 * TP) + i * 256:(5 * TP) + (i + 1) * 256] for i in range(2)]
    for cb in range(D // 256):
        slot = cb % 2
        wsl = cx.wB32[slot]
        dd = [wb_free[slot]] + (rd_dep if cb < 2 else [])
        t_w = P.dma("gpsimd", wsl, wout_v[:, :, cb * 256:(cb + 1) * 256], "wB", 2, deps=dd)
        bob = BOB[slot]
        t_b = P.dma("sync", bob, bo_d[0:1, cb * 256:(cb + 1) * 256].to_broadcast([128, 256]), "bo", 2, deps=[bo_free[slot], stats_done])
        for tt in range(NT):
            pbi = 6 + (g2 % 2)
            g2 += 1
            pb = cx.ps[pbi]
            for k in range(KC):
                mm = P.op("tensor", lambda e, pb=pb, wsl=wsl, k=k, tt=tt: e.matmul(pb[:, 0:256], cx.hT[:, k, tt * 128:(tt + 1) * 128], wsl[:, k, :],
                                                                                  start=(k == 0), stop=(k == KC - 1)),
                          deps=([t_w, cx.psfree[pbi]] + z_done) if k == 0 else [], sig=(k == KC - 1))
            xi = cx.xs_i % 2
            cx.xs_i += 1
            xs = cx.XS[:, xi, 0:256]
            tl = P.dma("sync", xs, x_rows[tt * 128:(tt + 1) * 128, cb * 256:(cb + 1) * 256], "xs_ld", 2, deps=[cx.xs_free[xi]])
            flush_store(cx)
            ti = cx.tmp_i % 2
            cx.tmp_i += 1
            tmp = cx.TMP[:, ti, 0:256]
            v1 = P.op("vector", lambda e, tmp=tmp, pb=pb, bob=bob: e.tensor_tensor(out=tmp, in0=pb[:, 0:256], in1=bob, op=ALU.add),
                      deps=[mm, cx.tmp_free[ti], t_b])
            cx.psfree[pbi] = v1
            v2 = P.op("vector", lambda e, tmp=tmp, cb=cb: e.tensor_tensor(out=tmp, in0=tmp, in1=cx.gate[:, cb * 256:(cb + 1) * 256], op=ALU.mult),
                      deps=[v1, gate_ready])
            v3 = P.op("vector", lambda e, xs=xs, tmp=tmp: e.tensor_tensor(out=xs, in0=xs, in1=tmp, op=ALU.add), deps=[v2, tl])
            cx.tmp_free[ti] = v3
            defer_store(cx, x_rows_out[tt * 128:(tt + 1) * 128, cb * 256:(cb + 1) * 256], xs, v3, xi, stores, (tt, cb))
            last_mm = mm
        wb_free[slot] = last_mm
        bo_free[slot] = v1
    flush_store(cx)
    cx.free["RA"] = [last_mm]
    cx.free["RB"] = [v3, zt_toks[-1]]
    cx.free["RC"] = [wa_free[0], wa_free[1]]
    cx.free["RD"] = [wb_free[0], wb_free[1]]
    cx.free["RE"] = [v3]
    cx.free["RF"] = [zt_toks[-1], ck]
    return list(stores.values())


def init_consts(cx, ident_d, identf_d, flag_d):
    P = cx.P
    t1 = P.dma("sync", cx.ident[:, :], ident_d, "const", 3)
    t2 = P.dma("sync", cx.trif[:, :], identf_d, "const", 3)
    P.op("vector", lambda e: e.memset(cx.onesf[:, :], 1.0))
    P.op("vector", lambda e: e.memset(cx.oneb, 1.0))
    t3 = P.dma("sync", cx.small[:, 61:62], flag_d, "const", 3)
    t4 = P.op("vector", lambda e: e.memset(cx.ones_bf[:, :], 1.0))
    t5 = P.op("vector", lambda e: e.memset(cx.epsb, EPS))
    return [t1, t2, t3, t4, t5]


def build_conv_layer(with_ffn=True):
    nc = bass.Bass("TRN2", target_bir_lowering=False)
    dt = lambda name, shape, kind="ExternalInput": nc.dram_tensor(name, shape, F32, kind=kind).ap()
    x_in = dt("x_in", [TCORE, D])
    x_halo = dt("x_halo", [128, D])
    flag_d = dt("flag", [128, 1])
    ident_d = nc.dram_tensor("ident", [128, 128], BF16, kind="ExternalInput").ap()
    identf_d = dt("identf", [64, 64])
    cT_in = dt("cT", [128, 32])
    condw = dt("cond_w", [D, R])
    condbT = dt("cond_bT", [128, 4])
    adaw = dt("ada_w", [R, 6 * D])
    adab = dt("ada_b", [1, 6 * D])
    g_mix = dt("g_mix", [1, D])
    g_ffn = dt("g_ffn", [1, D])
    w_in = dt("w_in", [D, 2 * D])
    cpar_d = dt("cpar", [128, 160 + 32 * CW])
    w_out = dt("w_out", [D, D])
    b_out = dt("b_out", [1, D])
    wg = dt("ffn_wg", [D, 2 * D])
    wu = dt("ffn_wu", [D, 2 * D])
    wd = dt("ffn_wd", [2 * D, D])
    x_out = dt("x_out", [TCORE, D], kind="ExternalOutput")
    mod_d = nc.dram_tensor("mod_s", [1, 6 * D], F32).ap()
    cT_s = nc.dram_tensor("cT_s", [32, 128, TP], F32).ap()
    hs_s = nc.dram_tensor("hs_s", [32, 128, 32], F32).ap()

    P = Prog(nc)
    cx = Ctx(nc, P)
    c0 = init_consts(cx, ident_d, identf_d, flag_d)
    mod_ready = phase_mods(cx, cT_in, condw, condbT, adaw, adab, mod_d, c0)
    final = []
    for ps_idx in range(NPASS):
        rows = slice(ps_idx * TP, (ps_idx + 1) * TP)
        hh_ready = []
        if ps_idx == 0:
            hh_ready, _ = phase_prep(cx, x_halo, g_mix, mod_d, 1, 0, 2, c0, mod_ready, n_tiles=1, halo=True)
        hT_ready, gate_ready = phase_prep(cx, x_in[rows, :], g_mix, mod_d, 1, 0, 2, c0, mod_ready)
        cpar_ready = P.dma("sync", cx.RF[:, 0:160 + 32 * CW], cpar_d, "cpar", 1, deps=cx.free["RF"])
        st = phase_conv(cx, ps_idx, x_in[rows, :], x_out[rows, :], w_in, w_out, b_out, cT_s, hs_s, hT_ready, gate_ready, cpar_ready, hh_ready)
        if with_ffn:
            hT_ready, gate_ready = phase_prep(cx, x_out[rows, :], g_ffn, mod_d, 4, 3, 5, st, mod_ready)
            blocks = [(wg[:, j * 2048:(j + 1) * 2048], wu[:, j * 2048:(j + 1) * 2048], wd[j * 2048:(j + 1) * 2048, :], None) for j in range(4)]
            st = phase_ffn(cx, x_out[rows, :], x_out[rows, :], blocks, hT_ready, gate_ready)
        final += st
    P.emit(final)
    return nc, P


def _pmajor(v, kc):
    return np.ascontiguousarray(np.asarray(v, np.float32).reshape(kc, 128).T)


def _consts():
    import ml_dtypes
    return {"ident": np.eye(128, dtype=ml_dtypes.bfloat16), "identf": np.triu(np.ones((64, 64), np.float32))}


def _core_rows(x, core):
    b, half = core // 2, core % 2
    return b, half, x[b, half * TCORE:(half + 1) * TCORE]


def _halo_rows(xfull_b, half):
    if half == 0:
        return np.zeros((128, D), np.float32)
    return np.ascontiguousarray(xfull_b[TCORE - 128:TCORE])


def conv_layer_inputs(I, x, i, core):
    j = i // 2
    b, half, xr = _core_rows(x, core)
    cpar = np.concatenate([
        _pmajor(I["conv_b_in"][j][:D], 32), _pmajor(I["conv_b_in"][j][D:], 32), _pmajor(I["conv_b_dw"][j], 32),
        _pmajor(I["conv_ln_g"][j], 32), _pmajor(I["conv_ln_b"][j], 32),
        np.ascontiguousarray(np.asarray(I["conv_w_dw"][j], np.float32).reshape(CW, 32, 128).transpose(2, 1, 0)).reshape(128, 32 * CW),
    ], axis=1)
    m = {
        "x_in": np.ascontiguousarray(xr), "x_halo": _halo_rows(x[b], half),
        "flag": np.full((128, 1), float(half), np.float32),
        "cT": _pmajor(I["c"][b], 32), "cond_w": I["cond_w"], "cond_bT": _pmajor(I["cond_b"], 4),
        "ada_w": I["ada_w"][i], "ada_b": I["ada_b"][i][None, :],
        "g_mix": I["mix_norm_g"][i][None, :], "g_ffn": I["ffn_norm_g"][i][None, :],
        "w_in": I["conv_w_in"][j], "cpar": np.ascontiguousarray(cpar), "w_out": I["conv_w_out"][j], "b_out": I["conv_b_out"][j][None, :],
        "ffn_wg": I["ffn_w_gate"][j], "ffn_wu": I["ffn_w_up"][j], "ffn_wd": I["ffn_w_down"][j],
    }
    m.update(_consts())
    return m


def phase_router(cx, wr_d, br_d, hT_ready):
    P = cx.P
    WR = cx.WRt[:, 0:KC * 8].rearrange("p (k e) -> p k e", e=8)
    t_w = P.dma("gpsimd", WR, wr_d.rearrange("(kc p) e -> p kc e", p=128), "wr", 1, deps=cx.comb_free)
    brb = cx.small[:, 64:72]
    t_b = P.dma("sync", brb, br_d.to_broadcast([128, 8]), "wrb", 1, deps=cx.comb_free)
    sm = cx.small
    last = None
    for tt in range(NT):
        pb = cx.ps[6 + tt % 2]
        for k in range(KC):
            mm = P.op("tensor", lambda e, pb=pb, k=k, tt=tt: e.matmul(pb[:, 0:8], cx.hT[:, k, tt * 128:(tt + 1) * 128], WR[:, k, :],
                                                                     start=(k == 0), stop=(k == KC - 1)),
                      deps=([t_w, cx.psfree[6 + tt % 2]] + list(hT_ready)) if k == 0 else [], sig=(k == KC - 1))
        lg = sm[:, 72:80]
        mx = sm[:, 80:88]
        w12 = sm[:, 88:90]
        dd = sm[:, 90:92]
        c1 = sm[:, 96:104]
        v0 = P.op("vector", lambda e, pb=pb: e.tensor_tensor(out=lg, in0=pb[:, 0:8], in1=brb, op=ALU.add), deps=[mm, t_b, last])
        cx.psfree[6 + tt % 2] = v0
        v1 = P.op("vector", lambda e: e.max(out=mx, in_=lg), deps=[v0])
        v2 = P.op("vector", lambda e: e.tensor_tensor(out=dd[:, 0:1], in0=mx[:, 0:1], in1=mx[:, 1:2], op=ALU.subtract), deps=[v1])
        v3 = P.op("vector", lambda e: e.tensor_tensor(out=dd[:, 1:2], in0=mx[:, 1:2], in1=mx[:, 0:1], op=ALU.subtract), deps=[v2])
        a1 = P.op("scalar", lambda e: e.activation(out=w12, in_=dd, func=AF.Sigmoid), deps=[v3])
        v4 = P.op("vector", lambda e: e.tensor_scalar(out=c1, in0=lg, scalar1=mx[:, 0:1], scalar2=w12[:, 0:1], op0=ALU.is_equal, op1=ALU.mult), deps=[a1])
        v5 = P.op("vector", lambda e, tt=tt: e.tensor_scalar(out=cx.comb[:, tt, :], in0=lg, scalar1=mx[:, 1:2], scalar2=w12[:, 1:2], op0=ALU.is_equal, op1=ALU.mult), deps=[v4])
        v6 = P.op("vector", lambda e, tt=tt: e.tensor_tensor(out=cx.comb[:, tt, :], in0=cx.comb[:, tt, :], in1=c1, op=ALU.add), deps=[v5])
        last = v6
    return [last]


def phase_mlstm(cx, ps_idx, x_rows, x_rows_out, w_in, w_out, bg_d, ng_d, mod_d, qk_d, v_d, o_d, g_d, st_c, st_n,
                hT_ready, mod_ready, state_ready):
    P = cx.P
    sm = cx.small
    win_v = w_in.rearrange("(kc p) c -> p kc c", p=128)
    wout_v = w_out.rearrange("(kc p) c -> p kc c", p=128)
    STG = [cx.TMP[:, i, :].bitcast(BF16) for i in range(2)]
    wa_free = [None, None]
    rc_dep = cx.free["RC"]
    stg_free = [cx.tmp_free[0], cx.tmp_free[1]]
    si = 0
    sc_writes = []
    pi = 0
    pe_last = None
    for m in range(32):
        slot = m % 2
        wsl = cx.wA[slot]
        dd = [wa_free[slot]] + (rc_dep if m < 2 else [])
        t_w = P.dma("gpsimd", wsl[:, :, 0:128], win_v[:, :, m * 128:(m + 1) * 128], "wA", 2, deps=dd)
        for n in range(2):
            pbi = pi % 4
            pi += 1
            pb = cx.ps[pbi]
            for k in range(KC):
                mm = P.op("tensor", lambda e, pb=pb, wsl=wsl, k=k, n=n: e.matmul(pb[:, :], wsl[:, k, 0:128], cx.hT[:, k, n * 512:(n + 1) * 512],
                                                                                start=(k == 0), stop=(k == KC - 1)),
                          deps=([t_w, cx.psfree[pbi]] + list(hT_ready)) if k == 0 else [], sig=(k == KC - 1))
            s = si % 2
            si += 1
            stg = STG[s][:, 0:512]
            sc = 1.0 if m < 16 else DK ** -0.5
            a1 = P.op("scalar", lambda e, stg=stg, pb=pb, sc=sc: e.activation(out=stg, in_=pb[:, :], func=AF.Copy, scale=sc), deps=[mm, stg_free[s]])
            cx.psfree[pbi] = a1
            st = P.dma("sync", qk_d[m, :, n * 512:(n + 1) * 512], stg, "sc_st", 4, deps=[a1])
            stg_free[s] = st
            sc_writes.append(st)
            pe_last = mm
        wa_free[slot] = pe_last
    for cbk in range(32):
        slot = cbk % 2
        wsl = cx.wA[slot]
        t_w = P.dma("gpsimd", wsl, win_v[:, :, 4096 + cbk * 256:4096 + (cbk + 1) * 256], "wA", 2, deps=[wa_free[slot]])
        dst_d = v_d if cbk < 16 else o_d
        c0 = (cbk % 16) * 256
        for tt in range(NT):
            pbi = pi % 4
            pi += 1
            pb = cx.ps[pbi]
            for k in range(KC):
                mm = P.op("tensor", lambda e, pb=pb, wsl=wsl, k=k, tt=tt: e.matmul(pb[:, 0:256], cx.hT[:, k, tt * 128:(tt + 1) * 128], wsl[:, k, :],
                                                                                  start=(k == 0), stop=(k == KC - 1)),
                          deps=[t_w, cx.psfree[pbi]] if k == 0 else [], sig=(k == KC - 1))
            s = si % 2
            si += 1
            stg = STG[s][:, 0:256]
            if tt % 2 == 0:
                a1 = P.op("scalar", lambda e, stg=stg, pb=pb: e.activation(out=stg, in_=pb[:, 0:256], func=AF.Copy), deps=[mm, stg_free[s]])
            else:
                a1 = P.op("vector", lambda e, stg=stg, pb=pb: e.tensor_copy(out=stg, in_=pb[:, 0:256]), deps=[mm, stg_free[s]])
            cx.psfree[pbi] = a1
            st = P.dma("sync", dst_d[tt * 128:(tt + 1) * 128, c0:c0 + 256], stg, "sc_st", 4, deps=[a1])
            stg_free[s] = st
            sc_writes.append(st)
            pe_last = mm
        wa_free[slot] = pe_last
    WG = cx.WRt[:, 0:KC * 16].rearrange("p (k e) -> p k e", e=16)
    t_wg = P.dma("gpsimd", WG, win_v[:, :, 12288:12304], "wr", 1, deps=cx.comb_free)
    bgb = sm[:, 104:120]
    t_bg = P.dma("sync", bgb, bg_d.to_broadcast([128, 16]), "wrb", 1)
    gst = sm[:, 120:152].rearrange("p (b e) -> p b e", e=16)
    gfree = [None, None]
    for tt in range(NT):
        pbi = pi % 4
        pi += 1
        pb = cx.ps[pbi]
        for k in range(KC):
            mm = P.op("tensor", lambda e, pb=pb, k=k, tt=tt: e.matmul(pb[:, 0:16], cx.hT[:, k, tt * 128:(tt + 1) * 128], WG[:, k, :],
                                                                     start=(k == 0), stop=(k == KC - 1)),
                      deps=[t_wg, cx.psfree[pbi]] if k == 0 else [], sig=(k == KC - 1))
        v0 = P.op("vector", lambda e, pb=pb, tt=tt: e.tensor_tensor(out=gst[:, tt % 2, :], in0=pb[:, 0:16], in1=bgb, op=ALU.add),
                  deps=[mm, t_bg, gfree[tt % 2]])
        cx.psfree[pbi] = v0
        st = P.dma("sync", g_d[tt * 128:(tt + 1) * 128, :], gst[:, tt % 2, :], "sc_st", 4, deps=[v0])
        gfree[tt % 2] = st
        sc_writes.append(st)
        pe_last = mm
    cx.comb_free = [pe_last]
    cx.tmp_free = [stg_free[0], stg_free[1]]
    proj_done = [pe_last]
    sc_done = sc_writes[-4:]
    Cf = cx.RB[:, :].rearrange("p (g v) -> p g v", v=DV)
    Cb = cx.RD[:, 0:4096].bitcast(BF16).rearrange("p (g v) -> p g v", v=DV)
    QK = [cx.RD[:, 4096 + i * 1024:4096 + (i + 1) * 1024].bitcast(BF16).rearrange("p (m t) -> p m t", t=LCH) for i in range(2)]
    VC = [cx.RC[:, i * 2048:(i + 1) * 2048].bitcast(BF16) for i in range(2)]
    OC = [cx.RC[:, 4096 + i * 2048:4096 + (i + 1) * 2048].bitcast(BF16) for i in range(2)]
    NGB = cx.RE[:, :]
    NGS = cx.RF[:, :]
    yT = cx.hT
    nf = sm[:, 152:168]
    nb = cx.nbt[:, :]
    tri = cx.trif[0:64, 0:64]
    onesf = cx.onesf
    free0 = proj_done + cx.free["RB"] + cx.free["RD"] + cx.free["RE"] + cx.free["RF"]
    if getattr(cx, "zero_state", False) and ps_idx == 0:
        t_c = P.op("vector", lambda e: e.memset(cx.RB[:, :], 0.0), deps=free0 + list(state_ready))
        t_n = P.op("vector", lambda e: e.memset(nf, 0.0), deps=free0)
    else:
        t_c = P.dma("sync", Cf, st_c.rearrange("p (g v) -> p g v", v=DV), "st_ld", 2, deps=free0 + list(state_ready))
        t_n = P.dma("sync", nf, st_n, "st_ld", 2, deps=free0 + list(state_ready))
    t_ng = P.dma("sync", NGB, ng_d.to_broadcast([128, D]), "bc3", 1, deps=free0)
    cbt = []
    for g in range(16):
        cbt.append(P.op("scalar", lambda e, g=g: e.activation(out=Cb[:, g, :], in_=Cf[:, g, :], func=AF.Copy), deps=[t_c]))
    nbt = P.op("vector", lambda e: e.tensor_copy(out=nb, in_=nf), deps=[t_n])
    cb_ready = [cbt[-1]] * 16
    nb_ready = nbt
    buf_free = [None, None]
    ngs_free = None
    y_tr = None
    G = sm[0:64, 168:184]
    gw = cx.gwork
    for c in range(TP // LCH):
        bsel = c % 2
        rows = slice(c * LCH, (c + 1) * LCH)
        qk = QK[bsel]
        vc = VC[bsel]
        oc = OC[bsel]
        ld = []
        for qq in range(4):
            ld.append(P.dma("sync", qk[:, qq * 8:(qq + 1) * 8, :], qk_d[qq * 8:(qq + 1) * 8, :, rows].rearrange("m p t -> p m t"), "ch_ld", 8,
                            deps=sc_done + [buf_free[bsel]] + free0))
        ld.append(P.dma("sync", vc[0:64, :], v_d[rows, :], "ch_ld", 8, deps=sc_done + [buf_free[bsel]]))
        ld.append(P.dma("sync", oc[0:64, :], o_d[rows, :], "ch_ld", 8, deps=sc_done + [buf_free[bsel]]))
        tg = P.dma("sync", G, g_d[rows, :], "g_ld", 1, deps=sc_done + [ngs_free])
        ig = G[:, 0:8]
        fp = G[:, 8:16]
        ax, ee, ll, lf, aa, ea, eb, eg, dec = (gw[:, i * 8:(i + 1) * 8] for i in range(9))
        d00 = P.op("vector", lambda e: e.tensor_scalar(out=ax, in0=fp, scalar1=-1.0, scalar2=None, op0=ALU.mult), deps=[tg])
        d0 = P.op("vector", lambda e: e.tensor_tensor(out=ax, in0=ax, in1=fp, op=ALU.max), deps=[d00])
        a0 = P.op("scalar", lambda e: e.activation(out=ee, in_=ax, func=AF.Exp, scale=-1.0), deps=[d0])
        a1 = P.op("scalar", lambda e: e.activation(out=ll, in_=ee, func=AF.Ln, bias=cx.oneb[0:64, :], scale=1.0), deps=[a0])
        d1 = P.op("vector", lambda e: e.tensor_scalar(out=lf, in0=fp, scalar1=0.0, scalar2=None, op0=ALU.min), deps=[a1])
        d2 = P.op("vector", lambda e: e.tensor_tensor(out=lf, in0=lf, in1=ll, op=ALU.subtract), deps=[d1])
        pg = cx.ps[7]
        m0 = P.op("tensor", lambda e: e.matmul(pg[0:64, 0:8], tri, lf, start=True, stop=True), deps=[d2, cx.psfree[7]], sig=False)
        m1 = P.op("tensor", lambda e: e.matmul(pg[:, 8:16], onesf[0:64, :], lf, start=True, stop=True), deps=[])
        d3 = P.op("vector", lambda e: e.tensor_tensor(out=aa, in0=ig, in1=pg[0:64, 0:8], op=ALU.subtract), deps=[m1])
        a2 = P.op("scalar", lambda e: e.activation(out=ea, in_=aa, func=AF.Exp), deps=[d3])
        a3 = P.op("scalar", lambda e: e.activation(out=eb, in_=pg[0:64, 0:8], func=AF.Exp), deps=[m1])
        d4 = P.op("vector", lambda e: e.tensor_tensor(out=aa, in0=aa, in1=pg[0:64, 8:16], op=ALU.add), deps=[a2])
        a4 = P.op("scalar", lambda e: e.activation(out=eg, in_=aa, func=AF.Exp), deps=[d4])
        decb = cx.decb[:, :]
        a5 = P.op("scalar", lambda e: e.activation(out=decb, in_=pg[:, 8:16], func=AF.Exp), deps=[m1])
        cx.psfree[7] = a5
        pg_readers = [a5, d4]
        a6 = P.op("scalar", lambda e, oc=oc: e.activation(out=NGS[0:64, :], in_=oc[0:64, :], func=AF.Sigmoid), deps=[ld[5], ngs_free])
        d5 = P.op("vector", lambda e: e.tensor_tensor(out=NGS[0:64, :], in0=NGS[0:64, :], in1=NGB[0:64, :], op=ALU.mult), deps=[a6, t_ng])
        yb = cx.ybt[0:64, :]
        hd_last = None
        for h in range(H):
            pS = cx.ps[6]
            for i in range(2):
                mS = P.op("tensor", lambda e, h=h, i=i, qk=qk: e.matmul(pS[0:64, 0:64], qk[:, 16 + 2 * h + i, :], qk[:, 2 * h + i, :],
                                                                       start=(i == 0), stop=(i == 1)),
                          deps=([cx.psfree[6]] + ld[0:4]) if i == 0 else [], sig=(i == 1))
            Sm = cx.smt[0:64, (h % 2) * 64:(h % 2 + 1) * 64]
            d6 = P.op("vector", lambda e, Sm=Sm, h=h: e.scalar_tensor_tensor(out=Sm, in0=pS[0:64, 0:64], scalar=ea[:, h:h + 1], in1=tri,
                                                                            op0=ALU.mult, op1=ALU.mult), deps=[mS, a2, cx.sm_free[h % 2]])
            cx.psfree[6] = d6
            pT = cx.ps[7]
            for i in range(2):
                mT = P.op("tensor", lambda e, pT=pT, qk=qk, h=h, i=i: e.transpose(
                    pT[0:64, :].bitcast(BF16)[:, i * 128:(i + 1) * 128], qk[:, 16 + 2 * h + i, :], cx.ident[:, :]),
                    deps=([cx.psfree[7]] + pg_readers) if i == 0 else [], sig=(i == 1))
            wk = cx.wkt[0:64, (h % 2) * 256:(h % 2 + 1) * 256]
            f1 = P.op("vector", lambda e, wk=wk, pT=pT, h=h: e.tensor_scalar(out=wk, in0=pT[0:64, :].bitcast(BF16)[:, 0:256], scalar1=eg[:, h:h + 1], scalar2=None,
                                                                            op0=ALU.mult), deps=[mT, a4, cx.wk_free[h % 2]])
            cx.psfree[7] = f1
            pbi = h % 2
            pN = cx.ps[pbi]
            pD = cx.ps[2 + pbi]
            P.op("tensor", lambda e, pN=pN, Sm=Sm, vc=vc, h=h: e.matmul(pN[0:64, :], Sm, vc[0:64, h * DV:(h + 1) * DV], start=True, stop=False),
                 deps=[d6, cx.psfree[pbi], ld[4], cb_ready[2 * h], cb_ready[2 * h + 1], nb_ready], sig=False)
            for i in range(2):
                P.op("tensor", lambda e, pN=pN, qk=qk, h=h, i=i: e.matmul(pN[0:64, :], qk[:, 2 * h + i, :], Cb[:, 2 * h + i, :], start=False, stop=(i == 1)),
                     deps=[], sig=False)
            P.op("tensor", lambda e, pD=pD, Sm=Sm: e.matmul(pD[0:64, 0:1], Sm, cx.ones_bf[0:64, 0:1], start=True, stop=False),
                 deps=[cx.psfree[2 + pbi]], sig=False)
            for i in range(2):
                mN = P.op("tensor", lambda e, pD=pD, qk=qk, h=h, i=i: e.matmul(pD[0:64, 0:1], qk[:, 2 * h + i, :], nb[:, 2 * h + i:2 * h + i + 1],
                                                                              start=False, stop=(i == 1)), deps=[], sig=(i == 1))
            cx.sm_free[h % 2] = mN
            hw = cx.hwork[0:64, (h % 2) * 8:(h % 2 + 1) * 8]
            t1, t2, ssq, rr = hw[:, 0:1], hw[:, 1:2], hw[:, 2:3], hw[:, 3:4]
            e1a = P.op("vector", lambda e, t1=t1, pD=pD: e.tensor_scalar(out=t1, in0=pD[0:64, 0:1], scalar1=-1.0, scalar2=None, op0=ALU.mult),
                       deps=[mN, a3, cx.hw_free[h % 2]])
            e1b = P.op("vector", lambda e, t1=t1, pD=pD: e.tensor_tensor(out=t1, in0=t1, in1=pD[0:64, 0:1], op=ALU.max), deps=[e1a])
            e1 = P.op("vector", lambda e, t1=t1, h=h: e.tensor_tensor(out=t1, in0=t1, in1=eb[:, h:h + 1], op=ALU.mult), deps=[e1b])
            cx.psfree[2 + pbi] = e1
            e2 = P.op("vector", lambda e, t1=t1: e.tensor_scalar(out=t1, in0=t1, scalar1=1.0, scalar2=None, op0=ALU.max), deps=[e1])
            e3 = P.op("vector", lambda e, t1=t1: e.reciprocal(out=t1, in_=t1), deps=[e2])
            e4 = P.op("vector", lambda e, t1=t1, h=h: e.tensor_tensor(out=t1, in0=t1, in1=eb[:, h:h + 1], op=ALU.mult), deps=[e3])
            junk = cx.junk[0:64, :]
            s1 = P.op("scalar", lambda e, pN=pN, ssq=ssq, junk=junk: e.activation(out=junk, in_=pN[0:64, :], func=AF.Square, accum_out=ssq),
                      deps=[mN, cx.hw_free[h % 2]])
            e5 = P.op("vector", lambda e, t1=t1, t2=t2: e.tensor_tensor(out=t2, in0=t1, in1=t1, op=ALU.mult), deps=[e4])
            e6 = P.op("vector", lambda e, t2=t2, ssq=ssq: e.tensor_tensor(out=t2, in0=t2, in1=ssq, op=ALU.mult), deps=[e5, s1])
            s2 = P.op("scalar", lambda e, t2=t2: e.activation(out=t2, in_=t2, func=AF.Sqrt, bias=cx.epsb[0:64, :], scale=1.0 / DV), deps=[e6])
            e7 = P.op("vector", lambda e, t2=t2: e.reciprocal(out=t2, in_=t2), deps=[s2])
            e8 = P.op("vector", lambda e, rr=rr, t1=t1, t2=t2: e.tensor_tensor(out=rr, in0=t1, in1=t2, op=ALU.mult), deps=[e7])
            e9 = P.op("vector", lambda e, yb=yb, pN=pN, rr=rr, h=h: e.scalar_tensor_tensor(
                out=yb[:, h * DV:(h + 1) * DV], in0=pN[0:64, :], scalar=rr, in1=NGS[0:64, h * DV:(h + 1) * DV], op0=ALU.mult, op1=ALU.mult),
                deps=[e8, d5, y_tr])
            cx.psfree[pbi] = e9
            cx.hw_free[h % 2] = e9
            pC = [cx.ps[4], cx.ps[5]]
            for i in range(2):
                mC = P.op("tensor", lambda e, pC=pC, wk=wk, vc=vc, h=h, i=i: e.matmul(pC[i][:, :], wk[:, i * 128:(i + 1) * 128], vc[0:64, h * DV:(h + 1) * DV],
                                                                                     start=True, stop=True),
                          deps=[f1, cx.psfree[4 + i]], sig=(i == 1))
            pn = pD
            for i in range(2):
                mn_ = P.op("tensor", lambda e, pn=pn, wk=wk, i=i: e.matmul(pn[:, 8 + i:9 + i], wk[:, i * 128:(i + 1) * 128], cx.ones_bf[0:64, 0:1],
                                                                          start=True, stop=True), deps=[cx.pn_free[pbi]] if i == 0 else [], sig=(i == 1))
            cx.wk_free[h % 2] = mn_
            for i in range(2):
                g_ = 2 * h + i
                u1 = P.op("vector", lambda e, g_=g_, h=h, i=i, pC=pC: e.scalar_tensor_tensor(out=Cf[:, g_, :], in0=Cf[:, g_, :], scalar=decb[:, h:h + 1], in1=pC[i][:, :],
                                                                                           op0=ALU.mult, op1=ALU.add), deps=[mC, a5, mN])
                cx.psfree[4 + i] = u1
                u2 = P.op("scalar", lambda e, g_=g_: e.activation(out=Cb[:, g_, :], in_=Cf[:, g_, :], func=AF.Copy), deps=[u1, mN])
                cb_ready[g_] = u2
            u3 = P.op("vector", lambda e, h=h, pn=pn: e.scalar_tensor_tensor(out=nf[:, 2 * h:2 * h + 2], in0=nf[:, 2 * h:2 * h + 2], scalar=decb[:, h:h + 1],
                                                                            in1=pn[:, 8:10], op0=ALU.mult, op1=ALU.add), deps=[mn_, mN])
            cx.pn_free[pbi] = u3
            u4 = P.op("vector", lambda e, h=h: e.tensor_copy(out=nb[:, 2 * h:2 * h + 2], in_=nf[:, 2 * h:2 * h + 2]), deps=[u3])
            nb_ready = u4
            hd_last = u4
        buf_free[bsel] = hd_last
        ngs_free = hd_last
        for gq in range(4):
            pbi = 6 + (gq % 2)
            pst = cx.ps[pbi][:, :].bitcast(BF16).rearrange("p (k t) -> p k t", k=8)[:, :, 0:64]
            for kk in range(8):
                k = gq * 8 + kk
                mm = P.op("tensor", lambda e, pst=pst, kk=kk, k=k, yb=yb: e.transpose(pst[:, kk, :], yb[:, k * 128:(k + 1) * 128], cx.ident[0:64, 0:64]),
                          deps=[hd_last, cx.psfree[pbi]] if kk == 0 else [], sig=(kk == 7))
            dst = yT[:, gq * 8:(gq + 1) * 8, c * LCH:(c + 1) * LCH]
            cp = P.op("scalar", lambda e, dst=dst, pst=pst: e.activation(out=dst, in_=pst, func=AF.Copy), deps=[mm] + proj_done)
            cx.psfree[pbi] = cp
        y_tr = mm
    y_done = [cp]
    s1 = P.dma("sync", st_c.rearrange("p (g v) -> p g v", v=DV), Cf, "st_st", 2, deps=[hd_last])
    s2 = P.dma("sync", st_n, nf, "st_st", 2, deps=[hd_last])
    t_gt = load_bc(cx, cx.gate, mod_d[0:1, 2 * D:3 * D], "bc3", [hd_last] + list(mod_ready))
    wb_free = [None, None]
    stores = {}
    g2 = 0
    for cb in range(D // 256):
        slot = cb % 2
        wsl = cx.wA[slot]
        t_w = P.dma("gpsimd", wsl, wout_v[:, :, cb * 256:(cb + 1) * 256], "wA", 2, deps=[wb_free[slot], hd_last])
        for tt in range(NT):
            pbi = (g2 % 2)
            g2 += 1
            pb = cx.ps[pbi]
            for k in range(KC):
                mm = P.op("tensor", lambda e, pb=pb, wsl=wsl, k=k, tt=tt: e.matmul(pb[:, 0:256], yT[:, k, tt * 128:(tt + 1) * 128], wsl[:, k, :],
                                                                                  start=(k == 0), stop=(k == KC - 1)),
                          deps=([t_w, cx.psfree[pbi]] + y_done) if k == 0 else [], sig=(k == KC - 1))
            xi = cx.xs_i % 2
            cx.xs_i += 1
            xs = cx.XS[:, xi, 0:256]
            tl = P.dma("sync", xs, x_rows[tt * 128:(tt + 1) * 128, cb * 256:(cb + 1) * 256], "xs_ld", 2, deps=[cx.xs_free[xi]])
            flush_store(cx)
            ti = cx.tmp_i % 2
            cx.tmp_i += 1
            tmp = cx.TMP[:, ti, 0:256]
            v2 = P.op("vector", lambda e, tmp=tmp, pb=pb, cb=cb: e.tensor_tensor(out=tmp, in0=pb[:, 0:256], in1=cx.gate[:, cb * 256:(cb + 1) * 256], op=ALU.mult),
                      deps=[mm, cx.tmp_free[ti], t_gt] + sc_done)
            cx.psfree[pbi] = v2
            v3 = P.op("vector", lambda e, xs=xs, tmp=tmp: e.tensor_tensor(out=xs, in0=xs, in1=tmp, op=ALU.add), deps=[v2, tl])
            cx.tmp_free[ti] = v3
            defer_store(cx, x_rows_out[tt * 128:(tt + 1) * 128, cb * 256:(cb + 1) * 256], xs, v3, xi, stores, (tt, cb))
            last_mm = mm
        wb_free[slot] = last_mm
    flush_store(cx)
    cx.free["RA"] = [last_mm]
    cx.free["RB"] = [s1, hd_last]
    cx.free["RC"] = [wb_free[0], wb_free[1]]
    cx.free["RD"] = [hd_last]
    cx.free["RE"] = [v3]
    cx.free["RF"] = [hd_last]
    return list(stores.values()), [s1, s2]


def build_mlstm_layer(with_ffn=True, final_norm=False):
    nc = bass.Bass("TRN2", target_bir_lowering=False)
    dt = lambda name, shape, kind="ExternalInput": nc.dram_tensor(name, shape, F32, kind=kind).ap()
    x_in = dt("x_in", [TCORE, D])
    flag_d = dt("flag", [128, 1])
    ident_d = nc.dram_tensor("ident", [128, 128], BF16, kind="ExternalInput").ap()
    identf_d = dt("identf", [64, 64])
    cT_in = dt("cT", [128, 32])
    condw = dt("cond_w", [D, R])
    condbT = dt("cond_bT", [128, 4])
    adaw = dt("ada_w", [R, 6 * D])
    adab = dt("ada_b", [1, 6 * D])
    g_mix = dt("g_mix", [1, D])
    g_ffn = dt("g_ffn", [1, D])
    w_in = dt("w_in", [D, NPROJ])
    bg_d = dt("b_gates", [1, 16])
    ng_d = dt("norm_g", [1, D])
    w_out = dt("w_out", [D, D])
    st_c_in = dt("st_c_in", [128, 16 * DV])
    st_n_in = dt("st_n_in", [128, 16])
    wr = dt("w_router", [D, 8])
    br = dt("b_router", [1, 8])
    wg = dt("moe_wg", [8, D, 2048])
    wu = dt("moe_wu", [8, D, 2048])
    wd = dt("moe_wd", [8, 2048, D])
    gfin = dt("g_final", [1, D])
    x_out = dt("x_out", [TCORE, D], kind="ExternalOutput")
    st_c = dt("st_c", [128, 16 * DV], kind="ExternalOutput")
    st_n = dt("st_n", [128, 16], kind="ExternalOutput")
    mod_d = nc.dram_tensor("mod_s", [1, 6 * D], F32).ap()
    qk_s = nc.dram_tensor("qk_s", [32, 128, TP], BF16).ap()
    v_s = nc.dram_tensor("v_s", [TP, D], BF16).ap()
    o_s = nc.dram_tensor("o_s", [TP, D], BF16).ap()
    g_s = nc.dram_tensor("g_s", [TP, 16], F32).ap()

    P = Prog(nc)
    cx = Ctx(nc, P)
    c0 = init_consts(cx, ident_d, identf_d, flag_d)
    mod_ready = phase_mods(cx, cT_in, condw, condbT, adaw, adab, mod_d, c0)
    i1 = P.dma("sync", st_c, st_c_in, "st_cp", 2)
    i2 = P.dma("sync", st_n, st_n_in, "st_cp", 2)
    state_ready = [i1, i2]
    final = []
    for ps_idx in range(NPASS):
        rows = slice(ps_idx * TP, (ps_idx + 1) * TP)
        hT_ready, _ = phase_prep(cx, x_in[rows, :], g_mix, mod_d, 1, 0, 2, c0, mod_ready)
        st, state_ready = phase_mlstm(cx, ps_idx, x_in[rows, :], x_out[rows, :], w_in, w_out, bg_d, ng_d, mod_d, qk_s, v_s, o_s, g_s, st_c, st_n,
                                      hT_ready, mod_ready, state_ready)
        if with_ffn:
            hT_ready, gate_ready = phase_prep(cx, x_out[rows, :], g_ffn, mod_d, 4, 3, 5, st, mod_ready)
            comb_ready = phase_router(cx, wr, br, hT_ready)
            blocks = [(wg[e], wu[e], wd[e], e) for e in range(8)]
            st = phase_ffn(cx, x_out[rows, :], x_out[rows, :], blocks, hT_ready, gate_ready, comb=cx.comb, comb_ready=comb_ready)
            cx.comb_free = list(cx.free["RA"])
        if final_norm:
            st = phase_final_norm(cx, x_out[rows, :], gfin, st)
        final += st
    final += state_ready
    P.emit(final)
    return nc, P


def phase_final_norm(cx, x_rows, g_row, x_ready):
    P = cx.P
    sm = cx.small
    t_g = load_bc(cx, cx.geff, g_row, "bc0", list(x_ready) + cx.free["RD"])
    xt_free = [None, None]
    stores = []
    d_rb = list(x_ready) + cx.free["RB"]
    rf_dep = cx.free["RF"]
    for tt in range(NT):
        b = tt % 2
        xt = cx.xt[b]
        hb = cx.hb[b]
        ss = sm[:, 40 + b:41 + b]
        rs = sm[:, 42 + b:43 + b]
        tl = P.dma("sync", xt, x_rows[tt * 128:(tt + 1) * 128, :], "xt", 2, deps=d_rb + [xt_free[b]])
        t1 = P.op("scalar", lambda e, xt=xt, hb=hb, ss=ss: e.activation(out=hb, in_=xt, func=AF.Square, accum_out=ss), deps=[tl] + rf_dep)
        t2 = P.op("scalar", lambda e, ss=ss, rs=rs: e.activation(out=rs, in_=ss, func=AF.Sqrt, bias=cx.epsb, scale=1.0 / D), deps=[t1])
        t3 = P.op("vector", lambda e, rs=rs: e.reciprocal(out=rs, in_=rs), deps=[t2])
        t4 = P.op("vector", lambda e, xt=xt, rs=rs: e.scalar_tensor_tensor(out=xt, in0=xt, scalar=rs, in1=cx.geff, op0=ALU.mult, op1=ALU.mult), deps=[t3, t_g, tl])
        st = P.dma("sync", x_rows[tt * 128:(tt + 1) * 128, :], xt, "fn_st", 2, deps=[t4])
        xt_free[b] = st
        stores.append(st)
    cx.free["RB"] = stores[-2:]
    cx.free["RD"] = [t4]
    cx.free["RF"] = [t1]
    return stores[-2:]


def mlstm_layer_inputs(I, x, i, core, st_c, st_n, b_override=None):
    j = i // 2
    b, half, xr = _core_rows(x, core)
    m = {
        "x_in": np.ascontiguousarray(xr), "flag": np.full((128, 1), float(half), np.float32),
        "cT": _pmajor(I["c"][b], 32), "cond_w": I["cond_w"], "cond_bT": _pmajor(I["cond_b"], 4),
        "ada_w": I["ada_w"][i], "ada_b": I["ada_b"][i][None, :],
        "g_mix": I["mix_norm_g"][i][None, :], "g_ffn": I["ffn_norm_g"][i][None, :],
        "w_in": I["mlstm_w_in"][j], "b_gates": I["mlstm_b_gates"][j][None, :], "norm_g": I["mlstm_norm_g"][j][None, :],
        "w_out": I["mlstm_w_out"][j], "st_c_in": st_c, "st_n_in": st_n,
        "w_router": I["moe_w_router"][j], "b_router": I["moe_b_router"][j][None, :],
        "moe_wg": I["moe_w_gate"][j], "moe_wu": I["moe_w_up"][j], "moe_wd": I["moe_w_down"][j],
        "g_final": I["final_norm_g"][None, :],
    }
    m.update(_consts())
    return m


_PROG_CACHE = {}


def _get_prog(kind):
    if kind not in _PROG_CACHE:
        if kind == "conv":
            _PROG_CACHE[kind] = build_conv_layer(True)[0]
        elif kind == "mlstm":
            _PROG_CACHE[kind] = build_mlstm_layer(True, False)[0]
        elif kind == "mlstm_final":
            _PROG_CACHE[kind] = build_mlstm_layer(True, True)[0]
        elif kind == "mlstm_state":
            _PROG_CACHE[kind] = build_mlstm_state()[0]
    return _PROG_CACHE[kind]


def _run(nc, in_maps):
    res = run_bass_kernel_spmd(nc, in_maps, core_ids=list(range(len(in_maps))))
    return res.results


def kernel(**inputs):
    I = {k: np.asarray(v) for k, v in inputs.items()}
    x = np.ascontiguousarray(I["x"], dtype=np.float32)
    B = x.shape[0]
    for i in range(4):
        if i % 2 == 0:
            nc = _get_prog("conv")
            maps = [conv_layer_inputs(I, x, i, core) for core in range(NCORES)]
            res = _run(nc, maps)
            xn = np.empty_like(x)
            for core in range(NCORES):
                b, half = core // 2, core % 2
                xn[b, half * TCORE:(half + 1) * TCORE] = res[core]["x_out"]
            x = xn
        else:
            nc = _get_prog("mlstm_final" if i == 3 else "mlstm")
            zc = np.zeros((128, 16 * DV), np.float32)
            zn = np.zeros((128, 16), np.float32)
            maps = [mlstm_layer_inputs(I, x, i, core, zc, zn) for core in range(NCORES)]
            res = _run(nc, maps)
            xn = np.empty_like(x)
            for b in range(B):
                xn[b, 0:TCORE] = res[2 * b]["x_out"]
            maps = [mlstm_layer_inputs(I, x, i, 2 * b + 1, res[2 * b]["st_c"], res[2 * b]["st_n"]) for b in range(B)]
            res2 = _run(nc, maps)
            for b in range(B):
                xn[b, TCORE:2 * TCORE] = res2[b]["x_out"]
            x = xn
    return x


def phase_mlstm_state(cx, ps_idx, w_in, bg_d, qk_d, v_d, g_d, st_c, st_n, hT_ready, state_ready):
    P = cx.P
    sm = cx.small
    win_v = w_in.rearrange("(kc p) c -> p kc c", p=128)
    STG = [cx.TMP[:, i, :].bitcast(BF16) for i in range(2)]
    wa_free = [None, None]
    rc_dep = cx.free["RC"]
    stg_free = [cx.tmp_free[0], cx.tmp_free[1]]
    si = 0
    sc_writes = []
    pi = 0
    pe_last = None
    wi = 0
    for m in range(16, 32):
        slot = wi % 2
        wi += 1
        wsl = cx.wA[slot]
        dd = [wa_free[slot]] + (rc_dep if wi <= 2 else [])
        t_w = P.dma("gpsimd", wsl[:, :, 0:128], win_v[:, :, m * 128:(m + 1) * 128], "wA", 2, deps=dd)
        for n in range(2):
            pbi = pi % 4
            pi += 1
            pb = cx.ps[pbi]
            for k in range(KC):
                mm = P.op("tensor", lambda e, pb=pb, wsl=wsl, k=k, n=n: e.matmul(pb[:, :], wsl[:, k, 0:128], cx.hT[:, k, n * 512:(n + 1) * 512],
                                                                                start=(k == 0), stop=(k == KC - 1)),
                          deps=([t_w, cx.psfree[pbi]] + list(hT_ready)) if k == 0 else [], sig=(k == KC - 1))
            s_ = si % 2
            si += 1
            stg = STG[s_][:, 0:512]
            a1 = P.op("scalar", lambda e, stg=stg, pb=pb: e.activation(out=stg, in_=pb[:, :], func=AF.Copy, scale=DK ** -0.5), deps=[mm, stg_free[s_]])
            cx.psfree[pbi] = a1
            st = P.dma("sync", qk_d[m, :, n * 512:(n + 1) * 512], stg, "sc_st", 4, deps=[a1])
            stg_free[s_] = st
            sc_writes.append(st)
            pe_last = mm
        wa_free[slot] = pe_last
    for cbk in range(16):
        slot = wi % 2
        wi += 1
        wsl = cx.wA[slot]
        t_w = P.dma("gpsimd", wsl, win_v[:, :, 4096 + cbk * 256:4096 + (cbk + 1) * 256], "wA", 2, deps=[wa_free[slot]])
        c0 = cbk * 256
        for tt in range(NT):
            pbi = pi % 4
            pi += 1
            pb = cx.ps[pbi]
            for k in range(KC):
                mm = P.op("tensor", lambda e, pb=pb, wsl=wsl, k=k, tt=tt: e.matmul(pb[:, 0:256], cx.hT[:, k, tt * 128:(tt + 1) * 128], wsl[:, k, :],
                                                                                  start=(k == 0), stop=(k == KC - 1)),
                          deps=[t_w, cx.psfree[pbi]] if k == 0 else [], sig=(k == KC - 1))
            s_ = si % 2
            si += 1
            stg = STG[s_][:, 0:256]
            a1 = P.op("scalar", lambda e, stg=stg, pb=pb: e.activation(out=stg, in_=pb[:, 0:256], func=AF.Copy), deps=[mm, stg_free[s_]])
            cx.psfree[pbi] = a1
            st = P.dma("sync", v_d[tt * 128:(tt + 1) * 128, c0:c0 + 256], stg, "sc_st", 4, deps=[a1])
            stg_free[s_] = st
            sc_writes.append(st)
            pe_last = mm
        wa_free[slot] = pe_last
    WG = cx.WRt[:, 0:KC * 16].rearrange("p (k e) -> p k e", e=16)
    t_wg = P.dma("gpsimd", WG, win_v[:, :, 12288:12304], "wr", 1, deps=cx.comb_free)
    bgb = sm[:, 104:120]
    t_bg = P.dma("sync", bgb, bg_d.to_broadcast([128, 16]), "wrb", 1)
    gst = sm[:, 120:152].rearrange("p (b e) -> p b e", e=16)
    gfree = [None, None]
    for tt in range(NT):
        pbi = pi % 4
        pi += 1
        pb = cx.ps[pbi]
        for k in range(KC):
            mm = P.op("tensor", lambda e, pb=pb, k=k, tt=tt: e.matmul(pb[:, 0:16], cx.hT[:, k, tt * 128:(tt + 1) * 128], WG[:, k, :],
                                                                     start=(k == 0), stop=(k == KC - 1)),
                      deps=[t_wg, cx.psfree[pbi]] if k == 0 else [], sig=(k == KC - 1))
        v0 = P.op("vector", lambda e, pb=pb, tt=tt: e.tensor_tensor(out=gst[:, tt % 2, :], in0=pb[:, 0:16], in1=bgb, op=ALU.add),
                  deps=[mm, t_bg, gfree[tt % 2]])
        cx.psfree[pbi] = v0
        st = P.dma("sync", g_d[tt * 128:(tt + 1) * 128, :], gst[:, tt % 2, :], "sc_st", 4, deps=[v0])
        gfree[tt % 2] = st
        sc_writes.append(st)
        pe_last = mm
    cx.comb_free = [pe_last]
    cx.tmp_free = [stg_free[0], stg_free[1]]
    proj_done = [pe_last]
    sc_done = sc_writes[-4:]
    Cf = cx.RB[:, :].rearrange("p (g v) -> p g v", v=DV)
    QK = [cx.RD[:, 4096 + i * 1024:4096 + (i + 1) * 1024].bitcast(BF16).rearrange("p (m t) -> p m t", t=LCH) for i in range(2)]
    VC = [cx.RC[:, i * 2048:(i + 1) * 2048].bitcast(BF16) for i in range(2)]
    nf = sm[:, 152:168]
    tri = cx.trif[0:64, 0:64]
    onesf = cx.onesf
    free0 = proj_done + cx.free["RB"] + cx.free["RD"]
    if ps_idx == 0:
        t_c = P.op("vector", lambda e: e.memset(cx.RB[:, :], 0.0), deps=free0 + list(state_ready))
        t_n = P.op("vector", lambda e: e.memset(nf, 0.0), deps=free0)
    else:
        t_c = P.dma("sync", Cf, st_c.rearrange("p (g v) -> p g v", v=DV), "st_ld", 2, deps=free0 + list(state_ready))
        t_n = P.dma("sync", nf, st_n, "st_ld", 2, deps=free0 + list(state_ready))
    buf_free = [None, None]
    g_free = None
    G = sm[0:64, 168:184]
    gw = cx.gwork
    hd_last = None
    for c in range(TP // LCH):
        bsel = c % 2
        rows = slice(c * LCH, (c + 1) * LCH)
        qk = QK[bsel]
        vc = VC[bsel]
        ld = []
        for qq in range(2, 4):
            ld.append(P.dma("sync", qk[:, qq * 8:(qq + 1) * 8, :], qk_d[qq * 8:(qq + 1) * 8, :, rows].rearrange("m p t -> p m t"), "ch_ld", 8,
                            deps=sc_done + [buf_free[bsel]] + free0))
        ld.append(P.dma("sync", vc[0:64, :], v_d[rows, :], "ch_ld", 8, deps=sc_done + [buf_free[bsel]]))
        tg = P.dma("sync", G, g_d[rows, :], "g_ld", 1, deps=sc_done + [g_free])
        ig = G[:, 0:8]
        fp = G[:, 8:16]
        ax, ee, ll, lf, aa, ea, eb, eg, dec = (gw[:, i * 8:(i + 1) * 8] for i in range(9))
        d00 = P.op("vector", lambda e: e.tensor_scalar(out=ax, in0=fp, scalar1=-1.0, scalar2=None, op0=ALU.mult), deps=[tg])
        d0 = P.op("vector", lambda e: e.tensor_tensor(out=ax, in0=ax, in1=fp, op=ALU.max), deps=[d00])
        a0 = P.op("scalar", lambda e: e.activation(out=ee, in_=ax, func=AF.Exp, scale=-1.0), deps=[d0])
        a1 = P.op("scalar", lambda e: e.activation(out=ll, in_=ee, func=AF.Ln, bias=cx.oneb[0:64, :], scale=1.0), deps=[a0])
        d1 = P.op("vector", lambda e: e.tensor_scalar(out=lf, in0=fp, scalar1=0.0, scalar2=None, op0=ALU.min), deps=[a1])
        d2 = P.op("vector", lambda e: e.tensor_tensor(out=lf, in0=lf, in1=ll, op=ALU.subtract), deps=[d1])
        pg = cx.ps[7]
        m0 = P.op("tensor", lambda e: e.matmul(pg[0:64, 0:8], tri, lf, start=True, stop=True), deps=[d2, cx.psfree[7]], sig=False)
        m1 = P.op("tensor", lambda e: e.matmul(pg[:, 8:16], onesf[0:64, :], lf, start=True, stop=True), deps=[])
        d3 = P.op("vector", lambda e: e.tensor_tensor(out=aa, in0=ig, in1=pg[0:64, 0:8], op=ALU.subtract), deps=[m1])
        d4 = P.op("vector", lambda e: e.tensor_tensor(out=aa, in0=aa, in1=pg[0:64, 8:16], op=ALU.add), deps=[d3])
        a4 = P.op("scalar", lambda e: e.activation(out=eg, in_=aa, func=AF.Exp), deps=[d4])
        decb = cx.decb[:, :]
        a5 = P.op("scalar", lambda e: e.activation(out=decb, in_=pg[:, 8:16], func=AF.Exp), deps=[m1])
        cx.psfree[7] = a5
        pg_readers = [a5, d4]
        for h in range(H):
            pbi = h % 2
            pT = cx.ps[7]
            for i in range(2):
                mT = P.op("tensor", lambda e, pT=pT, qk=qk, h=h, i=i: e.transpose(
                    pT[0:64, :].bitcast(BF16)[:, i * 128:(i + 1) * 128], qk[:, 16 + 2 * h + i, :], cx.ident[:, :]),
                    deps=([cx.psfree[7]] + pg_readers + ld[0:2]) if i == 0 else [], sig=(i == 1))
            wk = cx.wkt[0:64, (h % 2) * 256:(h % 2 + 1) * 256]
            f1 = P.op("vector", lambda e, wk=wk, pT=pT, h=h: e.tensor_scalar(out=wk, in0=pT[0:64, :].bitcast(BF16)[:, 0:256], scalar1=eg[:, h:h + 1], scalar2=None,
                                                                            op0=ALU.mult), deps=[mT, a4, cx.wk_free[h % 2]])
            cx.psfree[7] = f1
            pC = [cx.ps[4], cx.ps[5]]
            for i in range(2):
                mC = P.op("tensor", lambda e, pC=pC, wk=wk, vc=vc, h=h, i=i: e.matmul(pC[i][:, :], wk[:, i * 128:(i + 1) * 128], vc[0:64, h * DV:(h + 1) * DV],
                                                                                     start=True, stop=True),
                          deps=[f1, cx.psfree[4 + i], ld[2]], sig=(i == 1))
            pn = cx.ps[2 + pbi]
            for i in range(2):
                mn_ = P.op("tensor", lambda e, pn=pn, wk=wk, i=i: e.matmul(pn[:, 8 + i:9 + i], wk[:, i * 128:(i + 1) * 128], cx.ones_bf[0:64, 0:1],
                                                                          start=True, stop=True), deps=[cx.pn_free[pbi], cx.psfree[2 + pbi]] if i == 0 else [], sig=(i == 1))
            cx.wk_free[h % 2] = mn_
            for i in range(2):
                g_ = 2 * h + i
                u1 = P.op("vector", lambda e, g_=g_, h=h, i=i, pC=pC: e.scalar_tensor_tensor(out=Cf[:, g_, :], in0=Cf[:, g_, :], scalar=decb[:, h:h + 1], in1=pC[i][:, :],
                                                                                           op0=ALU.mult, op1=ALU.add), deps=[mC, a5, t_c])
                cx.psfree[4 + i] = u1
            u3 = P.op("vector", lambda e, h=h, pn=pn: e.scalar_tensor_tensor(out=nf[:, 2 * h:2 * h + 2], in0=nf[:, 2 * h:2 * h + 2], scalar=decb[:, h:h + 1],
                                                                            in1=pn[:, 8:10], op0=ALU.mult, op1=ALU.add), deps=[mn_, t_n])
            cx.pn_free[pbi] = u3
            cx.psfree[2 + pbi] = u3
            hd_last = u3
        buf_free[bsel] = hd_last
        g_free = hd_last
    s1 = P.dma("sync", st_c.rearrange("p (g v) -> p g v", v=DV), Cf, "st_st", 2, deps=[hd_last])
    s2 = P.dma("sync", st_n, nf, "st_st", 2, deps=[hd_last])
    cx.free["RA"] = proj_done
    cx.free["RB"] = [s1, hd_last]
    cx.free["RC"] = [hd_last, wa_free[0], wa_free[1]]
    cx.free["RD"] = [hd_last]
    return [s1, s2]


def build_mlstm_state():
    nc = bass.Bass("TRN2", target_bir_lowering=False)
    dt = lambda name, shape, kind="ExternalInput": nc.dram_tensor(name, shape, F32, kind=kind).ap()
    x_in = dt("x_in", [TCORE, D])
    flag_d = dt("flag", [128, 1])
    ident_d = nc.dram_tensor("ident", [128, 128], BF16, kind="ExternalInput").ap()
    identf_d = dt("identf", [64, 64])
    cT_in = dt("cT", [128, 32])
    condw = dt("cond_w", [D, R])
    condbT = dt("cond_bT", [128, 4])
    adaw = dt("ada_w", [R, 6 * D])
    adab = dt("ada_b", [1, 6 * D])
    g_mix = dt("g_mix", [1, D])
    w_in = dt("w_in", [D, NPROJ])
    bg_d = dt("b_gates", [1, 16])
    st_c = dt("st_c", [128, 16 * DV], kind="ExternalOutput")
    st_n = dt("st_n", [128, 16], kind="ExternalOutput")
    mod_d = nc.dram_tensor("mod_s", [1, 6 * D], F32).ap()
    qk_s = nc.dram_tensor("qk_s", [32, 128, TP], BF16).ap()
    v_s = nc.dram_tensor("v_s", [TP, D], BF16).ap()
    g_s = nc.dram_tensor("g_s", [TP, 16], F32).ap()
    P = Prog(nc)
    cx = Ctx(nc, P)
    c0 = init_consts(cx, ident_d, identf_d, flag_d)
    mod_ready = phase_mods(cx, cT_in, condw, condbT, adaw, adab, mod_d, c0, nblk=8)
    state_ready = []
    for ps_idx in range(NPASS):
        rows = slice(ps_idx * TP, (ps_idx + 1) * TP)
        hT_ready, _ = phase_prep(cx, x_in[rows, :], g_mix, mod_d, 1, 0, 2, c0, mod_ready)
        state_ready = phase_mlstm_state(cx, ps_idx, w_in, bg_d, qk_s, v_s, g_s, st_c, st_n, hT_ready, state_ready)
    P.emit(state_ready)
    return nc, P


def mlstm_state_inputs(I, x, i, core):
    j = i // 2
    b, half, xr = _core_rows(x, core)
    m = {
        "x_in": np.ascontiguousarray(xr), "flag": np.full((128, 1), float(half), np.float32),
        "cT": _pmajor(I["c"][b], 32), "cond_w": I["cond_w"], "cond_bT": _pmajor(I["cond_b"], 4),
        "ada_w": I["ada_w"][i], "ada_b": I["ada_b"][i][None, :], "g_mix": I["mix_norm_g"][i][None, :],
        "w_in": I["mlstm_w_in"][j], "b_gates": I["mlstm_b_gates"][j][None, :],
    }
    m.update(_consts())
    return m


F_NPASS = 4
F_T = F_NPASS * TP


def build_fused(n_layers=4, layers=None, npass=None):
    layers = list(range(n_layers)) if layers is None else list(layers)
    npass = F_NPASS if npass is None else npass
    nc = bass.Bass("TRN2", target_bir_lowering=False)
    dt = lambda name, shape, kind="ExternalInput": nc.dram_tensor(name, shape, F32, kind=kind).ap()
    x_in = dt("x_in", [F_T, D])
    flag_d = dt("flag", [128, 1])
    ident_d = nc.dram_tensor("ident", [128, 128], BF16, kind="ExternalInput").ap()
    identf_d = dt("identf", [64, 64])
    cT_in = dt("cT", [128, 32])
    condw = dt("cond_w", [D, R])
    condbT = dt("cond_bT", [128, 4])
    adaw = dt("ada_w", [4, R, 6 * D])
    adab = dt("ada_b", [4, 6 * D])
    g_mix = dt("g_mix", [4, D])
    g_ffn = dt("g_ffn", [4, D])
    gfin = dt("g_final", [1, D])
    c_w_in = dt("conv_w_in", [2, D, 2 * D])
    c_par = dt("conv_par", [2, 128, 160 + 32 * CW])
    c_w_out = dt("conv_w_out", [2, D, D])
    c_b_out = dt("conv_b_out", [2, D])
    f_wg = dt("ffn_wg", [2, D, 2 * D])
    f_wu = dt("ffn_wu", [2, D, 2 * D])
    f_wd = dt("ffn_wd", [2, 2 * D, D])
    m_w_in = dt("ml_w_in", [2, D, NPROJ])
    m_bg = dt("ml_bg", [2, 16])
    m_ng = dt("ml_ng", [2, D])
    m_w_out = dt("ml_w_out", [2, D, D])
    e_wr = dt("moe_wr", [2, D, 8])
    e_br = dt("moe_br", [2, 8])
    e_wg = dt("moe_wg", [2, 8, D, 2048])
    e_wu = dt("moe_wu", [2, 8, D, 2048])
    e_wd = dt("moe_wd", [2, 8, 2048, D])
    x_out = dt("x_out", [F_T, D], kind="ExternalOutput")
    mod_s = nc.dram_tensor("mod_s", [4, 6 * D], F32).ap()
    cT_s = nc.dram_tensor("cT_s", [32, 128, TP], F32).ap()
    hs_s = nc.dram_tensor("hs_s", [32, 128, 32], F32).ap()
    qk_s = nc.dram_tensor("qk_s", [32, 128, TP], BF16).ap()
    v_s = nc.dram_tensor("v_s", [TP, D], BF16).ap()
    o_s = nc.dram_tensor("o_s", [TP, D], BF16).ap()
    g_s = nc.dram_tensor("g_s", [TP, 16], F32).ap()
    st_c = nc.dram_tensor("st_c", [128, 16 * DV], F32).ap()
    st_n = nc.dram_tensor("st_n", [128, 16], F32).ap()

    P = Prog(nc)
    cx = Ctx(nc, P)
    cx.zero_state = True
    c0 = init_consts(cx, ident_d, identf_d, flag_d)
    mod_ready = []
    for i in range(4):
        mod_ready = phase_mods(cx, cT_in, condw, condbT, adaw[i], adab[i:i + 1, :], mod_s[i:i + 1, :], c0 + mod_ready)
    x_ready = list(c0)
    final = []
    for li, i in enumerate(layers):
        j = i // 2
        mod_d = mod_s[i:i + 1, :]
        src = x_in if li == 0 else x_out
        layer_tokens = []
        state_ready = []
        for ps_idx in range(npass):
            rows = slice(ps_idx * TP, (ps_idx + 1) * TP)
            hT_ready, gate_ready = phase_prep(cx, src[rows, :], g_mix[i:i + 1, :], mod_d, 1, 0, 2, x_ready, mod_ready)
            if i % 2 == 0:
                cx.halo_mode = "zero" if ps_idx == 0 else "load"
                cx.save_halo = ps_idx < npass - 1
                cpar_ready = P.dma("sync", cx.RF[:, 0:160 + 32 * CW], c_par[j], "cpar", 1, deps=cx.free["RF"])
                st = phase_conv(cx, ps_idx, src[rows, :], x_out[rows, :], c_w_in[j], c_w_out[j], c_b_out[j:j + 1, :], cT_s, hs_s,
                                hT_ready, gate_ready, cpar_ready, [])
                hT_ready, gate_ready = phase_prep(cx, x_out[rows, :], g_ffn[i:i + 1, :], mod_d, 4, 3, 5, st, mod_ready)
                blocks = [(f_wg[j][:, q * 2048:(q + 1) * 2048], f_wu[j][:, q * 2048:(q + 1) * 2048], f_wd[j][q * 2048:(q + 1) * 2048, :], None)
                          for q in range(4)]
                st = phase_ffn(cx, x_out[rows, :], x_out[rows, :], blocks, hT_ready, gate_ready)
            else:
                st, state_ready = phase_mlstm(cx, ps_idx, src[rows, :], x_out[rows, :], m_w_in[j], m_w_out[j], m_bg[j:j + 1, :], m_ng[j:j + 1, :],
                                              mod_d, qk_s, v_s, o_s, g_s, st_c, st_n, hT_ready, mod_ready, state_ready)
                hT_ready, gate_ready = phase_prep(cx, x_out[rows, :], g_ffn[i:i + 1, :], mod_d, 4, 3, 5, st, mod_ready)
                comb_ready = phase_router(cx, e_wr[j], e_br[j:j + 1, :], hT_ready)
                blocks = [(e_wg[j, e], e_wu[j, e], e_wd[j, e], e) for e in range(8)]
                st = phase_ffn(cx, x_out[rows, :], x_out[rows, :], blocks, hT_ready, gate_ready, comb=cx.comb, comb_ready=comb_ready)
                cx.comb_free = list(cx.free["RA"])
                if i == 3:
                    st = phase_final_norm(cx, x_out[rows, :], gfin, st)
            layer_tokens += st
            if i == 3:
                final += st
        x_ready = layer_tokens + state_ready
    P.emit(final + x_ready)
    return nc, P


def fused_inputs(I, b):
    def cpar(j):
        return np.concatenate([
            _pmajor(I["conv_b_in"][j][:D], 32), _pmajor(I["conv_b_in"][j][D:], 32), _pmajor(I["conv_b_dw"][j], 32),
            _pmajor(I["conv_ln_g"][j], 32), _pmajor(I["conv_ln_b"][j], 32),
            np.ascontiguousarray(np.asarray(I["conv_w_dw"][j], np.float32).reshape(CW, 32, 128).transpose(2, 1, 0)).reshape(128, 32 * CW),
        ], axis=1)
    m = {
        "x_in": np.ascontiguousarray(I["x"][b]), "flag": np.zeros((128, 1), np.float32),
        "cT": _pmajor(I["c"][b], 32), "cond_w": I["cond_w"], "cond_bT": _pmajor(I["cond_b"], 4),
        "ada_w": I["ada_w"], "ada_b": I["ada_b"], "g_mix": I["mix_norm_g"], "g_ffn": I["ffn_norm_g"], "g_final": I["final_norm_g"][None, :],
        "conv_w_in": I["conv_w_in"], "conv_par": np.ascontiguousarray(np.stack([cpar(0), cpar(1)])), "conv_w_out": I["conv_w_out"],
        "conv_b_out": I["conv_b_out"], "ffn_wg": I["ffn_w_gate"], "ffn_wu": I["ffn_w_up"], "ffn_wd": I["ffn_w_down"],
        "ml_w_in": I["mlstm_w_in"], "ml_bg": I["mlstm_b_gates"], "ml_ng": I["mlstm_norm_g"], "ml_w_out": I["mlstm_w_out"],
        "moe_wr": I["moe_w_router"], "moe_br": I["moe_b_router"], "moe_wg": I["moe_w_gate"], "moe_wu": I["moe_w_up"], "moe_wd": I["moe_w_down"],
    }
    m.update(_consts())
    return m


def kernel(**inputs):
    I = {k: np.asarray(v) for k, v in inputs.items()}
    x = np.ascontiguousarray(I["x"], dtype=np.float32)
    B = x.shape[0]
    for i in range(4):
        xn = np.empty_like(x)
        if i % 2 == 0:
            nc = _get_prog("conv")
            res = _run(nc, [conv_layer_inputs(I, x, i, core) for core in range(NCORES)])
        else:
            ncs = _get_prog("mlstm_state")
            rs = _run(ncs, [mlstm_state_inputs(I, x, i, 2 * b) for b in range(B)])
            nc = _get_prog("mlstm_final" if i == 3 else "mlstm")
            zc = np.zeros((128, 16 * DV), np.float32)
            zn = np.zeros((128, 16), np.float32)
            maps = []
            for core in range(NCORES):
                b, half = core // 2, core % 2
                maps.append(mlstm_layer_inputs(I, x, i, core, rs[b]["st_c"] if half else zc, rs[b]["st_n"] if half else zn))
            res = _run(nc, maps)
        for core in range(NCORES):
            b, half = core // 2, core % 2
            xn[b, half * TCORE:(half + 1) * TCORE] = res[core]["x_out"]
        x = xn
    return x
```

```python
import numpy as np
import concourse.bass as bass
import concourse.mybir as mybir
from concourse.bass_utils import run_bass_kernel_spmd

F32 = mybir.dt.float32
BF16 = mybir.dt.bfloat16
AF = mybir.ActivationFunctionType
ALU = mybir.AluOpType

D = 4096
KC = 32
TP = 1024
NPASS = 2
TCORE = TP * NPASS
NT = TP // 128
R = 512
CW = 31
HALO = 32
H = 8
DK = 256
DV = 512
LCH = 64
NPROJ = 12304
EPS = 1e-6
NCORES = 8

ENGS = ("tensor", "vector", "scalar", "gpsimd", "sync")
SEM_LIMIT = 1 << 40


class Prog:
    def __init__(self, nc):
        self.nc = nc
        self.q = {e: [] for e in ENGS}
        self.esem = {e: nc.alloc_semaphore(name=f"es_{e}") for e in ENGS if e != "sync"}
        self.ecnt = {e: 0 for e in ENGS}
        self.waited = {e: {} for e in ENGS}
        self.rings = {}
        self.n_inst = 0

    def _wait(self, eng, deps):
        for d in deps:
            if d is None:
                continue
            sem, val = d
            key = id(sem)
            if self.waited[eng].get(key, 0) >= val:
                continue
            self.waited[eng][key] = val
            self.q[eng].append(lambda e, sem=sem, val=val: e.wait_ge(sem, val))
            self.n_inst += 1

    def op(self, eng, fn, deps=(), sig=True):
        self._wait(eng, deps)
        self.n_inst += 1
        if sig:
            if self.ecnt[eng] >= SEM_LIMIT:
                self.esem[eng] = self.nc.alloc_semaphore(name=f"es_{eng}_{self.n_inst}")
                self.ecnt[eng] = 0
            self.ecnt[eng] += 1
            n = self.ecnt[eng]
            sem = self.esem[eng]
            self.q[eng].append(lambda e, fn=fn, sem=sem: fn(e).then_inc(sem, 1))
            return (sem, n)
        self.q[eng].append(lambda e, fn=fn: fn(e))
        return None

    def dma(self, queue, out, in_, ring, nring=1, deps=()):
        r = self.rings.setdefault(ring, {"i": 0, "sems": [], "cnt": []})
        j = r["i"] % nring
        r["i"] += 1
        if len(r["sems"]) <= j:
            r["sems"].append(self.nc.alloc_semaphore(name=f"ds_{ring}_{j}"))
            r["cnt"].append(0)
        sem = r["sems"][j]
        deps = list(deps)
        if r["cnt"][j] > 0:
            deps.append((sem, r["cnt"][j]))
        self._wait(queue, deps)
        if r["cnt"][j] >= SEM_LIMIT:
            sem = self.nc.alloc_semaphore(name=f"ds_{ring}_{j}_{self.n_inst}")
            r["sems"][j] = sem
            r["cnt"][j] = 0
        r["cnt"][j] += 16
        self.n_inst += 1
        self.q[queue].append(lambda e, out=out, in_=in_, sem=sem: e.dma_start(out=out, in_=in_).then_inc(sem, 16))
        return (sem, r["cnt"][j])

    def emit(self, final_waits):
        nc = self.nc
        self._wait("sync", final_waits)
        with nc.Block() as block:
            @block.tensor
            def _(e):
                for f in self.q["tensor"]:
                    f(e)

            @block.vector
            def _(e):
                for f in self.q["vector"]:
                    f(e)

            @block.scalar
            def _(e):
                for f in self.q["scalar"]:
                    f(e)

            @block.gpsimd
            def _(e):
                for f in self.q["gpsimd"]:
                    f(e)

            @block.sync
            def _(e):
                for f in self.q["sync"]:
                    f(e)


class Ctx:
    def __init__(self, nc, P):
        self.nc = nc
        self.P = P
        a = nc.alloc_sbuf_tensor
        self.RA = a("RA", [128, 16384], F32)
        self.RB = a("RB", [128, 8192], F32)
        self.RC = a("RC", [128, 8192], F32)
        self.RD = a("RD", [128, 8192], F32)
        self.RE = a("RE", [128, 4096], F32)
        self.RF = a("RF", [128, 4096], F32)
        self.XS = a("XS", [128, 2, 512], F32)
        self.TMP = a("TMP", [128, 2, 512], F32)
        self.ident = a("ident_sb", [128, 128], BF16)
        self.identf = None
        self.small = a("small", [128, 256], F32)
        self.HHt = a("HH", [128, KC * 32], BF16)
        self.HH = self.HHt[:, :].rearrange("p (k t) -> p k t", k=KC)
        self.ones_bf = a("ones_bf", [128, 128], BF16)
        self.epsb = self.small[:, 60:61]
        self.oneb = self.small[:, 62:63]
        self.WRt = a("WRt", [128, KC * 16], BF16)
        self.comb = a("comb", [128, NT, 8], F32)
        self.comb_free = []
        self.nbt = a("nbt", [128, 16], BF16)
        self.trif = a("trif", [64, 64], F32)
        self.onesf = a("onesf", [64, 128], F32)
        self.gwork = a("gwork", [64, 128], F32)
        self.decb = a("decb", [128, 8], F32)
        self.smt = a("smt", [64, 128], BF16)
        self.hwork = a("hwork", [64, 16], F32)
        self.junk = self.HHt[:, 0:512]
        self.wkt = self.HHt[:, 512:1024]
        self.ybt = self.RD[:, 6144:8192].bitcast(BF16)
        self.sm_free = [None, None]
        self.hw_free = [None, None]
        self.wk_free = [None, None]
        self.pn_free = [None, None]
        self.ps = [nc.alloc_psum_tensor(f"ps{i}", [128, 512], F32) for i in range(8)]
        self.hT = self.RA[:, :].bitcast(BF16).rearrange("p (k t) -> p k t", k=KC)
        self.midT = self.RB[:, :].bitcast(BF16).rearrange("p (k t) -> p k t", k=16)
        self.xt = [self.RB[:, 0:4096], self.RB[:, 4096:8192]]
        self.wA = [self.RC[:, i * 4096:(i + 1) * 4096].bitcast(BF16).rearrange("p (k c) -> p k c", k=KC) for i in range(2)]
        self.wB16 = [self.RD[:, i * 4096:(i + 1) * 4096].bitcast(BF16).rearrange("p (k c) -> p k c", k=16) for i in range(2)]
        self.wB32 = [self.RD[:, i * 4096:(i + 1) * 4096].bitcast(BF16).rearrange("p (k c) -> p k c", k=KC) for i in range(2)]
        self.geff = self.RD[:, 0:4096]
        self.shift = self.RD[:, 4096:8192]
        self.gate = self.RE[:, :]
        self.hb = [self.RF[:, i * 2048:(i + 1) * 2048].bitcast(BF16) for i in range(2)]
        self.free = {k: [] for k in ("RA", "RB", "RC", "RD", "RE", "RF")}
        self.psfree = [None] * 8
        self.xs_free = [None, None]
        self.tmp_free = [None, None]
        self.xs_i = 0
        self.tmp_i = 0


def load_consts(cx, ident_d, identf_d):
    P = cx.P
    t1 = P.dma("sync", cx.ident[:, :], ident_d, "const")
    t2 = P.dma("sync", cx.identf[:, :], identf_d, "const2")
    return [t1, t2]


def phase_mods(cx, cT_d, condw_d, condbT_d, adaw_d, adab_d, mod_d, start_deps, nblk=None):
    P, nc = cx.P, cx.nc
    sm = cx.small
    cT = sm[:, 0:32]
    cb = sm[:, 32:36]
    eT = sm[:, 36:40]
    deps0 = list(start_deps) + cx.free["RB"] + cx.free["RF"]
    t_c = P.dma("sync", cT, cT_d, "mods_in", 2, deps=deps0)
    t_cb = P.dma("sync", cb, condbT_d, "mods_in", 2)
    cw_v = condw_d.rearrange("(kc p) r -> p kc r", p=128)
    psb = cx.ps[6]
    last = None
    act_tok = None
    for j in range(4):
        buf = cx.xt[j % 2].rearrange("p (k c) -> p k c", k=KC)
        tw = P.dma("sync", buf, cw_v[:, :, j * 128:(j + 1) * 128], "mods_w", 2, deps=deps0 + [act_tok])
        for k in range(KC):
            mm = P.op("tensor", lambda e, buf=buf, k=k: e.matmul(psb[:, 0:1], buf[:, k, :], cT[:, k:k + 1],
                                                               start=(k == 0), stop=(k == KC - 1)),
                      deps=[tw, t_c, cx.psfree[6]] if k == 0 else [], sig=(k == KC - 1))
        act_tok = P.op("scalar", lambda e, j=j: e.activation(out=eT[:, j:j + 1], in_=psb[:, 0:1], func=AF.Silu,
                                                             bias=cb[:, j:j + 1], scale=1.0), deps=[mm, t_cb])
        cx.psfree[6] = act_tok
    aw_v = adaw_d.rearrange("(j p) c -> p j c", p=128)
    nblk = (6 * D) // 1024 if nblk is None else nblk
    row = cx.hb[0].bitcast(F32)
    ev_tok = [None, None]
    st_tok = [None, None]
    stores = []
    for b in range(nblk):
        buf = cx.xt[b % 2].rearrange("p (j c) -> p j c", j=4)
        tw = P.dma("sync", buf, aw_v[:, :, b * 1024:(b + 1) * 1024], "mods_w", 2, deps=[act_tok, ev_tok[b % 2]])
        slot = b % 2
        brow = row[0:1, slot * 1024:(slot + 1) * 1024]
        tb = P.dma("sync", brow, adab_d[0:1, b * 1024:(b + 1) * 1024], "mods_b", 2, deps=[st_tok[slot]] + deps0)
        evs = []
        for hf in range(2):
            pst = cx.ps[6 + hf]
            for j in range(4):
                mm = P.op("tensor", lambda e, buf=buf, j=j, hf=hf, pst=pst: e.matmul(
                    pst[0:1, :], eT[:, j:j + 1], buf[:, j, hf * 512:(hf + 1) * 512], start=(j == 0), stop=(j == 3)),
                    deps=[tw, act_tok, cx.psfree[6 + hf]] if j == 0 else [], sig=(j == 3))
            ev = P.op("vector", lambda e, pst=pst, brow=brow, hf=hf: e.tensor_tensor(
                out=brow[:, hf * 512:(hf + 1) * 512], in0=pst[0:1, :], in1=brow[:, hf * 512:(hf + 1) * 512], op=ALU.add),
                deps=[mm, tb])
            cx.psfree[6 + hf] = ev
            evs.append(ev)
        ev_tok[b % 2] = evs[-1]
        st = P.dma("sync", mod_d[0:1, b * 1024:(b + 1) * 1024], brow, "mods_st", 2, deps=evs)
        st_tok[slot] = st
        stores.append(st)
    cx.free["RB"] = [ev_tok[0], ev_tok[1]]
    cx.free["RF"] = stores[-2:]
    return stores[-2:]


def load_bc(cx, dst, row_ap, ring, deps):
    return cx.P.dma("sync", dst, row_ap.to_broadcast([128, row_ap.shape[-1]]), ring, 1, deps=deps)


def phase_prep(cx, x_rows, g_row, mod_d, i_scale, i_shift, i_gate, x_ready, mod_ready, n_tiles=NT, tok_off=0, halo=False):
    P = cx.P
    pre = list(x_ready) + list(mod_ready)
    d_rd = pre + cx.free["RD"]
    d_rb = pre + cx.free["RB"]
    t_sc = load_bc(cx, cx.geff, mod_d[0:1, i_scale * D:(i_scale + 1) * D], "bc0", d_rd)
    t_g = load_bc(cx, cx.xt[0], g_row, "bc1", d_rb)
    t_sh = load_bc(cx, cx.shift, mod_d[0:1, i_shift * D:(i_shift + 1) * D], "bc2", d_rd)
    t_gt = load_bc(cx, cx.gate, mod_d[0:1, i_gate * D:(i_gate + 1) * D], "bc3", pre + cx.free["RE"])
    tg = P.op("vector", lambda e: e.scalar_tensor_tensor(out=cx.geff, in0=cx.geff, scalar=1.0, in1=cx.xt[0],
                                                         op0=ALU.add, op1=ALU.mult), deps=[t_sc, t_g])
    sm = cx.small
    xt_free = [tg, None]
    xt_free[1] = None
    hb_free = [None, None]
    rf_dep = cx.free["RF"]
    ra_dep = cx.free["RA"]
    done = []
    xt_last = []
    for tt in range(n_tiles):
        b = tt % 2
        xt = cx.xt[b]
        hb = cx.hb[b]
        ss = sm[:, 40 + b:41 + b]
        rs = sm[:, 42 + b:43 + b]
        tl = P.dma("sync", xt, x_rows[tt * 128:(tt + 1) * 128, :], "xt", 2, deps=d_rb + [xt_free[b]])
        t1 = P.op("scalar", lambda e, xt=xt, hb=hb, ss=ss: e.activation(out=hb, in_=xt, func=AF.Square, accum_out=ss),
                  deps=[tl, hb_free[b]] + rf_dep)
        t2 = P.op("scalar", lambda e, ss=ss, rs=rs: e.activation(out=rs, in_=ss, func=AF.Sqrt, bias=cx.epsb, scale=1.0 / D),
                  deps=[t1])
        t3 = P.op("vector", lambda e, rs=rs: e.reciprocal(out=rs, in_=rs), deps=[t2])
        t4 = P.op("vector", lambda e, xt=xt, rs=rs: e.scalar_tensor_tensor(out=xt, in0=xt, scalar=rs, in1=cx.geff,
                                                                           op0=ALU.mult, op1=ALU.mult), deps=[t3, tg, tl])
        t5 = P.op("vector", lambda e, xt=xt, hb=hb: e.tensor_tensor(out=hb, in0=xt, in1=cx.shift, op=ALU.add),
                  deps=[t4, t_sh, t1])
        xt_free[b] = t5
        xt_last.append(t5)
        cps = []
        for gq in range(4):
            pb = 6 + (gq % 2)
            pst = cx.ps[pb][:, :].bitcast(BF16).rearrange("p (k t) -> p k t", k=8)
            for kk in range(8):
                k = gq * 8 + kk
                mm = P.op("tensor", lambda e, pst=pst, kk=kk, k=k, hb=hb: e.transpose(pst[:, kk, :], hb[:, k * 128:(k + 1) * 128], cx.ident[:, :]),
                          deps=[t5, cx.psfree[pb]] if kk == 0 else [], sig=(kk == 7))
            dst = cx.hT[:, gq * 8:(gq + 1) * 8, tok_off + tt * 128: tok_off + (tt + 1) * 128]
            if halo:
                dst = cx.HH[:, gq * 8:(gq + 1) * 8, :]
                pst = pst[:, :, 96:128]
            if gq % 2 == 0:
                cp = P.op("scalar", lambda e, dst=dst, pst=pst: e.activation(out=dst, in_=pst, func=AF.Copy), deps=[mm] + ra_dep)
            else:
                cp = P.op("vector", lambda e, dst=dst, pst=pst: e.tensor_copy(out=dst, in_=pst), deps=[mm] + ra_dep)
            cx.psfree[pb] = cp
            cps.append(cp)
        hb_free[b] = mm
        done += cps[-2:]
    cx.free["RB"] = xt_last[-2:]
    cx.free["RF"] = [hb_free[0], hb_free[1]]
    cx.free["RD"] = xt_last[-2:]
    if not halo:
        cx.free["RA"] = []
    return done, t_gt


def defer_store(cx, dst, xs, dep, xi, blk_store, key):
    cx.pending = (dst, xs, dep, xi, blk_store, key)


def flush_store(cx):
    if getattr(cx, "pending", None) is None:
        return
    dst, xs, dep, xi, blk_store, key = cx.pending
    cx.pending = None
    st = cx.P.dma("sync", dst, xs, "xs_st", 4, deps=[dep])
    cx.xs_free[xi] = st
    blk_store[key] = st


def phase_ffn(cx, x_rows_in, x_rows_out, blocks, hT_ready, gate_ready, comb=None, comb_ready=()):
    P = cx.P
    FB = 2048
    MB = FB // 128
    wa_free = [None, None]
    wb_free = [None, None]
    blk_store = {}
    wa_it = 0
    wb_it = 0
    g1_ps = 0
    g2_ps = 0
    last_stores = []
    mid_ready_prev = cx.free["RB"]
    for bi, (wg, wu, wd, ei) in enumerate(blocks):
        wg_v = wg.rearrange("(kc p) c -> p kc c", p=128)
        wu_v = wu.rearrange("(kc p) c -> p kc c", p=128)
        wd_v = wd.rearrange("(kc p) c -> p kc c", p=128)
        mid_toks = []
        for m in range(MB):
            slot = wa_it % 2
            wa_it += 1
            wsl = cx.wA[slot]
            dd = [wa_free[slot]] + (cx.free["RC"] if wa_it <= 2 else [])
            t_wg = P.dma("gpsimd", wsl[:, :, 0:128], wg_v[:, :, m * 128:(m + 1) * 128], "wA", 2, deps=dd)
            t_wu = P.dma("gpsimd", wsl[:, :, 128:256], wu_v[:, :, m * 128:(m + 1) * 128], "wAu", 2, deps=dd)
            for n in range(TP // 512):
                pg = cx.ps[(g1_ps % 2) * 2]
                pu = cx.ps[(g1_ps % 2) * 2 + 1]
                pgi = (g1_ps % 2) * 2
                g1_ps += 1
                for k in range(KC):
                    P.op("tensor", lambda e, pg=pg, wsl=wsl, k=k, n=n: e.matmul(pg[:, :], wsl[:, k, 0:128], cx.hT[:, k, n * 512:(n + 1) * 512],
                                                                               start=(k == 0), stop=(k == KC - 1)),
                         deps=([t_wg, cx.psfree[pgi]] + list(hT_ready)) if k == 0 else [], sig=False)
                for k in range(KC):
                    mm = P.op("tensor", lambda e, pu=pu, wsl=wsl, k=k, n=n: e.matmul(pu[:, :], wsl[:, k, 128:256], cx.hT[:, k, n * 512:(n + 1) * 512],
                                                                                    start=(k == 0), stop=(k == KC - 1)),
                              deps=[t_wu, cx.psfree[pgi + 1]] if k == 0 else [], sig=(k == KC - 1))
                ti = cx.tmp_i % 2
                cx.tmp_i += 1
                tmp = cx.TMP[:, ti, :]
                a1 = P.op("scalar", lambda e, tmp=tmp, pg=pg: e.activation(out=tmp, in_=pg[:, :], func=AF.Silu),
                          deps=[mm, cx.tmp_free[ti]])
                dst = cx.midT[:, m, n * 512:(n + 1) * 512]
                v1 = P.op("vector", lambda e, dst=dst, tmp=tmp, pu=pu: e.tensor_tensor(out=dst, in0=tmp, in1=pu[:, :], op=ALU.mult),
                          deps=[a1, mm] + list(mid_ready_prev))
                cx.tmp_free[ti] = v1
                cx.psfree[pgi] = a1
                cx.psfree[pgi + 1] = v1
                mid_toks.append(v1)
            wa_free[slot] = mm
        mid_done = mid_toks[-1:]
        gemm2_last = None
        for cb in range(D // 512):
            slot = wb_it % 2
            wb_it += 1
            wsl = cx.wB16[slot]
            dd = [wb_free[slot]] + (cx.free["RD"] if wb_it <= 2 else [])
            t_wd = P.dma("gpsimd", wsl, wd_v[:, :, cb * 512:(cb + 1) * 512], "wB", 2, deps=dd)
            for tt in range(NT):
                pbi = 4 + (g2_ps % 2)
                g2_ps += 1
                pb = cx.ps[pbi]
                for k in range(MB):
                    mm = P.op("tensor", lambda e, pb=pb, wsl=wsl, k=k, tt=tt: e.matmul(pb[:, :], cx.midT[:, k, tt * 128:(tt + 1) * 128], wsl[:, k, :],
                                                                                      start=(k == 0), stop=(k == MB - 1)),
                              deps=([t_wd, cx.psfree[pbi]] + mid_done) if k == 0 else [], sig=(k == MB - 1))
                xi = cx.xs_i % 2
                cx.xs_i += 1
                xs = cx.XS[:, xi, :]
                key = (tt, cb)
                src = x_rows_in if bi == 0 else x_rows_out
                tl = P.dma("sync", xs, src[tt * 128:(tt + 1) * 128, cb * 512:(cb + 1) * 512], "xs_ld", 2,
                           deps=[cx.xs_free[xi], blk_store.get(key)])
                flush_store(cx)
                ti = cx.tmp_i % 2
                cx.tmp_i += 1
                tmp = cx.TMP[:, ti, :]
                gsl = cx.gate[:, cb * 512:(cb + 1) * 512]
                if ei is None:
                    v1 = P.op("vector", lambda e, tmp=tmp, pb=pb, gsl=gsl: e.tensor_tensor(out=tmp, in0=pb[:, :], in1=gsl, op=ALU.mult),
                              deps=[mm, cx.tmp_free[ti], gate_ready])
                else:
                    csc = comb[:, tt, ei:ei + 1]
                    v1 = P.op("vector", lambda e, tmp=tmp, pb=pb, gsl=gsl, csc=csc: e.scalar_tensor_tensor(
                        out=tmp, in0=pb[:, :], scalar=csc, in1=gsl, op0=ALU.mult, op1=ALU.mult),
                        deps=[mm, cx.tmp_free[ti], gate_ready] + list(comb_ready))
                cx.psfree[pbi] = v1
                v2 = P.op("vector", lambda e, xs=xs, tmp=tmp: e.tensor_tensor(out=xs, in0=xs, in1=tmp, op=ALU.add), deps=[v1, tl])
                cx.tmp_free[ti] = v2
                defer_store(cx, x_rows_out[tt * 128:(tt + 1) * 128, cb * 512:(cb + 1) * 512], xs, v2, xi, blk_store, key)
                gemm2_last = mm
            wb_free[slot] = gemm2_last
        mid_ready_prev = [gemm2_last]
    flush_store(cx)
    cx.free["RB"] = [gemm2_last]
    cx.free["RC"] = [wa_free[0], wa_free[1]]
    cx.free["RD"] = [wb_free[0], wb_free[1]]
    cx.free["RA"] = [wa_free[0], wa_free[1]]
    cx.free["RE"] = [v2]
    return list(blk_store.values())


def phase_conv(cx, ps_idx, x_rows, x_rows_out, w_in, w_out, bo_d, cT_d, hs_d, hT_ready, gate_ready, cpar_ready, hh_ready):
    P = cx.P
    cp = cx.RF
    bval, bgate, bdw, lng, lnb = (cp[:, i * 32:(i + 1) * 32] for i in range(5))
    wdw = cp[:, 160:160 + 32 * CW].rearrange("p (m k) -> p m k", k=CW)
    flag = cx.small[:, 61:62]
    win_v = w_in.rearrange("(kc p) c -> p kc c", p=128)
    wout_v = w_out.rearrange("(kc p) c -> p kc c", p=128)
    GLW = 32 + TP
    GL = [cx.RB[:, i * GLW:(i + 1) * GLW] for i in range(2)]
    o = 2 * GLW
    ACC = [cx.RB[:, o + i * TP:o + (i + 1) * TP] for i in range(2)]
    o += 2 * TP
    CBF = [cx.RB[:, o + i * 512:o + (i + 1) * 512].bitcast(BF16) for i in range(2)]
    o += 1024
    CSQ = [cx.RB[:, o + i * 512:o + (i + 1) * 512].bitcast(BF16) for i in range(2)]
    o += 1024
    assert o <= 8192
    ones = cx.ones_bf
    rb_dep = cx.free["RB"]
    rc_dep = cx.free["RC"]
    wa_free = [None, None]
    gl_free = [None, None]
    acc_free = [None, None]
    cb_free = [None, None]
    cst_tok = []
    pe_last = None
    for m in range(32):
        slot = m % 2
        wsl = cx.wA[slot]
        dd = [wa_free[slot]] + (rc_dep if m < 2 else [])
        t_wv = P.dma("gpsimd", wsl[:, :, 0:128], win_v[:, :, m * 128:(m + 1) * 128], "wA", 2, deps=dd)
        t_wg = P.dma("gpsimd", wsl[:, :, 128:256], win_v[:, :, D + m * 128:D + (m + 1) * 128], "wAu", 2, deps=dd)
        gl = GL[slot]
        acc = ACC[slot]
        glu_toks = []
        segs = [(n * 512, 512, 32 + n * 512, cx.hT[:, :, n * 512:(n + 1) * 512], 0, 1) for n in range(2)]
        halo_mode = getattr(cx, "halo_mode", None) or ("hh" if ps_idx == 0 else "load")
        save_halo = getattr(cx, "save_halo", None)
        if save_halo is None:
            save_halo = (ps_idx == 0)
        if halo_mode == "hh":
            segs.append((0, 32, 0, cx.HH, 6, 7))
        for (t0, tn, g0, src, bv, bg) in segs:
            pv = cx.ps[bv]
            pg = cx.ps[bg]
            for k in range(KC):
                P.op("tensor", lambda e, pv=pv, wsl=wsl, k=k, src=src, tn=tn: e.matmul(pv[:, 0:tn], wsl[:, k, 0:128], src[:, k, :],
                                                                                      start=(k == 0), stop=(k == KC - 1)),
                     deps=([t_wv, cx.psfree[bv]] + list(hT_ready) + list(hh_ready)) if k == 0 else [], sig=False)
            for k in range(KC):
                mm = P.op("tensor", lambda e, pg=pg, wsl=wsl, k=k, src=src, tn=tn: e.matmul(pg[:, 0:tn], wsl[:, k, 128:256], src[:, k, :],
                                                                                           start=(k == 0), stop=(k == KC - 1)),
                          deps=[t_wg, cx.psfree[bg]] if k == 0 else [], sig=(k == KC - 1))
            ti = cx.tmp_i % 2
            cx.tmp_i += 1
            tmp = cx.TMP[:, ti, 0:tn]
            a1 = P.op("scalar", lambda e, tmp=tmp, pg=pg, tn=tn, m=m: e.activation(out=tmp, in_=pg[:, 0:tn], func=AF.Sigmoid,
                                                                                 bias=bgate[:, m:m + 1], scale=1.0),
                      deps=[mm, cx.tmp_free[ti], cpar_ready])
            v1 = P.op("vector", lambda e, gl=gl, g0=g0, tn=tn, pv=pv, tmp=tmp, m=m: e.scalar_tensor_tensor(
                out=gl[:, g0:g0 + tn], in0=pv[:, 0:tn], scalar=bval[:, m:m + 1], in1=tmp, op0=ALU.add, op1=ALU.mult),
                deps=[a1, gl_free[slot], cpar_ready] + rb_dep)
            cx.tmp_free[ti] = v1
            cx.psfree[bv] = v1
            cx.psfree[bg] = a1
            glu_toks.append(v1)
            pe_last = mm
        wa_free[slot] = pe_last
        hl = None
        if halo_mode == "hh":
            hv = P.op("vector", lambda e, gl=gl: e.tensor_scalar(out=gl[:, 0:32], in0=gl[:, 0:32], scalar1=flag, scalar2=None, op0=ALU.mult),
                      deps=[glu_toks[-1]])
            glu_toks.append(hv)
        elif halo_mode == "zero":
            hv = P.op("vector", lambda e, gl=gl: e.memset(gl[:, 0:32], 0.0), deps=[gl_free[slot]] + rb_dep)
            glu_toks.append(hv)
        else:
            hl = P.dma("sync", gl[:, 0:32], hs_d[m], "hs_ld", 2, deps=[gl_free[slot]] + rb_dep)
            glu_toks.append(hl)
        if save_halo:
            P.dma("sync", hs_d[m], gl[:, TP:TP + 32], "hs_st", 2, deps=[glu_toks[1], hl])
        c0 = P.op("vector", lambda e, acc=acc, gl=gl, m=m: e.tensor_scalar(out=acc, in0=gl[:, 2:2 + TP], scalar1=wdw[:, m, 0:1], scalar2=bdw[:, m:m + 1],
                                                                          op0=ALU.mult, op1=ALU.add),
                  deps=glu_toks + [acc_free[slot]])
        for k in range(1, CW):
            ck = P.op("vector", lambda e, acc=acc, gl=gl, m=m, k=k: e.scalar_tensor_tensor(
                out=acc, in0=gl[:, 2 + k:2 + k + TP], scalar=wdw[:, m, k:k + 1], in1=acc, op0=ALU.mult, op1=ALU.add),
                deps=[], sig=(k == CW - 1))
        gl_free[slot] = ck
        cbf = CBF[slot]
        csq = CSQ[slot]
        s1 = P.op("scalar", lambda e, cbf=cbf, acc=acc: e.activation(out=cbf, in_=acc, func=AF.Copy), deps=[ck, cb_free[slot]])
        s2 = P.op("scalar", lambda e, csq=csq, acc=acc: e.activation(out=csq, in_=acc, func=AF.Square), deps=[])
        st = P.dma("sync", cT_d[m], acc, "ct_st", 2, deps=[ck])
        cst_tok.append(st)
        for n in range(2):
            P.op("tensor", lambda e, n=n, cbf=cbf, m=m: e.matmul(cx.ps[2 + n][:, :], ones[:, :], cbf[:, n * 512:(n + 1) * 512],
                                                                start=(m == 0), stop=(m == 31)),
                 deps=[s2] + ([cx.psfree[2 + n], cx.psfree[4 + n]] if m == 0 else []), sig=False)
            mm = P.op("tensor", lambda e, n=n, csq=csq, m=m: e.matmul(cx.ps[4 + n][:, :], ones[:, :], csq[:, n * 512:(n + 1) * 512],
                                                                     start=(m == 0), stop=(m == 31)),
                      deps=[], sig=(n == 1))
        cb_free[slot] = mm
        acc_free[slot] = s2
        acc_free[slot] = st
        pe_last = mm
    o = 0
    MU = cx.RB[:, 0:TP]
    RS = cx.RB[:, TP:2 * TP]
    CT = [cx.RB[:, (2 + i) * TP:(3 + i) * TP] for i in range(3)]
    stat_deps = [pe_last, gl_free[0], gl_free[1], acc_free[0], acc_free[1]]
    for n in range(2):
        sl = slice(n * 512, (n + 1) * 512)
        a = P.op("vector", lambda e, n=n, sl=sl: e.tensor_scalar(out=MU[:, sl], in0=cx.ps[2 + n][:, :], scalar1=1.0 / D, scalar2=None, op0=ALU.mult),
                 deps=stat_deps)
        b = P.op("vector", lambda e, n=n, sl=sl: e.tensor_tensor(out=RS[:, sl], in0=MU[:, sl], in1=MU[:, sl], op=ALU.mult), deps=[a])
        c = P.op("vector", lambda e, n=n, sl=sl: e.scalar_tensor_tensor(out=RS[:, sl], in0=cx.ps[4 + n][:, :], scalar=1.0 / D, in1=RS[:, sl],
                                                                       op0=ALU.mult, op1=ALU.subtract), deps=[b])
        d = P.op("scalar", lambda e, sl=sl: e.activation(out=RS[:, sl], in_=RS[:, sl], func=AF.Sqrt, bias=cx.epsb, scale=1.0), deps=[c])
        f = P.op("vector", lambda e, sl=sl: e.reciprocal(out=RS[:, sl], in_=RS[:, sl]), deps=[d])
        cx.psfree[2 + n] = a
        cx.psfree[4 + n] = c
    stats_done = f
    ra_dep = [wa_free[0], wa_free[1]]
    ct_free = [None] * 3
    zt_toks = []
    for m in range(32):
        bi = m % 3
        ct = CT[bi]
        tl = P.dma("sync", ct, cT_d[m], "ct_ld", 3, deps=[cst_tok[m], ct_free[bi], stats_done])
        v1 = P.op("vector", lambda e, ct=ct: e.tensor_tensor(out=ct, in0=ct, in1=MU, op=ALU.subtract), deps=[tl, stats_done])
        v2 = P.op("vector", lambda e, ct=ct: e.tensor_tensor(out=ct, in0=ct, in1=RS, op=ALU.mult), deps=[v1])
        a1 = P.op("scalar", lambda e, ct=ct, m=m: e.activation(out=cx.hT[:, m, :], in_=ct, func=AF.Silu, bias=lnb[:, m:m + 1], scale=lng[:, m:m + 1]),
                  deps=[v2] + ra_dep)
        ct_free[bi] = a1
        zt_toks.append(a1)
    z_done = zt_toks[-1:]
    wb_free = [None, None]
    rd_dep = cx.free["RD"]
    bo_free = [None, None]
    stores = {}
    g2 = 0
    BO = [cx.small[:, 128:192], cx.small[:, 192:256]]
    BOB = [cx.RB[:, (5 * TP) + i * 256:(5 * TP) + (i + 1) * 256] for i in range(2)]
    for cb in range(D // 256):
        slot = cb % 2
        wsl = cx.wB32[slot]
        dd = [wb_free[slot]] + (rd_dep if cb < 2 else [])
        t_w = P.dma("gpsimd", wsl, wout_v[:, :, cb * 256:(cb + 1) * 256], "wB", 2, deps=dd)
        bob = BOB[slot]
        t_b = P.dma("sync", bob, bo_d[0:1, cb * 256:(cb + 1) * 256].to_broadcast([128, 256]), "bo", 2, deps=[bo_free[slot], stats_done])
        for tt in range(NT):
            pbi = 6 + (g2 % 2)
            g2 += 1
            pb = cx.ps[pbi]
            for k in range(KC):
                mm = P.op("tensor", lambda e, pb=pb, wsl=wsl, k=k, tt=tt: e.matmul(pb[:, 0:256], cx.hT[:, k, tt * 128:(tt + 1) * 128], wsl[:, k, :],
                                                                                  start=(k == 0), stop=(k == KC - 1)),
                          deps=([t_w, cx.psfree[pbi]] + z_done) if k == 0 else [], sig=(k == KC - 1))
            xi = cx.xs_i % 2
            cx.xs_i += 1
            xs = cx.XS[:, xi, 0:256]
            tl = P.dma("sync", xs, x_rows[tt * 128:(tt + 1) * 128, cb * 256:(cb + 1) * 256], "xs_ld", 2, deps=[cx.xs_free[xi]])
            flush_store(cx)
            ti = cx.tmp_i % 2
            cx.tmp_i += 1
            tmp = cx.TMP[:, ti, 0:256]
            v1 = P.op("vector", lambda e, tmp=tmp, pb=pb, bob=bob: e.tensor_tensor(out=tmp, in0=pb[:, 0:256], in1=bob, op=ALU.add),
                      deps=[mm, cx.tmp_free[ti], t_b])
            cx.psfree[pbi] = v1
            v2 = P.op("vector", lambda e, tmp=tmp, cb=cb: e.tensor_tensor(out=tmp, in0=tmp, in1=cx.gate[:, cb * 256:(cb + 1) * 256], op=ALU.mult),
                      deps=[v1, gate_ready])
            v3 = P.op("vector", lambda e, xs=xs, tmp=tmp: e.tensor_tensor(out=xs, in0=xs, in1=tmp, op=ALU.add), deps=[v2, tl])
            cx.tmp_free[ti] = v3
            defer_store(cx, x_rows_out[tt * 128:(tt + 1) * 128, cb * 256:(cb + 1) * 256], xs, v3, xi, stores, (tt, cb))
            last_mm = mm
        wb_free[slot] = last_mm
        bo_free[slot] = v1
    flush_store(cx)
    cx.free["RA"] = [last_mm]
    cx.free["RB"] = [v3, zt_toks[-1]]
    cx.free["RC"] = [wa_free[0], wa_free[1]]
    cx.free["RD"] = [wb_free[0], wb_free[1]]
    cx.free["RE"] = [v3]
    cx.free["RF"] = [zt_toks[-1], ck]
    return list(stores.values())


def init_consts(cx, ident_d, identf_d, flag_d):
    P = cx.P
    t1 = P.dma("sync", cx.ident[:, :], ident_d, "const", 3)
    t2 = P.dma("sync", cx.trif[:, :], identf_d, "const", 3)
    P.op("vector", lambda e: e.memset(cx.onesf[:, :], 1.0))
    P.op("vector", lambda e: e.memset(cx.oneb, 1.0))
    t3 = P.dma("sync", cx.small[:, 61:62], flag_d, "const", 3)
    t4 = P.op("vector", lambda e: e.memset(cx.ones_bf[:, :], 1.0))
    t5 = P.op("vector", lambda e: e.memset(cx.epsb, EPS))
    return [t1, t2, t3, t4, t5]


def build_conv_layer(with_ffn=True):
    nc = bass.Bass("TRN2", target_bir_lowering=False)
    dt = lambda name, shape, kind="ExternalInput": nc.dram_tensor(name, shape, F32, kind=kind).ap()
    x_in = dt("x_in", [TCORE, D])
    x_halo = dt("x_halo", [128, D])
    flag_d = dt("flag", [128, 1])
    ident_d = nc.dram_tensor("ident", [128, 128], BF16, kind="ExternalInput").ap()
    identf_d = dt("identf", [64, 64])
    cT_in = dt("cT", [128, 32])
    condw = dt("cond_w", [D, R])
    condbT = dt("cond_bT", [128, 4])
    adaw = dt("ada_w", [R, 6 * D])
    adab = dt("ada_b", [1, 6 * D])
    g_mix = dt("g_mix", [1, D])
    g_ffn = dt("g_ffn", [1, D])
    w_in = dt("w_in", [D, 2 * D])
    cpar_d = dt("cpar", [128, 160 + 32 * CW])
    w_out = dt("w_out", [D, D])
    b_out = dt("b_out", [1, D])
    wg = dt("ffn_wg", [D, 2 * D])
    wu = dt("ffn_wu", [D, 2 * D])
    wd = dt("ffn_wd", [2 * D, D])
    x_out = dt("x_out", [TCORE, D], kind="ExternalOutput")
    mod_d = nc.dram_tensor("mod_s", [1, 6 * D], F32).ap()
    cT_s = nc.dram_tensor("cT_s", [32, 128, TP], F32).ap()
    hs_s = nc.dram_tensor("hs_s", [32, 128, 32], F32).ap()

    P = Prog(nc)
    cx = Ctx(nc, P)
    c0 = init_consts(cx, ident_d, identf_d, flag_d)
    mod_ready = phase_mods(cx, cT_in, condw, condbT, adaw, adab, mod_d, c0)
    final = []
    for ps_idx in range(NPASS):
        rows = slice(ps_idx * TP, (ps_idx + 1) * TP)
        hh_ready = []
        if ps_idx == 0:
            hh_ready, _ = phase_prep(cx, x_halo, g_mix, mod_d, 1, 0, 2, c0, mod_ready, n_tiles=1, halo=True)
        hT_ready, gate_ready = phase_prep(cx, x_in[rows, :], g_mix, mod_d, 1, 0, 2, c0, mod_ready)
        cpar_ready = P.dma("sync", cx.RF[:, 0:160 + 32 * CW], cpar_d, "cpar", 1, deps=cx.free["RF"])
        st = phase_conv(cx, ps_idx, x_in[rows, :], x_out[rows, :], w_in, w_out, b_out, cT_s, hs_s, hT_ready, gate_ready, cpar_ready, hh_ready)
        if with_ffn:
            hT_ready, gate_ready = phase_prep(cx, x_out[rows, :], g_ffn, mod_d, 4, 3, 5, st, mod_ready)
            blocks = [(wg[:, j * 2048:(j + 1) * 2048], wu[:, j * 2048:(j + 1) * 2048], wd[j * 2048:(j + 1) * 2048, :], None) for j in range(4)]
            st = phase_ffn(cx, x_out[rows, :], x_out[rows, :], blocks, hT_ready, gate_ready)
        final += st
    P.emit(final)
    return nc, P


def _pmajor(v, kc):
    return np.ascontiguousarray(np.asarray(v, np.float32).reshape(kc, 128).T)


def _consts():
    import ml_dtypes
    return {"ident": np.eye(128, dtype=ml_dtypes.bfloat16), "identf": np.triu(np.ones((64, 64), np.float32))}


def _core_rows(x, core):
    b, half = core // 2, core % 2
    return b, half, x[b, half * TCORE:(half + 1) * TCORE]


def _halo_rows(xfull_b, half):
    if half == 0:
        return np.zeros((128, D), np.float32)
    return np.ascontiguousarray(xfull_b[TCORE - 128:TCORE])


def conv_layer_inputs(I, x, i, core):
    j = i // 2
    b, half, xr = _core_rows(x, core)
    cpar = np.concatenate([
        _pmajor(I["conv_b_in"][j][:D], 32), _pmajor(I["conv_b_in"][j][D:], 32), _pmajor(I["conv_b_dw"][j], 32),
        _pmajor(I["conv_ln_g"][j], 32), _pmajor(I["conv_ln_b"][j], 32),
        np.ascontiguousarray(np.asarray(I["conv_w_dw"][j], np.float32).reshape(CW, 32, 128).transpose(2, 1, 0)).reshape(128, 32 * CW),
    ], axis=1)
    m = {
        "x_in": np.ascontiguousarray(xr), "x_halo": _halo_rows(x[b], half),
        "flag": np.full((128, 1), float(half), np.float32),
        "cT": _pmajor(I["c"][b], 32), "cond_w": I["cond_w"], "cond_bT": _pmajor(I["cond_b"], 4),
        "ada_w": I["ada_w"][i], "ada_b": I["ada_b"][i][None, :],
        "g_mix": I["mix_norm_g"][i][None, :], "g_ffn": I["ffn_norm_g"][i][None, :],
        "w_in": I["conv_w_in"][j], "cpar": np.ascontiguousarray(cpar), "w_out": I["conv_w_out"][j], "b_out": I["conv_b_out"][j][None, :],
        "ffn_wg": I["ffn_w_gate"][j], "ffn_wu": I["ffn_w_up"][j], "ffn_wd": I["ffn_w_down"][j],
    }
    m.update(_consts())
    return m


def phase_router(cx, wr_d, br_d, hT_ready):
    P = cx.P
    WR = cx.WRt[:, 0:KC * 8].rearrange("p (k e) -> p k e", e=8)
    t_w = P.dma("gpsimd", WR, wr_d.rearrange("(kc p) e -> p kc e", p=128), "wr", 1, deps=cx.comb_free)
    brb = cx.small[:, 64:72]
    t_b = P.dma("sync", brb, br_d.to_broadcast([128, 8]), "wrb", 1, deps=cx.comb_free)
    sm = cx.small
    last = None
    for tt in range(NT):
        pb = cx.ps[6 + tt % 2]
        for k in range(KC):
            mm = P.op("tensor", lambda e, pb=pb, k=k, tt=tt: e.matmul(pb[:, 0:8], cx.hT[:, k, tt * 128:(tt + 1) * 128], WR[:, k, :],
                                                                     start=(k == 0), stop=(k == KC - 1)),
                      deps=([t_w, cx.psfree[6 + tt % 2]] + list(hT_ready)) if k == 0 else [], sig=(k == KC - 1))
        lg = sm[:, 72:80]
        mx = sm[:, 80:88]
        w12 = sm[:, 88:90]
        dd = sm[:, 90:92]
        c1 = sm[:, 96:104]
        v0 = P.op("vector", lambda e, pb=pb: e.tensor_tensor(out=lg, in0=pb[:, 0:8], in1=brb, op=ALU.add), deps=[mm, t_b, last])
        cx.psfree[6 + tt % 2] = v0
        v1 = P.op("vector", lambda e: e.max(out=mx, in_=lg), deps=[v0])
        v2 = P.op("vector", lambda e: e.tensor_tensor(out=dd[:, 0:1], in0=mx[:, 0:1], in1=mx[:, 1:2], op=ALU.subtract), deps=[v1])
        v3 = P.op("vector", lambda e: e.tensor_tensor(out=dd[:, 1:2], in0=mx[:, 1:2], in1=mx[:, 0:1], op=ALU.subtract), deps=[v2])
        a1 = P.op("scalar", lambda e: e.activation(out=w12, in_=dd, func=AF.Sigmoid), deps=[v3])
        v4 = P.op("vector", lambda e: e.tensor_scalar(out=c1, in0=lg, scalar1=mx[:, 0:1], scalar2=w12[:, 0:1], op0=ALU.is_equal, op1=ALU.mult), deps=[a1])
        v5 = P.op("vector", lambda e, tt=tt: e.tensor_scalar(out=cx.comb[:, tt, :], in0=lg, scalar1=mx[:, 1:2], scalar2=w12[:, 1:2], op0=ALU.is_equal, op1=ALU.mult), deps=[v4])
        v6 = P.op("vector", lambda e, tt=tt: e.tensor_tensor(out=cx.comb[:, tt, :], in0=cx.comb[:, tt, :], in1=c1, op=ALU.add), deps=[v5])
        last = v6
    return [last]


def phase_mlstm(cx, ps_idx, x_rows, x_rows_out, w_in, w_out, bg_d, ng_d, mod_d, qk_d, v_d, o_d, g_d, st_c, st_n,
                hT_ready, mod_ready, state_ready):
    P = cx.P
    sm = cx.small
    win_v = w_in.rearrange("(kc p) c -> p kc c", p=128)
    wout_v = w_out.rearrange("(kc p) c -> p kc c", p=128)
    STG = [cx.TMP[:, i, :].bitcast(BF16) for i in range(2)]
    wa_free = [None, None]
    rc_dep = cx.free["RC"]
    stg_free = [cx.tmp_free[0], cx.tmp_free[1]]
    si = 0
    sc_writes = []
    pi = 0
    pe_last = None
    for m in range(32):
        slot = m % 2
        wsl = cx.wA[slot]
        dd = [wa_free[slot]] + (rc_dep if m < 2 else [])
        t_w = P.dma("gpsimd", wsl[:, :, 0:128], win_v[:, :, m * 128:(m + 1) * 128], "wA", 2, deps=dd)
        for n in range(2):
            pbi = pi % 4
            pi += 1
            pb = cx.ps[pbi]
            for k in range(KC):
                mm = P.op("tensor", lambda e, pb=pb, wsl=wsl, k=k, n=n: e.matmul(pb[:, :], wsl[:, k, 0:128], cx.hT[:, k, n * 512:(n + 1) * 512],
                                                                                start=(k == 0), stop=(k == KC - 1)),
                          deps=([t_w, cx.psfree[pbi]] + list(hT_ready)) if k == 0 else [], sig=(k == KC - 1))
            s = si % 2
            si += 1
            stg = STG[s][:, 0:512]
            sc = 1.0 if m < 16 else DK ** -0.5
            a1 = P.op("scalar", lambda e, stg=stg, pb=pb, sc=sc: e.activation(out=stg, in_=pb[:, :], func=AF.Copy, scale=sc), deps=[mm, stg_free[s]])
            cx.psfree[pbi] = a1
            st = P.dma("sync", qk_d[m, :, n * 512:(n + 1) * 512], stg, "sc_st", 4, deps=[a1])
            stg_free[s] = st
            sc_writes.append(st)
            pe_last = mm
        wa_free[slot] = pe_last
    for cbk in range(32):
        slot = cbk % 2
        wsl = cx.wA[slot]
        t_w = P.dma("gpsimd", wsl, win_v[:, :, 4096 + cbk * 256:4096 + (cbk + 1) * 256], "wA", 2, deps=[wa_free[slot]])
        dst_d = v_d if cbk < 16 else o_d
        c0 = (cbk % 16) * 256
        for tt in range(NT):
            pbi = pi % 4
            pi += 1
            pb = cx.ps[pbi]
            for k in range(KC):
                mm = P.op("tensor", lambda e, pb=pb, wsl=wsl, k=k, tt=tt: e.matmul(pb[:, 0:256], cx.hT[:, k, tt * 128:(tt + 1) * 128], wsl[:, k, :],
                                                                                  start=(k == 0), stop=(k == KC - 1)),
                          deps=[t_w, cx.psfree[pbi]] if k == 0 else [], sig=(k == KC - 1))
            s = si % 2
            si += 1
            stg = STG[s][:, 0:256]
            if tt % 2 == 0:
                a1 = P.op("scalar", lambda e, stg=stg, pb=pb: e.activation(out=stg, in_=pb[:, 0:256], func=AF.Copy), deps=[mm, stg_free[s]])
            else:
                a1 = P.op("vector", lambda e, stg=stg, pb=pb: e.tensor_copy(out=stg, in_=pb[:, 0:256]), deps=[mm, stg_free[s]])
            cx.psfree[pbi] = a1
            st = P.dma("sync", dst_d[tt * 128:(tt + 1) * 128, c0:c0 + 256], stg, "sc_st", 4, deps=[a1])
            stg_free[s] = st
            sc_writes.append(st)
            pe_last = mm
        wa_free[slot] = pe_last
    WG = cx.WRt[:, 0:KC * 16].rearrange("p (k e) -> p k e", e=16)
    t_wg = P.dma("gpsimd", WG, win_v[:, :, 12288:12304], "wr", 1, deps=cx.comb_free)
    bgb = sm[:, 104:120]
    t_bg = P.dma("sync", bgb, bg_d.to_broadcast([128, 16]), "wrb", 1)
    gst = sm[:, 120:152].rearrange("p (b e) -> p b e", e=16)
    gfree = [None, None]
    for tt in range(NT):
        pbi = pi % 4
        pi += 1
        pb = cx.ps[pbi]
        for k in range(KC):
            mm = P.op("tensor", lambda e, pb=pb, k=k, tt=tt: e.matmul(pb[:, 0:16], cx.hT[:, k, tt * 128:(tt + 1) * 128], WG[:, k, :],
                                                                     start=(k == 0), stop=(k == KC - 1)),
                      deps=[t_wg, cx.psfree[pbi]] if k == 0 else [], sig=(k == KC - 1))
        v0 = P.op("vector", lambda e, pb=pb, tt=tt: e.tensor_tensor(out=gst[:, tt % 2, :], in0=pb[:, 0:16], in1=bgb, op=ALU.add),
                  deps=[mm, t_bg, gfree[tt % 2]])
        cx.psfree[pbi] = v0
        st = P.dma("sync", g_d[tt * 128:(tt + 1) * 128, :], gst[:, tt % 2, :], "sc_st", 4, deps=[v0])
        gfree[tt % 2] = st
        sc_writes.append(st)
        pe_last = mm
    cx.comb_free = [pe_last]
    cx.tmp_free = [stg_free[0], stg_free[1]]
    proj_done = [pe_last]
    sc_done = sc_writes[-4:]
    Cf = cx.RB[:, :].rearrange("p (g v) -> p g v", v=DV)
    Cb = cx.RD[:, 0:4096].bitcast(BF16).rearrange("p (g v) -> p g v", v=DV)
    QK = [cx.RD[:, 4096 + i * 1024:4096 + (i + 1) * 1024].bitcast(BF16).rearrange("p (m t) -> p m t", t=LCH) for i in range(2)]
    VC = [cx.RC[:, i * 2048:(i + 1) * 2048].bitcast(BF16) for i in range(2)]
    OC = [cx.RC[:, 4096 + i * 2048:4096 + (i + 1) * 2048].bitcast(BF16) for i in range(2)]
    NGB = cx.RE[:, :]
    NGS = cx.RF[:, :]
    yT = cx.hT
    nf = sm[:, 152:168]
    nb = cx.nbt[:, :]
    tri = cx.trif[0:64, 0:64]
    onesf = cx.onesf
    free0 = proj_done + cx.free["RB"] + cx.free["RD"] + cx.free["RE"] + cx.free["RF"]
    if getattr(cx, "zero_state", False) and ps_idx == 0:
        t_c = P.op("vector", lambda e: e.memset(cx.RB[:, :], 0.0), deps=free0 + list(state_ready))
        t_n = P.op("vector", lambda e: e.memset(nf, 0.0), deps=free0)
    else:
        t_c = P.dma("sync", Cf, st_c.rearrange("p (g v) -> p g v", v=DV), "st_ld", 2, deps=free0 + list(state_ready))
        t_n = P.dma("sync", nf, st_n, "st_ld", 2, deps=free0 + list(state_ready))
    t_ng = P.dma("sync", NGB, ng_d.to_broadcast([128, D]), "bc3", 1, deps=free0)
    cbt = []
    for g in range(16):
        cbt.append(P.op("scalar", lambda e, g=g: e.activation(out=Cb[:, g, :], in_=Cf[:, g, :], func=AF.Copy), deps=[t_c]))
    nbt = P.op("vector", lambda e: e.tensor_copy(out=nb, in_=nf), deps=[t_n])
    cb_ready = [cbt[-1]] * 16
    nb_ready = nbt
    buf_free = [None, None]
    ngs_free = None
    y_tr = None
    G = sm[0:64, 168:184]
    gw = cx.gwork
    for c in range(TP // LCH):
        bsel = c % 2
        rows = slice(c * LCH, (c + 1) * LCH)
        qk = QK[bsel]
        vc = VC[bsel]
        oc = OC[bsel]
        ld = []
        for qq in range(4):
            ld.append(P.dma("sync", qk[:, qq * 8:(qq + 1) * 8, :], qk_d[qq * 8:(qq + 1) * 8, :, rows].rearrange("m p t -> p m t"), "ch_ld", 8,
                            deps=sc_done + [buf_free[bsel]] + free0))
        ld.append(P.dma("sync", vc[0:64, :], v_d[rows, :], "ch_ld", 8, deps=sc_done + [buf_free[bsel]]))
        ld.append(P.dma("sync", oc[0:64, :], o_d[rows, :], "ch_ld", 8, deps=sc_done + [buf_free[bsel]]))
        tg = P.dma("sync", G, g_d[rows, :], "g_ld", 1, deps=sc_done + [ngs_free])
        ig = G[:, 0:8]
        fp = G[:, 8:16]
        ax, ee, ll, lf, aa, ea, eb, eg, dec = (gw[:, i * 8:(i + 1) * 8] for i in range(9))
        d00 = P.op("vector", lambda e: e.tensor_scalar(out=ax, in0=fp, scalar1=-1.0, scalar2=None, op0=ALU.mult), deps=[tg])
        d0 = P.op("vector", lambda e: e.tensor_tensor(out=ax, in0=ax, in1=fp, op=ALU.max), deps=[d00])
        a0 = P.op("scalar", lambda e: e.activation(out=ee, in_=ax, func=AF.Exp, scale=-1.0), deps=[d0])
        a1 = P.op("scalar", lambda e: e.activation(out=ll, in_=ee, func=AF.Ln, bias=cx.oneb[0:64, :], scale=1.0), deps=[a0])
        d1 = P.op("vector", lambda e: e.tensor_scalar(out=lf, in0=fp, scalar1=0.0, scalar2=None, op0=ALU.min), deps=[a1])
        d2 = P.op("vector", lambda e: e.tensor_tensor(out=lf, in0=lf, in1=ll, op=ALU.subtract), deps=[d1])
        pg = cx.ps[7]
        m0 = P.op("tensor", lambda e: e.matmul(pg[0:64, 0:8], tri, lf, start=True, stop=True), deps=[d2, cx.psfree[7]], sig=False)
        m1 = P.op("tensor", lambda e: e.matmul(pg[:, 8:16], onesf[0:64, :], lf, start=True, stop=True), deps=[])
        d3 = P.op("vector", lambda e: e.tensor_tensor(out=aa, in0=ig, in1=pg[0:64, 0:8], op=ALU.subtract), deps=[m1])
        a2 = P.op("scalar", lambda e: e.activation(out=ea, in_=aa, func=AF.Exp), deps=[d3])
        a3 = P.op("scalar", lambda e: e.activation(out=eb, in_=pg[0:64, 0:8], func=AF.Exp), deps=[m1])
        d4 = P.op("vector", lambda e: e.tensor_tensor(out=aa, in0=aa, in1=pg[0:64, 8:16], op=ALU.add), deps=[a2])
        a4 = P.op("scalar", lambda e: e.activation(out=eg, in_=aa, func=AF.Exp), deps=[d4])
        decb = cx.decb[:, :]
        a5 = P.op("scalar", lambda e: e.activation(out=decb, in_=pg[:, 8:16], func=AF.Exp), deps=[m1])
        cx.psfree[7] = a5
        pg_readers = [a5, d4]
        a6 = P.op("scalar", lambda e, oc=oc: e.activation(out=NGS[0:64, :], in_=oc[0:64, :], func=AF.Sigmoid), deps=[ld[5], ngs_free])
        d5 = P.op("vector", lambda e: e.tensor_tensor(out=NGS[0:64, :], in0=NGS[0:64, :], in1=NGB[0:64, :], op=ALU.mult), deps=[a6, t_ng])
        yb = cx.ybt[0:64, :]
        hd_last = None
        for h in range(H):
            pS = cx.ps[6]
            for i in range(2):
                mS = P.op("tensor", lambda e, h=h, i=i, qk=qk: e.matmul(pS[0:64, 0:64], qk[:, 16 + 2 * h + i, :], qk[:, 2 * h + i, :],
                                                                       start=(i == 0), stop=(i == 1)),
                          deps=([cx.psfree[6]] + ld[0:4]) if i == 0 else [], sig=(i == 1))
            Sm = cx.smt[0:64, (h % 2) * 64:(h % 2 + 1) * 64]
            d6 = P.op("vector", lambda e, Sm=Sm, h=h: e.scalar_tensor_tensor(out=Sm, in0=pS[0:64, 0:64], scalar=ea[:, h:h + 1], in1=tri,
                                                                            op0=ALU.mult, op1=ALU.mult), deps=[mS, a2, cx.sm_free[h % 2]])
            cx.psfree[6] = d6
            pbi = h % 2
            pN = cx.ps[pbi]
            pD = cx.ps[2 + pbi]
            P.op("tensor", lambda e, pN=pN, Sm=Sm, vc=vc, h=h: e.matmul(pN[0:64, :], Sm, vc[0:64, h * DV:(h + 1) * DV], start=True, stop=False),
                 deps=[d6, cx.psfree[pbi], ld[4], cb_ready[2 * h], cb_ready[2 * h + 1], nb_ready], sig=False)
            for i in range(2):
                P.op("tensor", lambda e, pN=pN, qk=qk, h=h, i=i: e.matmul(pN[0:64, :], qk[:, 2 * h + i, :], Cb[:, 2 * h + i, :], start=False, stop=(i == 1)),
                     deps=[], sig=False)
            P.op("tensor", lambda e, pD=pD, Sm=Sm: e.matmul(pD[0:64, 0:1], Sm, cx.ones_bf[0:64, 0:1], start=True, stop=False),
                 deps=[cx.psfree[2 + pbi]], sig=False)
            for i in range(2):
                mN = P.op("tensor", lambda e, pD=pD, qk=qk, h=h, i=i: e.matmul(pD[0:64, 0:1], qk[:, 2 * h + i, :], nb[:, 2 * h + i:2 * h + i + 1],
                                                                              start=False, stop=(i == 1)), deps=[], sig=(i == 1))
            cx.sm_free[h % 2] = mN
            hw = cx.hwork[0:64, (h % 2) * 8:(h % 2 + 1) * 8]
            t1, t2, ssq, rr = hw[:, 0:1], hw[:, 1:2], hw[:, 2:3], hw[:, 3:4]
            e1a = P.op("vector", lambda e, t1=t1, pD=pD: e.tensor_scalar(out=t1, in0=pD[0:64, 0:1], scalar1=-1.0, scalar2=None, op0=ALU.mult),
                       deps=[mN, a3, cx.hw_free[h % 2]])
            e1b = P.op("vector", lambda e, t1=t1, pD=pD: e.tensor_tensor(out=t1, in0=t1, in1=pD[0:64, 0:1], op=ALU.max), deps=[e1a])
            e1 = P.op("vector", lambda e, t1=t1, h=h: e.tensor_tensor(out=t1, in0=t1, in1=eb[:, h:h + 1], op=ALU.mult), deps=[e1b])
            cx.psfree[2 + pbi] = e1
            e2 = P.op("vector", lambda e, t1=t1: e.tensor_scalar(out=t1, in0=t1, scalar1=1.0, scalar2=None, op0=ALU.max), deps=[e1])
            e3 = P.op("vector", lambda e, t1=t1: e.reciprocal(out=t1, in_=t1), deps=[e2])
            e4 = P.op("vector", lambda e, t1=t1, h=h: e.tensor_tensor(out=t1, in0=t1, in1=eb[:, h:h + 1], op=ALU.mult), deps=[e3])
            junk = cx.junk[0:64, :]
            s1 = P.op("scalar", lambda e, pN=pN, ssq=ssq, junk=junk: e.activation(out=junk, in_=pN[0:64, :], func=AF.Square, accum_out=ssq),
                      deps=[mN, cx.hw_free[h % 2]])
            e5 = P.op("vector", lambda e, t1=t1, t2=t2: e.tensor_tensor(out=t2, in0=t1, in1=t1, op=ALU.mult), deps=[e4])
            e6 = P.op("vector", lambda e, t2=t2, ssq=ssq: e.tensor_tensor(out=t2, in0=t2, in1=ssq, op=ALU.mult), deps=[e5, s1])
            s2 = P.op("scalar", lambda e, t2=t2: e.activation(out=t2, in_=t2, func=AF.Sqrt, bias=cx.epsb[0:64, :], scale=1.0 / DV), deps=[e6])
            e7 = P.op("vector", lambda e, t2=t2: e.reciprocal(out=t2, in_=t2), deps=[s2])
            e8 = P.op("vector", lambda e, rr=rr, t1=t1, t2=t2: e.tensor_tensor(out=rr, in0=t1, in1=t2, op=ALU.mult), deps=[e7])
            e9 = P.op("vector", lambda e, yb=yb, pN=pN, rr=rr, h=h: e.scalar_tensor_tensor(
                out=yb[:, h * DV:(h + 1) * DV], in0=pN[0:64, :], scalar=rr, in1=NGS[0:64, h * DV:(h + 1) * DV], op0=ALU.mult, op1=ALU.mult),
                deps=[e8, d5, y_tr])
            cx.psfree[pbi] = e9
            cx.hw_free[h % 2] = e9
            pT = cx.ps[7]
            for i in range(2):
                mT = P.op("tensor", lambda e, pT=pT, qk=qk, h=h, i=i: e.transpose(
                    pT[0:64, :].bitcast(BF16)[:, i * 128:(i + 1) * 128], qk[:, 16 + 2 * h + i, :], cx.ident[:, :]),
                    deps=([cx.psfree[7]] + pg_readers) if i == 0 else [], sig=(i == 1))
            wk = cx.wkt[0:64, (h % 2) * 256:(h % 2 + 1) * 256]
            f1 = P.op("vector", lambda e, wk=wk, pT=pT, h=h: e.tensor_scalar(out=wk, in0=pT[0:64, :].bitcast(BF16)[:, 0:256], scalar1=eg[:, h:h + 1], scalar2=None,
                                                                            op0=ALU.mult), deps=[mT, a4, cx.wk_free[h % 2]])
            cx.psfree[7] = f1
            pC = [cx.ps[4], cx.ps[5]]
            for i in range(2):
                mC = P.op("tensor", lambda e, pC=pC, wk=wk, vc=vc, h=h, i=i: e.matmul(pC[i][:, :], wk[:, i * 128:(i + 1) * 128], vc[0:64, h * DV:(h + 1) * DV],
                                                                                     start=True, stop=True),
                          deps=[f1, cx.psfree[4 + i]], sig=(i == 1))
            pn = pD
            for i in range(2):
                mn_ = P.op("tensor", lambda e, pn=pn, wk=wk, i=i: e.matmul(pn[:, 8 + i:9 + i], wk[:, i * 128:(i + 1) * 128], cx.ones_bf[0:64, 0:1],
                                                                          start=True, stop=True), deps=[cx.pn_free[pbi]] if i == 0 else [], sig=(i == 1))
            cx.wk_free[h % 2] = mn_
            for i in range(2):
                g_ = 2 * h + i
                u1 = P.op("vector", lambda e, g_=g_, h=h, i=i, pC=pC: e.scalar_tensor_tensor(out=Cf[:, g_, :], in0=Cf[:, g_, :], scalar=decb[:, h:h + 1], in1=pC[i][:, :],
                                                                                           op0=ALU.mult, op1=ALU.add), deps=[mC, a5, mN])
                cx.psfree[4 + i] = u1
                u2 = P.op("scalar", lambda e, g_=g_: e.activation(out=Cb[:, g_, :], in_=Cf[:, g_, :], func=AF.Copy), deps=[u1, mN])
                cb_ready[g_] = u2
            u3 = P.op("vector", lambda e, h=h, pn=pn: e.scalar_tensor_tensor(out=nf[:, 2 * h:2 * h + 2], in0=nf[:, 2 * h:2 * h + 2], scalar=decb[:, h:h + 1],
                                                                            in1=pn[:, 8:10], op0=ALU.mult, op1=ALU.add), deps=[mn_, mN])
            cx.pn_free[pbi] = u3
            u4 = P.op("vector", lambda e, h=h: e.tensor_copy(out=nb[:, 2 * h:2 * h + 2], in_=nf[:, 2 * h:2 * h + 2]), deps=[u3])
            nb_ready = u4
            hd_last = u4
        buf_free[bsel] = hd_last
        ngs_free = hd_last
        for gq in range(4):
            pbi = 6 + (gq % 2)
            pst = cx.ps[pbi][:, :].bitcast(BF16).rearrange("p (k t) -> p k t", k=8)[:, :, 0:64]
            for kk in range(8):
                k = gq * 8 + kk
                mm = P.op("tensor", lambda e, pst=pst, kk=kk, k=k, yb=yb: e.transpose(pst[:, kk, :], yb[:, k * 128:(k + 1) * 128], cx.ident[0:64, 0:64]),
                          deps=[hd_last, cx.psfree[pbi]] if kk == 0 else [], sig=(kk == 7))
            dst = yT[:, gq * 8:(gq + 1) * 8, c * LCH:(c + 1) * LCH]
            cp = P.op("scalar", lambda e, dst=dst, pst=pst: e.activation(out=dst, in_=pst, func=AF.Copy), deps=[mm] + proj_done)
            cx.psfree[pbi] = cp
        y_tr = mm
    y_done = [cp]
    s1 = P.dma("sync", st_c.rearrange("p (g v) -> p g v", v=DV), Cf, "st_st", 2, deps=[hd_last])
    s2 = P.dma("sync", st_n, nf, "st_st", 2, deps=[hd_last])
    t_gt = load_bc(cx, cx.gate, mod_d[0:1, 2 * D:3 * D], "bc3", [hd_last] + list(mod_ready))
    wb_free = [None, None]
    stores = {}
    g2 = 0
    for cb in range(D // 256):
        slot = cb % 2
        wsl = cx.wA[slot]
        t_w = P.dma("gpsimd", wsl, wout_v[:, :, cb * 256:(cb + 1) * 256], "wA", 2, deps=[wb_free[slot], hd_last])
        for tt in range(NT):
            pbi = (g2 % 2)
            g2 += 1
            pb = cx.ps[pbi]
            for k in range(KC):
                mm = P.op("tensor", lambda e, pb=pb, wsl=wsl, k=k, tt=tt: e.matmul(pb[:, 0:256], yT[:, k, tt * 128:(tt + 1) * 128], wsl[:, k, :],
                                                                                  start=(k == 0), stop=(k == KC - 1)),
                          deps=([t_w, cx.psfree[pbi]] + y_done) if k == 0 else [], sig=(k == KC - 1))
            xi = cx.xs_i % 2
            cx.xs_i += 1
            xs = cx.XS[:, xi, 0:256]
            tl = P.dma("sync", xs, x_rows[tt * 128:(tt + 1) * 128, cb * 256:(cb + 1) * 256], "xs_ld", 2, deps=[cx.xs_free[xi]])
            flush_store(cx)
            ti = cx.tmp_i % 2
            cx.tmp_i += 1
            tmp = cx.TMP[:, ti, 0:256]
            v2 = P.op("vector", lambda e, tmp=tmp, pb=pb, cb=cb: e.tensor_tensor(out=tmp, in0=pb[:, 0:256], in1=cx.gate[:, cb * 256:(cb + 1) * 256], op=ALU.mult),
                      deps=[mm, cx.tmp_free[ti], t_gt] + sc_done)
            cx.psfree[pbi] = v2
            v3 = P.op("vector", lambda e, xs=xs, tmp=tmp: e.tensor_tensor(out=xs, in0=xs, in1=tmp, op=ALU.add), deps=[v2, tl])
            cx.tmp_free[ti] = v3
            defer_store(cx, x_rows_out[tt * 128:(tt + 1) * 128, cb * 256:(cb + 1) * 256], xs, v3, xi, stores, (tt, cb))
            last_mm = mm
        wb_free[slot] = last_mm
    flush_store(cx)
    cx.free["RA"] = [last_mm]
    cx.free["RB"] = [s1, hd_last]
    cx.free["RC"] = [wb_free[0], wb_free[1]]
    cx.free["RD"] = [hd_last]
    cx.free["RE"] = [v3]
    cx.free["RF"] = [hd_last]
    return list(stores.values()), [s1, s2]


def build_mlstm_layer(with_ffn=True, final_norm=False):
    nc = bass.Bass("TRN2", target_bir_lowering=False)
    dt = lambda name, shape, kind="ExternalInput": nc.dram_tensor(name, shape, F32, kind=kind).ap()
    x_in = dt("x_in", [TCORE, D])
    flag_d = dt("flag", [128, 1])
    ident_d = nc.dram_tensor("ident", [128, 128], BF16, kind="ExternalInput").ap()
    identf_d = dt("identf", [64, 64])
    cT_in = dt("cT", [128, 32])
    condw = dt("cond_w", [D, R])
    condbT = dt("cond_bT", [128, 4])
    adaw = dt("ada_w", [R, 6 * D])
    adab = dt("ada_b", [1, 6 * D])
    g_mix = dt("g_mix", [1, D])
    g_ffn = dt("g_ffn", [1, D])
    w_in = dt("w_in", [D, NPROJ])
    bg_d = dt("b_gates", [1, 16])
    ng_d = dt("norm_g", [1, D])
    w_out = dt("w_out", [D, D])
    st_c_in = dt("st_c_in", [128, 16 * DV])
    st_n_in = dt("st_n_in", [128, 16])
    wr = dt("w_router", [D, 8])
    br = dt("b_router", [1, 8])
    wg = dt("moe_wg", [8, D, 2048])
    wu = dt("moe_wu", [8, D, 2048])
    wd = dt("moe_wd", [8, 2048, D])
    gfin = dt("g_final", [1, D])
    x_out = dt("x_out", [TCORE, D], kind="ExternalOutput")
    st_c = dt("st_c", [128, 16 * DV], kind="ExternalOutput")
    st_n = dt("st_n", [128, 16], kind="ExternalOutput")
    mod_d = nc.dram_tensor("mod_s", [1, 6 * D], F32).ap()
    qk_s = nc.dram_tensor("qk_s", [32, 128, TP], BF16).ap()
    v_s = nc.dram_tensor("v_s", [TP, D], BF16).ap()
    o_s = nc.dram_tensor("o_s", [TP, D], BF16).ap()
    g_s = nc.dram_tensor("g_s", [TP, 16], F32).ap()

    P = Prog(nc)
    cx = Ctx(nc, P)
    c0 = init_consts(cx, ident_d, identf_d, flag_d)
    mod_ready = phase_mods(cx, cT_in, condw, condbT, adaw, adab, mod_d, c0)
    i1 = P.dma("sync", st_c, st_c_in, "st_cp", 2)
    i2 = P.dma("sync", st_n, st_n_in, "st_cp", 2)
    state_ready = [i1, i2]
    final = []
    for ps_idx in range(NPASS):
        rows = slice(ps_idx * TP, (ps_idx + 1) * TP)
        hT_ready, _ = phase_prep(cx, x_in[rows, :], g_mix, mod_d, 1, 0, 2, c0, mod_ready)
        st, state_ready = phase_mlstm(cx, ps_idx, x_in[rows, :], x_out[rows, :], w_in, w_out, bg_d, ng_d, mod_d, qk_s, v_s, o_s, g_s, st_c, st_n,
                                      hT_ready, mod_ready, state_ready)
        if with_ffn:
            hT_ready, gate_ready = phase_prep(cx, x_out[rows, :], g_ffn, mod_d, 4, 3, 5, st, mod_ready)
            comb_ready = phase_router(cx, wr, br, hT_ready)
            blocks = [(wg[e], wu[e], wd[e], e) for e in range(8)]
            st = phase_ffn(cx, x_out[rows, :], x_out[rows, :], blocks, hT_ready, gate_ready, comb=cx.comb, comb_ready=comb_ready)
            cx.comb_free = list(cx.free["RA"])
        if final_norm:
            st = phase_final_norm(cx, x_out[rows, :], gfin, st)
        final += st
    final += state_ready
    P.emit(final)
    return nc, P


def phase_final_norm(cx, x_rows, g_row, x_ready):
    P = cx.P
    sm = cx.small
    t_g = load_bc(cx, cx.geff, g_row, "bc0", list(x_ready) + cx.free["RD"])
    xt_free = [None, None]
    stores = []
    d_rb = list(x_ready) + cx.free["RB"]
    rf_dep = cx.free["RF"]
    for tt in range(NT):
        b = tt % 2
        xt = cx.xt[b]
        hb = cx.hb[b]
        ss = sm[:, 40 + b:41 + b]
        rs = sm[:, 42 + b:43 + b]
        tl = P.dma("sync", xt, x_rows[tt * 128:(tt + 1) * 128, :], "xt", 2, deps=d_rb + [xt_free[b]])
        t1 = P.op("scalar", lambda e, xt=xt, hb=hb, ss=ss: e.activation(out=hb, in_=xt, func=AF.Square, accum_out=ss), deps=[tl] + rf_dep)
        t2 = P.op("scalar", lambda e, ss=ss, rs=rs: e.activation(out=rs, in_=ss, func=AF.Sqrt, bias=cx.epsb, scale=1.0 / D), deps=[t1])
        t3 = P.op("vector", lambda e, rs=rs: e.reciprocal(out=rs, in_=rs), deps=[t2])
        t4 = P.op("vector", lambda e, xt=xt, rs=rs: e.scalar_tensor_tensor(out=xt, in0=xt, scalar=rs, in1=cx.geff, op0=ALU.mult, op1=ALU.mult), deps=[t3, t_g, tl])
        st = P.dma("sync", x_rows[tt * 128:(tt + 1) * 128, :], xt, "fn_st", 2, deps=[t4])
        xt_free[b] = st
        stores.append(st)
    cx.free["RB"] = stores[-2:]
    cx.free["RD"] = [t4]
    cx.free["RF"] = [t1]
    return stores[-2:]


def mlstm_layer_inputs(I, x, i, core, st_c, st_n, b_override=None):
    j = i // 2
    b, half, xr = _core_rows(x, core)
    m = {
        "x_in": np.ascontiguousarray(xr), "flag": np.full((128, 1), float(half), np.float32),
        "cT": _pmajor(I["c"][b], 32), "cond_w": I["cond_w"], "cond_bT": _pmajor(I["cond_b"], 4),
        "ada_w": I["ada_w"][i], "ada_b": I["ada_b"][i][None, :],
        "g_mix": I["mix_norm_g"][i][None, :], "g_ffn": I["ffn_norm_g"][i][None, :],
        "w_in": I["mlstm_w_in"][j], "b_gates": I["mlstm_b_gates"][j][None, :], "norm_g": I["mlstm_norm_g"][j][None, :],
        "w_out": I["mlstm_w_out"][j], "st_c_in": st_c, "st_n_in": st_n,
        "w_router": I["moe_w_router"][j], "b_router": I["moe_b_router"][j][None, :],
        "moe_wg": I["moe_w_gate"][j], "moe_wu": I["moe_w_up"][j], "moe_wd": I["moe_w_down"][j],
        "g_final": I["final_norm_g"][None, :],
    }
    m.update(_consts())
    return m


_PROG_CACHE = {}


def _get_prog(kind):
    if kind not in _PROG_CACHE:
        if kind == "conv":
            _PROG_CACHE[kind] = build_conv_layer(True)[0]
        elif kind == "mlstm":
            _PROG_CACHE[kind] = build_mlstm_layer(True, False)[0]
        elif kind == "mlstm_final":
            _PROG_CACHE[kind] = build_mlstm_layer(True, True)[0]
        elif kind == "mlstm_state":
            _PROG_CACHE[kind] = build_mlstm_state()[0]
        elif kind == "conv_state":
            _PROG_CACHE[kind] = build_conv_state_layer()[0]
    return _PROG_CACHE[kind]


def _run(nc, in_maps):
    res = run_bass_kernel_spmd(nc, in_maps, core_ids=list(range(len(in_maps))))
    return res.results


def kernel(**inputs):
    I = {k: np.asarray(v) for k, v in inputs.items()}
    x = np.ascontiguousarray(I["x"], dtype=np.float32)
    B = x.shape[0]
    for i in range(4):
        if i % 2 == 0:
            nc = _get_prog("conv")
            maps = [conv_layer_inputs(I, x, i, core) for core in range(NCORES)]
            res = _run(nc, maps)
            xn = np.empty_like(x)
            for core in range(NCORES):
                b, half = core // 2, core % 2
                xn[b, half * TCORE:(half + 1) * TCORE] = res[core]["x_out"]
            x = xn
        else:
            nc = _get_prog("mlstm_final" if i == 3 else "mlstm")
            zc = np.zeros((128, 16 * DV), np.float32)
            zn = np.zeros((128, 16), np.float32)
            maps = [mlstm_layer_inputs(I, x, i, core, zc, zn) for core in range(NCORES)]
            res = _run(nc, maps)
            xn = np.empty_like(x)
            for b in range(B):
                xn[b, 0:TCORE] = res[2 * b]["x_out"]
            maps = [mlstm_layer_inputs(I, x, i, 2 * b + 1, res[2 * b]["st_c"], res[2 * b]["st_n"]) for b in range(B)]
            res2 = _run(nc, maps)
            for b in range(B):
                xn[b, TCORE:2 * TCORE] = res2[b]["x_out"]
            x = xn
    return x


def phase_mlstm_state(cx, ps_idx, w_in, bg_d, qk_d, v_d, g_d, st_c, st_n, hT_ready, state_ready):
    P = cx.P
    sm = cx.small
    win_v = w_in.rearrange("(kc p) c -> p kc c", p=128)
    STG = [cx.TMP[:, i, :].bitcast(BF16) for i in range(2)]
    wa_free = [None, None]
    rc_dep = cx.free["RC"]
    stg_free = [cx.tmp_free[0], cx.tmp_free[1]]
    si = 0
    sc_writes = []
    pi = 0
    pe_last = None
    wi = 0
    for m in range(16, 32):
        slot = wi % 2
        wi += 1
        wsl = cx.wA[slot]
        dd = [wa_free[slot]] + (rc_dep if wi <= 2 else [])
        t_w = P.dma("gpsimd", wsl[:, :, 0:128], win_v[:, :, m * 128:(m + 1) * 128], "wA", 2, deps=dd)
        for n in range(2):
            pbi = pi % 4
            pi += 1
            pb = cx.ps[pbi]
            for k in range(KC):
                mm = P.op("tensor", lambda e, pb=pb, wsl=wsl, k=k, n=n: e.matmul(pb[:, :], wsl[:, k, 0:128], cx.hT[:, k, n * 512:(n + 1) * 512],
                                                                                start=(k == 0), stop=(k == KC - 1)),
                          deps=([t_w, cx.psfree[pbi]] + list(hT_ready)) if k == 0 else [], sig=(k == KC - 1))
            s_ = si % 2
            si += 1
            stg = STG[s_][:, 0:512]
            a1 = P.op("scalar", lambda e, stg=stg, pb=pb: e.activation(out=stg, in_=pb[:, :], func=AF.Copy, scale=DK ** -0.5), deps=[mm, stg_free[s_]])
            cx.psfree[pbi] = a1
            st = P.dma("sync", qk_d[m, :, n * 512:(n + 1) * 512], stg, "sc_st", 4, deps=[a1])
            stg_free[s_] = st
            sc_writes.append(st)
            pe_last = mm
        wa_free[slot] = pe_last
    for cbk in range(16):
        slot = wi % 2
        wi += 1
        wsl = cx.wA[slot]
        t_w = P.dma("gpsimd", wsl, win_v[:, :, 4096 + cbk * 256:4096 + (cbk + 1) * 256], "wA", 2, deps=[wa_free[slot]])
        c0 = cbk * 256
        for tt in range(NT):
            pbi = pi % 4
            pi += 1
            pb = cx.ps[pbi]
            for k in range(KC):
                mm = P.op("tensor", lambda e, pb=pb, wsl=wsl, k=k, tt=tt: e.matmul(pb[:, 0:256], cx.hT[:, k, tt * 128:(tt + 1) * 128], wsl[:, k, :],
                                                                                  start=(k == 0), stop=(k == KC - 1)),
                          deps=[t_w, cx.psfree[pbi]] if k == 0 else [], sig=(k == KC - 1))
            s_ = si % 2
            si += 1
            stg = STG[s_][:, 0:256]
            a1 = P.op("scalar", lambda e, stg=stg, pb=pb: e.activation(out=stg, in_=pb[:, 0:256], func=AF.Copy), deps=[mm, stg_free[s_]])
            cx.psfree[pbi] = a1
            st = P.dma("sync", v_d[tt * 128:(tt + 1) * 128, c0:c0 + 256], stg, "sc_st", 4, deps=[a1])
            stg_free[s_] = st
            sc_writes.append(st)
            pe_last = mm
        wa_free[slot] = pe_last
    WG = cx.WRt[:, 0:KC * 16].rearrange("p (k e) -> p k e", e=16)
    t_wg = P.dma("gpsimd", WG, win_v[:, :, 12288:12304], "wr", 1, deps=cx.comb_free)
    bgb = sm[:, 104:120]
    t_bg = P.dma("sync", bgb, bg_d.to_broadcast([128, 16]), "wrb", 1)
    gst = sm[:, 120:152].rearrange("p (b e) -> p b e", e=16)
    gfree = [None, None]
    for tt in range(NT):
        pbi = pi % 4
        pi += 1
        pb = cx.ps[pbi]
        for k in range(KC):
            mm = P.op("tensor", lambda e, pb=pb, k=k, tt=tt: e.matmul(pb[:, 0:16], cx.hT[:, k, tt * 128:(tt + 1) * 128], WG[:, k, :],
                                                                     start=(k == 0), stop=(k == KC - 1)),
                      deps=[t_wg, cx.psfree[pbi]] if k == 0 else [], sig=(k == KC - 1))
        v0 = P.op("vector", lambda e, pb=pb, tt=tt: e.tensor_tensor(out=gst[:, tt % 2, :], in0=pb[:, 0:16], in1=bgb, op=ALU.add),
                  deps=[mm, t_bg, gfree[tt % 2]])
        cx.psfree[pbi] = v0
        st = P.dma("sync", g_d[tt * 128:(tt + 1) * 128, :], gst[:, tt % 2, :], "sc_st", 4, deps=[v0])
        gfree[tt % 2] = st
        sc_writes.append(st)
        pe_last = mm
    cx.comb_free = [pe_last]
    cx.tmp_free = [stg_free[0], stg_free[1]]
    proj_done = [pe_last]
    sc_done = sc_writes[-4:]
    Cf = cx.RB[:, :].rearrange("p (g v) -> p g v", v=DV)
    QK = [cx.RD[:, 4096 + i * 1024:4096 + (i + 1) * 1024].bitcast(BF16).rearrange("p (m t) -> p m t", t=LCH) for i in range(2)]
    VC = [cx.RC[:, i * 2048:(i + 1) * 2048].bitcast(BF16) for i in range(2)]
    nf = sm[:, 152:168]
    tri = cx.trif[0:64, 0:64]
    onesf = cx.onesf
    free0 = proj_done + cx.free["RB"] + cx.free["RD"]
    if ps_idx == 0:
        t_c = P.op("vector", lambda e: e.memset(cx.RB[:, :], 0.0), deps=free0 + list(state_ready))
        t_n = P.op("vector", lambda e: e.memset(nf, 0.0), deps=free0)
    else:
        t_c = P.dma("sync", Cf, st_c.rearrange("p (g v) -> p g v", v=DV), "st_ld", 2, deps=free0 + list(state_ready))
        t_n = P.dma("sync", nf, st_n, "st_ld", 2, deps=free0 + list(state_ready))
    buf_free = [None, None]
    g_free = None
    G = sm[0:64, 168:184]
    gw = cx.gwork
    hd_last = None
    for c in range(TP // LCH):
        bsel = c % 2
        rows = slice(c * LCH, (c + 1) * LCH)
        qk = QK[bsel]
        vc = VC[bsel]
        ld = []
        for qq in range(2, 4):
            ld.append(P.dma("sync", qk[:, qq * 8:(qq + 1) * 8, :], qk_d[qq * 8:(qq + 1) * 8, :, rows].rearrange("m p t -> p m t"), "ch_ld", 8,
                            deps=sc_done + [buf_free[bsel]] + free0))
        ld.append(P.dma("sync", vc[0:64, :], v_d[rows, :], "ch_ld", 8, deps=sc_done + [buf_free[bsel]]))
        tg = P.dma("sync", G, g_d[rows, :], "g_ld", 1, deps=sc_done + [g_free])
        ig = G[:, 0:8]
        fp = G[:, 8:16]
        ax, ee, ll, lf, aa, ea, eb, eg, dec = (gw[:, i * 8:(i + 1) * 8] for i in range(9))
        d00 = P.op("vector", lambda e: e.tensor_scalar(out=ax, in0=fp, scalar1=-1.0, scalar2=None, op0=ALU.mult), deps=[tg])
        d0 = P.op("vector", lambda e: e.tensor_tensor(out=ax, in0=ax, in1=fp, op=ALU.max), deps=[d00])
        a0 = P.op("scalar", lambda e: e.activation(out=ee, in_=ax, func=AF.Exp, scale=-1.0), deps=[d0])
        a1 = P.op("scalar", lambda e: e.activation(out=ll, in_=ee, func=AF.Ln, bias=cx.oneb[0:64, :], scale=1.0), deps=[a0])
        d1 = P.op("vector", lambda e: e.tensor_scalar(out=lf, in0=fp, scalar1=0.0, scalar2=None, op0=ALU.min), deps=[a1])
        d2 = P.op("vector", lambda e: e.tensor_tensor(out=lf, in0=lf, in1=ll, op=ALU.subtract), deps=[d1])
        pg = cx.ps[7]
        m0 = P.op("tensor", lambda e: e.matmul(pg[0:64, 0:8], tri, lf, start=True, stop=True), deps=[d2, cx.psfree[7]], sig=False)
        m1 = P.op("tensor", lambda e: e.matmul(pg[:, 8:16], onesf[0:64, :], lf, start=True, stop=True), deps=[])
        d3 = P.op("vector", lambda e: e.tensor_tensor(out=aa, in0=ig, in1=pg[0:64, 0:8], op=ALU.subtract), deps=[m1])
        d4 = P.op("vector", lambda e: e.tensor_tensor(out=aa, in0=aa, in1=pg[0:64, 8:16], op=ALU.add), deps=[d3])
        a4 = P.op("scalar", lambda e: e.activation(out=eg, in_=aa, func=AF.Exp), deps=[d4])
        decb = cx.decb[:, :]
        a5 = P.op("scalar", lambda e: e.activation(out=decb, in_=pg[:, 8:16], func=AF.Exp), deps=[m1])
        cx.psfree[7] = a5
        pg_readers = [a5, d4]
        for h in range(H):
            pbi = h % 2
            pT = cx.ps[7]
            for i in range(2):
                mT = P.op("tensor", lambda e, pT=pT, qk=qk, h=h, i=i: e.transpose(
                    pT[0:64, :].bitcast(BF16)[:, i * 128:(i + 1) * 128], qk[:, 16 + 2 * h + i, :], cx.ident[:, :]),
                    deps=([cx.psfree[7]] + pg_readers + ld[0:2]) if i == 0 else [], sig=(i == 1))
            wk = cx.wkt[0:64, (h % 2) * 256:(h % 2 + 1) * 256]
            f1 = P.op("vector", lambda e, wk=wk, pT=pT, h=h: e.tensor_scalar(out=wk, in0=pT[0:64, :].bitcast(BF16)[:, 0:256], scalar1=eg[:, h:h + 1], scalar2=None,
                                                                            op0=ALU.mult), deps=[mT, a4, cx.wk_free[h % 2]])
            cx.psfree[7] = f1
            pC = [cx.ps[4], cx.ps[5]]
            for i in range(2):
                mC = P.op("tensor", lambda e, pC=pC, wk=wk, vc=vc, h=h, i=i: e.matmul(pC[i][:, :], wk[:, i * 128:(i + 1) * 128], vc[0:64, h * DV:(h + 1) * DV],
                                                                                     start=True, stop=True),
                          deps=[f1, cx.psfree[4 + i], ld[2]], sig=(i == 1))
            pn = cx.ps[2 + pbi]
            for i in range(2):
                mn_ = P.op("tensor", lambda e, pn=pn, wk=wk, i=i: e.matmul(pn[:, 8 + i:9 + i], wk[:, i * 128:(i + 1) * 128], cx.ones_bf[0:64, 0:1],
                                                                          start=True, stop=True), deps=[cx.pn_free[pbi], cx.psfree[2 + pbi]] if i == 0 else [], sig=(i == 1))
            cx.wk_free[h % 2] = mn_
            for i in range(2):
                g_ = 2 * h + i
                u1 = P.op("vector", lambda e, g_=g_, h=h, i=i, pC=pC: e.scalar_tensor_tensor(out=Cf[:, g_, :], in0=Cf[:, g_, :], scalar=decb[:, h:h + 1], in1=pC[i][:, :],
                                                                                           op0=ALU.mult, op1=ALU.add), deps=[mC, a5, t_c])
                cx.psfree[4 + i] = u1
            u3 = P.op("vector", lambda e, h=h, pn=pn: e.scalar_tensor_tensor(out=nf[:, 2 * h:2 * h + 2], in0=nf[:, 2 * h:2 * h + 2], scalar=decb[:, h:h + 1],
                                                                            in1=pn[:, 8:10], op0=ALU.mult, op1=ALU.add), deps=[mn_, t_n])
            cx.pn_free[pbi] = u3
            cx.psfree[2 + pbi] = u3
            hd_last = u3
        buf_free[bsel] = hd_last
        g_free = hd_last
    s1 = P.dma("sync", st_c.rearrange("p (g v) -> p g v", v=DV), Cf, "st_st", 2, deps=[hd_last])
    s2 = P.dma("sync", st_n, nf, "st_st", 2, deps=[hd_last])
    cx.free["RA"] = proj_done
    cx.free["RB"] = [s1, hd_last]
    cx.free["RC"] = [hd_last, wa_free[0], wa_free[1]]
    cx.free["RD"] = [hd_last]
    return [s1, s2]


def build_conv_state_layer():
    nc = bass.Bass("TRN2", target_bir_lowering=False)
    dt = lambda name, shape, kind="ExternalInput": nc.dram_tensor(name, shape, F32, kind=kind).ap()
    x_in = dt("x_in", [TCORE, D])
    x_halo = dt("x_halo", [128, D])
    flag_d = dt("flag", [128, 1])
    ident_d = nc.dram_tensor("ident", [128, 128], BF16, kind="ExternalInput").ap()
    identf_d = dt("identf", [64, 64])
    cT_in = dt("cT", [128, 32])
    condw = dt("cond_w", [D, R])
    condbT = dt("cond_bT", [128, 4])
    adaw = dt("ada_w", [R, 6 * D])
    adab = dt("ada_b", [1, 6 * D])
    g_mix = dt("g_mix", [1, D])
    g_ffn = dt("g_ffn", [1, D])
    w_in = dt("w_in", [D, 2 * D])
    cpar_d = dt("cpar", [128, 160 + 32 * CW])
    w_out = dt("w_out", [D, D])
    b_out = dt("b_out", [1, D])
    wg = dt("ffn_wg", [D, 2 * D])
    wu = dt("ffn_wu", [D, 2 * D])
    wd = dt("ffn_wd", [2 * D, D])
    adaw2 = dt("ada_w2", [R, 6 * D])
    adab2 = dt("ada_b2", [1, 6 * D])
    g_mix2 = dt("g_mix2", [1, D])
    ml_w_in = dt("ml_w_in", [D, NPROJ])
    bg_d = dt("b_gates", [1, 16])
    x_out = dt("x_out", [TCORE, D], kind="ExternalOutput")
    st_c = dt("st_c", [128, 16 * DV], kind="ExternalOutput")
    st_n = dt("st_n", [128, 16], kind="ExternalOutput")
    mod_d = nc.dram_tensor("mod_s", [1, 6 * D], F32).ap()
    mod2 = nc.dram_tensor("mod_s2", [1, 6 * D], F32).ap()
    cT_s = nc.dram_tensor("cT_s", [32, 128, TP], F32).ap()
    hs_s = nc.dram_tensor("hs_s", [32, 128, 32], F32).ap()
    qk_s = nc.dram_tensor("qk_s", [32, 128, TP], BF16).ap()
    v_s = nc.dram_tensor("v_s", [TP, D], BF16).ap()
    g_s = nc.dram_tensor("g_s", [TP, 16], F32).ap()

    P = Prog(nc)
    cx = Ctx(nc, P)
    c0 = init_consts(cx, ident_d, identf_d, flag_d)
    mod_ready = phase_mods(cx, cT_in, condw, condbT, adaw, adab, mod_d, c0)
    final = []
    for ps_idx in range(NPASS):
        rows = slice(ps_idx * TP, (ps_idx + 1) * TP)
        hh_ready = []
        if ps_idx == 0:
            hh_ready, _ = phase_prep(cx, x_halo, g_mix, mod_d, 1, 0, 2, c0, mod_ready, n_tiles=1, halo=True)
        hT_ready, gate_ready = phase_prep(cx, x_in[rows, :], g_mix, mod_d, 1, 0, 2, c0, mod_ready)
        cpar_ready = P.dma("sync", cx.RF[:, 0:160 + 32 * CW], cpar_d, "cpar", 1, deps=cx.free["RF"])
        st = phase_conv(cx, ps_idx, x_in[rows, :], x_out[rows, :], w_in, w_out, b_out, cT_s, hs_s, hT_ready, gate_ready, cpar_ready, hh_ready)
        hT_ready, gate_ready = phase_prep(cx, x_out[rows, :], g_ffn, mod_d, 4, 3, 5, st, mod_ready)
        blocks = [(wg[:, j * 2048:(j + 1) * 2048], wu[:, j * 2048:(j + 1) * 2048], wd[j * 2048:(j + 1) * 2048, :], None) for j in range(4)]
        st = phase_ffn(cx, x_out[rows, :], x_out[rows, :], blocks, hT_ready, gate_ready)
        final += st
    mod_ready2 = phase_mods(cx, cT_in, condw, condbT, adaw2, adab2, mod2, c0 + final, nblk=8)
    state_ready = []
    for ps_idx in range(NPASS):
        rows = slice(ps_idx * TP, (ps_idx + 1) * TP)
        hT_ready, _ = phase_prep(cx, x_out[rows, :], g_mix2, mod2, 1, 0, 2, final, mod_ready2)
        state_ready = phase_mlstm_state(cx, ps_idx, ml_w_in, bg_d, qk_s, v_s, g_s, st_c, st_n, hT_ready, state_ready)
    P.emit(final + state_ready)
    return nc, P


def build_mlstm_state():
    nc = bass.Bass("TRN2", target_bir_lowering=False)
    dt = lambda name, shape, kind="ExternalInput": nc.dram_tensor(name, shape, F32, kind=kind).ap()
    x_in = dt("x_in", [TCORE, D])
    flag_d = dt("flag", [128, 1])
    ident_d = nc.dram_tensor("ident", [128, 128], BF16, kind="ExternalInput").ap()
    identf_d = dt("identf", [64, 64])
    cT_in = dt("cT", [128, 32])
    condw = dt("cond_w", [D, R])
    condbT = dt("cond_bT", [128, 4])
    adaw = dt("ada_w", [R, 6 * D])
    adab = dt("ada_b", [1, 6 * D])
    g_mix = dt("g_mix", [1, D])
    w_in = dt("w_in", [D, NPROJ])
    bg_d = dt("b_gates", [1, 16])
    st_c = dt("st_c", [128, 16 * DV], kind="ExternalOutput")
    st_n = dt("st_n", [128, 16], kind="ExternalOutput")
    mod_d = nc.dram_tensor("mod_s", [1, 6 * D], F32).ap()
    qk_s = nc.dram_tensor("qk_s", [32, 128, TP], BF16).ap()
    v_s = nc.dram_tensor("v_s", [TP, D], BF16).ap()
    g_s = nc.dram_tensor("g_s", [TP, 16], F32).ap()
    P = Prog(nc)
    cx = Ctx(nc, P)
    c0 = init_consts(cx, ident_d, identf_d, flag_d)
    mod_ready = phase_mods(cx, cT_in, condw, condbT, adaw, adab, mod_d, c0)
    state_ready = []
    for ps_idx in range(NPASS):
        rows = slice(ps_idx * TP, (ps_idx + 1) * TP)
        hT_ready, _ = phase_prep(cx, x_in[rows, :], g_mix, mod_d, 1, 0, 2, c0, mod_ready)
        state_ready = phase_mlstm_state(cx, ps_idx, w_in, bg_d, qk_s, v_s, g_s, st_c, st_n, hT_ready, state_ready)
    P.emit(state_ready)
    return nc, P


def mlstm_state_inputs(I, x, i, core):
    j = i // 2
    b, half, xr = _core_rows(x, core)
    m = {
        "x_in": np.ascontiguousarray(xr), "flag": np.full((128, 1), float(half), np.float32),
        "cT": _pmajor(I["c"][b], 32), "cond_w": I["cond_w"], "cond_bT": _pmajor(I["cond_b"], 4),
        "ada_w": I["ada_w"][i], "ada_b": I["ada_b"][i][None, :], "g_mix": I["mix_norm_g"][i][None, :],
        "w_in": I["mlstm_w_in"][j], "b_gates": I["mlstm_b_gates"][j][None, :],
    }
    m.update(_consts())
    return m


F_NPASS = 4
F_T = F_NPASS * TP


def build_fused(n_layers=4, layers=None, npass=None):
    layers = list(range(n_layers)) if layers is None else list(layers)
    npass = F_NPASS if npass is None else npass
    nc = bass.Bass("TRN2", target_bir_lowering=False)
    dt = lambda name, shape, kind="ExternalInput": nc.dram_tensor(name, shape, F32, kind=kind).ap()
    x_in = dt("x_in", [F_T, D])
    flag_d = dt("flag", [128, 1])
    ident_d = nc.dram_tensor("ident", [128, 128], BF16, kind="ExternalInput").ap()
    identf_d = dt("identf", [64, 64])
    cT_in = dt("cT", [128, 32])
    condw = dt("cond_w", [D, R])
    condbT = dt("cond_bT", [128, 4])
    adaw = dt("ada_w", [4, R, 6 * D])
    adab = dt("ada_b", [4, 6 * D])
    g_mix = dt("g_mix", [4, D])
    g_ffn = dt("g_ffn", [4, D])
    gfin = dt("g_final", [1, D])
    c_w_in = dt("conv_w_in", [2, D, 2 * D])
    c_par = dt("conv_par", [2, 128, 160 + 32 * CW])
    c_w_out = dt("conv_w_out", [2, D, D])
    c_b_out = dt("conv_b_out", [2, D])
    f_wg = dt("ffn_wg", [2, D, 2 * D])
    f_wu = dt("ffn_wu", [2, D, 2 * D])
    f_wd = dt("ffn_wd", [2, 2 * D, D])
    m_w_in = dt("ml_w_in", [2, D, NPROJ])
    m_bg = dt("ml_bg", [2, 16])
    m_ng = dt("ml_ng", [2, D])
    m_w_out = dt("ml_w_out", [2, D, D])
    e_wr = dt("moe_wr", [2, D, 8])
    e_br = dt("moe_br", [2, 8])
    e_wg = dt("moe_wg", [2, 8, D, 2048])
    e_wu = dt("moe_wu", [2, 8, D, 2048])
    e_wd = dt("moe_wd", [2, 8, 2048, D])
    x_out = dt("x_out", [F_T, D], kind="ExternalOutput")
    mod_s = nc.dram_tensor("mod_s", [4, 6 * D], F32).ap()
    cT_s = nc.dram_tensor("cT_s", [32, 128, TP], F32).ap()
    hs_s = nc.dram_tensor("hs_s", [32, 128, 32], F32).ap()
    qk_s = nc.dram_tensor("qk_s", [32, 128, TP], BF16).ap()
    v_s = nc.dram_tensor("v_s", [TP, D], BF16).ap()
    o_s = nc.dram_tensor("o_s", [TP, D], BF16).ap()
    g_s = nc.dram_tensor("g_s", [TP, 16], F32).ap()
    st_c = nc.dram_tensor("st_c", [128, 16 * DV], F32).ap()
    st_n = nc.dram_tensor("st_n", [128, 16], F32).ap()

    P = Prog(nc)
    cx = Ctx(nc, P)
    cx.zero_state = True
    c0 = init_consts(cx, ident_d, identf_d, flag_d)
    mod_ready = []
    for i in range(4):
        mod_ready = phase_mods(cx, cT_in, condw, condbT, adaw[i], adab[i:i + 1, :], mod_s[i:i + 1, :], c0 + mod_ready)
    x_ready = list(c0)
    final = []
    for li, i in enumerate(layers):
        j = i // 2
        mod_d = mod_s[i:i + 1, :]
        src = x_in if li == 0 else x_out
        layer_tokens = []
        state_ready = []
        for ps_idx in range(npass):
            rows = slice(ps_idx * TP, (ps_idx + 1) * TP)
            hT_ready, gate_ready = phase_prep(cx, src[rows, :], g_mix[i:i + 1, :], mod_d, 1, 0, 2, x_ready, mod_ready)
            if i % 2 == 0:
                cx.halo_mode = "zero" if ps_idx == 0 else "load"
                cx.save_halo = ps_idx < npass - 1
                cpar_ready = P.dma("sync", cx.RF[:, 0:160 + 32 * CW], c_par[j], "cpar", 1, deps=cx.free["RF"])
                st = phase_conv(cx, ps_idx, src[rows, :], x_out[rows, :], c_w_in[j], c_w_out[j], c_b_out[j:j + 1, :], cT_s, hs_s,
                                hT_ready, gate_ready, cpar_ready, [])
                hT_ready, gate_ready = phase_prep(cx, x_out[rows, :], g_ffn[i:i + 1, :], mod_d, 4, 3, 5, st, mod_ready)
                blocks = [(f_wg[j][:, q * 2048:(q + 1) * 2048], f_wu[j][:, q * 2048:(q + 1) * 2048], f_wd[j][q * 2048:(q + 1) * 2048, :], None)
                          for q in range(4)]
                st = phase_ffn(cx, x_out[rows, :], x_out[rows, :], blocks, hT_ready, gate_ready)
            else:
                st, state_ready = phase_mlstm(cx, ps_idx, src[rows, :], x_out[rows, :], m_w_in[j], m_w_out[j], m_bg[j:j + 1, :], m_ng[j:j + 1, :],
                                              mod_d, qk_s, v_s, o_s, g_s, st_c, st_n, hT_ready, mod_ready, state_ready)
                hT_ready, gate_ready = phase_prep(cx, x_out[rows, :], g_ffn[i:i + 1, :], mod_d, 4, 3, 5, st, mod_ready)
                comb_ready = phase_router(cx, e_wr[j], e_br[j:j + 1, :], hT_ready)
                blocks = [(e_wg[j, e], e_wu[j, e], e_wd[j, e], e) for e in range(8)]
                st = phase_ffn(cx, x_out[rows, :], x_out[rows, :], blocks, hT_ready, gate_ready, comb=cx.comb, comb_ready=comb_ready)
                cx.comb_free = list(cx.free["RA"])
                if i == 3:
                    st = phase_final_norm(cx, x_out[rows, :], gfin, st)
            layer_tokens += st
            if i == 3:
                final += st
        x_ready = layer_tokens + state_ready
    P.emit(final + x_ready)
    return nc, P


def fused_inputs(I, b):
    def cpar(j):
        return np.concatenate([
            _pmajor(I["conv_b_in"][j][:D], 32), _pmajor(I["conv_b_in"][j][D:], 32), _pmajor(I["conv_b_dw"][j], 32),
            _pmajor(I["conv_ln_g"][j], 32), _pmajor(I["conv_ln_b"][j], 32),
            np.ascontiguousarray(np.asarray(I["conv_w_dw"][j], np.float32).reshape(CW, 32, 128).transpose(2, 1, 0)).reshape(128, 32 * CW),
        ], axis=1)
    m = {
        "x_in": np.ascontiguousarray(I["x"][b]), "flag": np.zeros((128, 1), np.float32),
        "cT": _pmajor(I["c"][b], 32), "cond_w": I["cond_w"], "cond_bT": _pmajor(I["cond_b"], 4),
        "ada_w": I["ada_w"], "ada_b": I["ada_b"], "g_mix": I["mix_norm_g"], "g_ffn": I["ffn_norm_g"], "g_final": I["final_norm_g"][None, :],
        "conv_w_in": I["conv_w_in"], "conv_par": np.ascontiguousarray(np.stack([cpar(0), cpar(1)])), "conv_w_out": I["conv_w_out"],
        "conv_b_out": I["conv_b_out"], "ffn_wg": I["ffn_w_gate"], "ffn_wu": I["ffn_w_up"], "ffn_wd": I["ffn_w_down"],
        "ml_w_in": I["mlstm_w_in"], "ml_bg": I["mlstm_b_gates"], "ml_ng": I["mlstm_norm_g"], "ml_w_out": I["mlstm_w_out"],
        "moe_wr": I["moe_w_router"], "moe_br": I["moe_b_router"], "moe_wg": I["moe_w_gate"], "moe_wu": I["moe_w_up"], "moe_wd": I["moe_w_down"],
    }
    m.update(_consts())
    return m


def kernel(**inputs):
    I = {k: np.asarray(v) for k, v in inputs.items()}
    x = np.ascontiguousarray(I["x"], dtype=np.float32)
    B = x.shape[0]
    states = None
    for i in range(4):
        xn = np.empty_like(x)
        if i % 2 == 0:
            nc = _get_prog("conv_state")
            j2 = (i + 1) // 2
            maps = []
            for core in range(NCORES):
                m = conv_layer_inputs(I, x, i, core)
                m.update({"ada_w2": I["ada_w"][i + 1], "ada_b2": I["ada_b"][i + 1][None, :], "g_mix2": I["mix_norm_g"][i + 1][None, :],
                          "ml_w_in": I["mlstm_w_in"][j2], "b_gates": I["mlstm_b_gates"][j2][None, :]})
                maps.append(m)
            res = _run(nc, maps)
            states = [(res[2 * b]["st_c"], res[2 * b]["st_n"]) for b in range(B)]
        else:
            nc = _get_prog("mlstm_final" if i == 3 else "mlstm")
            zc = np.zeros((128, 16 * DV), np.float32)
            zn = np.zeros((128, 16), np.float32)
            maps = []
            for core in range(NCORES):
                b, half = core // 2, core % 2
                maps.append(mlstm_layer_inputs(I, x, i, core, states[b][0] if half else zc, states[b][1] if half else zn))
            res = _run(nc, maps)
        for core in range(NCORES):
            b, half = core // 2, core % 2
            xn[b, half * TCORE:(half + 1) * TCORE] = res[core]["x_out"]
        x = xn
    return x
```
